# Optimizing a Trainium2 kernel written in Bass

```python
import jax, jax.numpy as jnp
from jax import lax
import numpy as np

D_MODEL = 2048
BATCH = 8
SEQ = 2048
DEPTH = 2

HEAD_DIM = 128
DSA_HEADS = 6
SB_HEADS = 5
MOBA_HEADS = 5
N_HEADS = DSA_HEADS + SB_HEADS + MOBA_HEADS
MIX_WIDTH = N_HEADS * HEAD_DIM
IDX_HEADS = 16
IDX_DIM = 64
DSA_TOPK = 256
MOBA_BLOCK = 256
MOBA_TOPK = 3
Q_BLOCK = 128
MOBA_Q_BLOCK = 16
PEER_HEADS = 8
PEER_NKEYS = 128
PEER_EXPERTS = PEER_NKEYS * PEER_NKEYS
PEER_DKEY = 256
PEER_TOPK = 16
PEER_TOK_BLOCK = 128
ROPE_THETA = 10000.0
EPS = 1e-6
IN_SPLITS = (DSA_HEADS * HEAD_DIM, HEAD_DIM, HEAD_DIM,
             IDX_HEADS * IDX_DIM, IDX_DIM, IDX_HEADS,
             SB_HEADS * HEAD_DIM, SB_HEADS * HEAD_DIM, SB_HEADS * HEAD_DIM,
             MOBA_HEADS * HEAD_DIM, MOBA_HEADS * HEAD_DIM, MOBA_HEADS * HEAD_DIM)
IN_WIDTH = sum(IN_SPLITS)

kernel_name = 'hybrid_dsa_stickbreak_moba_peer_adaln'


def _rmsnorm(x, g):
    xf = x.astype(jnp.float32)
    y = xf * lax.rsqrt(jnp.mean(xf * xf, axis=-1, keepdims=True) + EPS)
    return (y * g.astype(jnp.float32)).astype(x.dtype)


def _rope(x):
    S, d = x.shape[1], x.shape[-1]
    half = d // 2
    inv = ROPE_THETA ** (-jnp.arange(half, dtype=jnp.float32) / half)
    ang = jnp.arange(S, dtype=jnp.float32)[:, None] * inv[None, :]
    cos = jnp.cos(ang)[None, :, None, :]
    sin = jnp.sin(ang)[None, :, None, :]
    xf = x.astype(jnp.float32)
    x1, x2 = xf[..., :half], xf[..., half:]
    return jnp.concatenate([x1 * cos - x2 * sin, x2 * cos + x1 * sin], axis=-1).astype(x.dtype)


def _unblock(y):
    n, B, T = y.shape[:3]
    return jnp.moveaxis(y, 0, 1).reshape(B, n * T, -1)


def _dsa_attention(q, k, v, iq, ik, iw):
    B, S, H, d = q.shape
    topk = min(DSA_TOPK, S // 4)
    kpos = jnp.arange(S)
    idx_scale = IDX_DIM ** -0.5
    w_scale = IDX_HEADS ** -0.5

    def block(i):
        t0 = i * Q_BLOCK
        qb = lax.dynamic_slice_in_dim(q, t0, Q_BLOCK, axis=1)
        iqb = lax.dynamic_slice_in_dim(iq, t0, Q_BLOCK, axis=1)
        iwb = lax.dynamic_slice_in_dim(iw, t0, Q_BLOCK, axis=1)
        qpos = t0 + jnp.arange(Q_BLOCK)
        rel = jax.nn.relu(jnp.einsum('bthe,bse->bths', iqb, ik).astype(jnp.float32) * idx_scale)
        score = jnp.einsum('bth,bths->bts', iwb.astype(jnp.float32) * w_scale, rel)
        score = jnp.where(kpos[None, None, :] <= qpos[None, :, None], score, -jnp.inf)
        _, sel = lax.top_k(score, topk)
        valid = sel <= qpos[None, :, None]
        kg = jax.vmap(lambda kk, ss: kk[ss])(k, sel)
        vg = jax.vmap(lambda vv, ss: vv[ss])(v, sel)
        s = jnp.einsum('bthd,btkd->bthk', qb, kg).astype(jnp.float32) * d ** -0.5
        s = jnp.where(valid[:, :, None, :], s, -jnp.inf)
        p = jax.nn.softmax(s, axis=-1).astype(v.dtype)
        return jnp.einsum('bthk,btkd->bthd', p, vg)

    return _unblock(lax.map(block, jnp.arange(S // Q_BLOCK)))


def _stick_breaking_attention(q, k, v):
    B, S, H, d = q.shape
    kpos = jnp.arange(S)

    def block(i):
        t0 = i * Q_BLOCK
        qb = lax.dynamic_slice_in_dim(q, t0, Q_BLOCK, axis=1)
        qpos = t0 + jnp.arange(Q_BLOCK)
        z = jnp.einsum('bthd,bshd->bhts', qb, k).astype(jnp.float32) * d ** -0.5
        strict = kpos[None, :] < qpos[:, None]
        log_keep = jnp.where(strict, jax.nn.log_sigmoid(-z), 0.0)
        log_between = lax.cumsum(log_keep, axis=3, reverse=True) - log_keep
        a = jnp.where(strict, jnp.exp(jax.nn.log_sigmoid(z) + log_between), 0.0)
        return jnp.einsum('bhts,bshd->bthd', a.astype(v.dtype), v)

    return _unblock(lax.map(block, jnp.arange(S // Q_BLOCK)))


def _moba_attention(q, k, v):
    B, S, H, d = q.shape
    nkb = -(-S // MOBA_BLOCK)
    pad = nkb * MOBA_BLOCK - S

    def to_blocks(t):
        t = jnp.pad(t, ((0, 0), (0, pad), (0, 0), (0, 0)))
        return t.reshape(B, nkb, MOBA_BLOCK, H, d).transpose(0, 3, 1, 2, 4)

    kb, vb = to_blocks(k), to_blocks(v)
    kmean = jnp.mean(kb.astype(jnp.float32), axis=3)
    topk = min(MOBA_TOPK, nkb - 1)
    scale = d ** -0.5
    blk_pos = jnp.arange(MOBA_BLOCK)
    bi = jnp.arange(B)[:, None, None, None]
    hi = jnp.arange(H)[None, :, None, None]

    def block(i):
        t0 = i * MOBA_Q_BLOCK
        qb = lax.dynamic_slice_in_dim(q, t0, MOBA_Q_BLOCK, axis=1).transpose(0, 2, 1, 3)
        qpos = t0 + jnp.arange(MOBA_Q_BLOCK)
        own = t0 // MOBA_BLOCK
        k_own = lax.dynamic_index_in_dim(kb, own, axis=2, keepdims=False)
        v_own = lax.dynamic_index_in_dim(vb, own, axis=2, keepdims=False)
        s_own = jnp.einsum('bhtd,bhpd->bhtp', qb, k_own).astype(jnp.float32) * scale
        s_own = jnp.where(own * MOBA_BLOCK + blk_pos[None, :] <= qpos[:, None], s_own, -jnp.inf)
        if topk == 0:
            p_own = jax.nn.softmax(s_own, axis=-1).astype(v.dtype)
            out = jnp.einsum('bhtp,bhpd->bhtd', p_own, v_own)
        else:
            gate = jnp.einsum('bhtd,bhnd->bhtn', qb.astype(jnp.float32), kmean)
            gate = jnp.where(jnp.arange(nkb) < own, gate, -jnp.inf)
            _, sel = lax.top_k(gate, topk)
            valid = sel < own
            kg = kb[bi, hi, sel]
            vg = vb[bi, hi, sel]
            s_sel = jnp.einsum('bhtd,bhtkpd->bhtkp', qb, kg).astype(jnp.float32) * scale
            s_sel = jnp.where(valid[..., None], s_sel, -jnp.inf)
            n_sel = topk * MOBA_BLOCK
            s_all = jnp.concatenate([s_sel.reshape(B, H, MOBA_Q_BLOCK, n_sel), s_own], axis=-1)
            p = jax.nn.softmax(s_all, axis=-1).astype(v.dtype)
            p_sel = p[..., :n_sel].reshape(B, H, MOBA_Q_BLOCK, topk, MOBA_BLOCK)
            out = (jnp.einsum('bhtkp,bhtkpd->bhtd', p_sel, vg)
                   + jnp.einsum('bhtp,bhpd->bhtd', p[..., n_sel:], v_own))
        return out.transpose(0, 2, 1, 3)

    return _unblock(lax.map(block, jnp.arange(S // MOBA_Q_BLOCK)))


def _mixer(h, w_in, out_g, w_out):
    B, S, _ = h.shape
    offsets = np.cumsum(IN_SPLITS)[:-1].tolist()
    q_a, k_a, v_a, iq, ik, iw, q_b, k_b, v_b, q_c, k_c, v_c = jnp.split(h @ w_in, offsets, axis=-1)
    heads = lambda t, n: t.reshape(B, S, n, -1)
    y_a = _dsa_attention(_rope(heads(q_a, DSA_HEADS)), _rope(k_a[:, :, None, :])[:, :, 0], v_a,
                         _rope(heads(iq, IDX_HEADS)), _rope(ik[:, :, None, :])[:, :, 0], iw)
    y_b = _stick_breaking_attention(heads(q_b, SB_HEADS), heads(k_b, SB_HEADS), heads(v_b, SB_HEADS))
    y_c = _moba_attention(_rope(heads(q_c, MOBA_HEADS)), _rope(heads(k_c, MOBA_HEADS)), heads(v_c, MOBA_HEADS))
    y = jnp.concatenate([y_a, y_b, y_c], axis=-1).reshape(B, S, N_HEADS, HEAD_DIM)
    y = _rmsnorm(y, out_g.reshape(N_HEADS, HEAD_DIM)).reshape(B, S, MIX_WIDTH)
    return y @ w_out


def _peer(h, w_q, sub_keys, expert_u, expert_v):
    B, S, D = h.shape
    T = B * S
    ht = h.reshape(T, D)
    q = (ht @ w_q).reshape(T, PEER_HEADS, 2, PEER_DKEY // 2)
    s = jnp.einsum('tpce,pcne->tpcn', q, sub_keys).astype(jnp.float32)
    sv, si = lax.top_k(s, PEER_TOPK)
    cand_s = (sv[:, :, 0, :, None] + sv[:, :, 1, None, :]).reshape(T, PEER_HEADS, PEER_TOPK * PEER_TOPK)
    cand_i = (si[:, :, 0, :, None] * PEER_NKEYS + si[:, :, 1, None, :]).reshape(T, PEER_HEADS, PEER_TOPK * PEER_TOPK)
    top_s, pick = lax.top_k(cand_s, PEER_TOPK)
    experts = jnp.take_along_axis(cand_i, pick, axis=-1)
    gates = jax.nn.softmax(top_s, axis=-1).astype(h.dtype)
    nb = T // PEER_TOK_BLOCK

    def block(args):
        xb, eb, gb = args
        act = jax.nn.gelu(jnp.einsum('td,tpkd->tpk', xb, expert_u[eb]), approximate=False)
        return jnp.einsum('tpk,tpkd->td', gb * act, expert_v[eb])

    out = lax.map(block, (ht.reshape(nb, PEER_TOK_BLOCK, D),
                          experts.reshape(nb, PEER_TOK_BLOCK, PEER_HEADS, PEER_TOPK),
                          gates.reshape(nb, PEER_TOK_BLOCK, PEER_HEADS, PEER_TOPK)))
    return out.reshape(B, S, D)


def setup_inputs(seed: int = 0) -> dict:
    key = jax.random.key(seed)
    ks = jax.random.split(key, 14)
    nrm = lambda k, shape, s: jax.random.normal(k, shape, jnp.float32) * s
    return {
        'x': nrm(ks[0], (BATCH, SEQ, D_MODEL), 1.0),
        'c': nrm(ks[1], (BATCH, D_MODEL), 1.0),
        'ada_w': nrm(ks[2], (DEPTH, D_MODEL, 6 * D_MODEL), 0.3 * D_MODEL ** -0.5),
        'ada_b': nrm(ks[3], (DEPTH, 6 * D_MODEL), 0.02),
        'norm1_g': 1.0 + nrm(ks[4], (DEPTH, D_MODEL), 0.02),
        'norm2_g': 1.0 + nrm(ks[5], (DEPTH, D_MODEL), 0.02),
        'w_in': nrm(ks[6], (DEPTH, D_MODEL, IN_WIDTH), D_MODEL ** -0.5),
        'out_norm_g': 1.0 + nrm(ks[7], (DEPTH, MIX_WIDTH), 0.02),
        'w_out': nrm(ks[8], (DEPTH, MIX_WIDTH, D_MODEL), MIX_WIDTH ** -0.5),
        'peer_wq': nrm(ks[9], (DEPTH, D_MODEL, PEER_HEADS * PEER_DKEY), D_MODEL ** -0.5),
        'peer_subkeys': nrm(ks[10], (DEPTH, PEER_HEADS, 2, PEER_NKEYS, PEER_DKEY // 2), (PEER_DKEY // 2) ** -0.5),
        'peer_u': nrm(ks[11], (DEPTH, PEER_EXPERTS, D_MODEL), D_MODEL ** -0.5),
        'peer_v': nrm(ks[12], (DEPTH, PEER_EXPERTS, D_MODEL), 0.5),
        'final_g': 1.0 + nrm(ks[13], (D_MODEL,), 0.02),
    }


def reference(x, c, ada_w, ada_b, norm1_g, norm2_g, w_in, out_norm_g, w_out,
              peer_wq, peer_subkeys, peer_u, peer_v, final_g):
    cond = jax.nn.silu(c)
    for l in range(DEPTH):
        mod = (cond @ ada_w[l] + ada_b[l])[:, None, :]
        shift1, scale1, gate1, shift2, scale2, gate2 = jnp.split(mod, 6, axis=-1)
        h = _rmsnorm(x, norm1_g[l]) * (1 + scale1) + shift1
        x = x + gate1 * _mixer(h, w_in[l], out_norm_g[l], w_out[l])
        h = _rmsnorm(x, norm2_g[l]) * (1 + scale2) + shift2
        x = x + gate2 * _peer(h, peer_wq[l], peer_subkeys[l], peer_u[l], peer_v[l])
    return _rmsnorm(x, final_g)
```

```python
import contextlib
import numpy as np
import concourse.bass as bass
import concourse.mybir as mybir
from concourse.bass_utils import run_bass_kernel_spmd

F32 = mybir.dt.float32
BF16 = mybir.dt.bfloat16
AF = mybir.ActivationFunctionType
ALU = mybir.AluOpType
AX = mybir.AxisListType

SEQ = 2048
D = 2048
NTT = SEQ // 128
DEPTH = 2
HD = 128
EPS = 1e-6
NEG = -1.0e30
IN_WIDTH = 5968
C_QA, C_KA, C_VA, C_IQ, C_IK, C_IW = 0, 768, 896, 1024, 2048, 2112
C_QB, C_KB, C_VB, C_QC, C_KC, C_VC = 2128, 2768, 3408, 4048, 4688, 5328
ATT_SCALE = HD ** -0.5
IDX_SCALE = (64 ** -0.5) * (16 ** -0.5)
DSA_TOPK = 256
NKEYS = 128
PEER_TG = 512
PEER_EG = 4
PEER_IG = 8


class Sched:
    NDMA = 40
    NHW = 24

    def __init__(self, nc, es):
        self.nc = nc
        self.engs = {"pe": nc.tensor, "act": nc.scalar, "dve": nc.vector,
                     "pool": nc.gpsimd, "sp": nc.sync}
        self.sem = {k: es.enter_context(nc.semaphore("sem_" + k)) for k in self.engs}
        self.cnt = {k: 0 for k in self.engs}
        self.dsem = [es.enter_context(nc.semaphore("dsem%d" % i)) for i in range(self.NDMA)]
        self.dcnt = [0] * self.NDMA
        self.dnext = 0
        self.dnext_sw = 0
        self.known = {k: {} for k in self.engs}
        self.res = {}
        self.ninstr = 0

    def _semobj(self, key):
        return self.sem[key] if isinstance(key, str) else self.dsem[key]

    def _wait(self, eng, key, val):
        if val <= 0:
            return
        kn = self.known[eng]
        if kn.get(key, 0) >= val:
            return
        self.engs[eng].wait_ge(self._semobj(key), val)
        kn[key] = val

    def _deps(self, r, w):
        deps = {}

        def add(k, v):
            if deps.get(k, 0) < v:
                deps[k] = v
        for key in r:
            st = self.res.get(key)
            if st is not None and st["w"] is not None:
                add(*st["w"])
        for key in w:
            st = self.res.get(key)
            if st is not None:
                if st["w"] is not None:
                    add(*st["w"])
                for k, v in st["r"].items():
                    add(k, v)
        return deps

    def _commit(self, r, w, stamp):
        k, v = stamp
        for key in r:
            st = self.res.setdefault(key, {"w": None, "r": {}})
            if st["r"].get(k, 0) < v:
                st["r"][k] = v
        for key in w:
            self.res[key] = {"w": stamp, "r": {}}

    def op(self, eng, fn, r=(), w=()):
        for k, v in self._deps(r, w).items():
            if eng == "pe" and k == "pe":
                continue
            self._wait(eng, k, v)
        ins = fn(self.engs[eng])
        self.cnt[eng] += 1
        ins.then_inc(self.sem[eng], 1)
        self._commit(r, w, (eng, self.cnt[eng]))
        self.ninstr += 1
        return ins

    def dma(self, eng, out, in_, r=(), w=(), **kw):
        for k, v in self._deps(r, w).items():
            self._wait(eng, k, v)
        if eng == "pool":
            i = self.NHW + self.dnext_sw
            self.dnext_sw = (self.dnext_sw + 1) % (self.NDMA - self.NHW)
        else:
            i = self.dnext
            self.dnext = (self.dnext + 1) % self.NHW
        self._wait(eng, i, self.dcnt[i])
        ins = self.engs[eng].dma_start(out=out, in_=in_, **kw)
        self.dcnt[i] += 16
        ins.then_inc(self.dsem[i], 16)
        self._commit(r, w, (i, self.dcnt[i]))
        self.ninstr += 1
        return ins

    def barrier(self):
        for eng in self.engs:
            for k in self.engs:
                if k != eng:
                    self._wait(eng, k, self.cnt[k])
            for i in range(self.NDMA):
                self._wait(eng, i, self.dcnt[i])

    def finish(self, eng="sp"):
        for k in self.engs:
            self._wait(eng, k, self.cnt[k])
        for i in range(self.NDMA):
            self._wait(eng, i, self.dcnt[i])


def build(nlayers=DEPTH, taps=(), stop=None, peer_dummy=False):
    nc = bass.Bass("TRN2", target_bir_lowering=False)

    def din(name, shape, dt=F32):
        return nc.dram_tensor(name, shape, dt, kind="ExternalInput").ap()

    def dscr(name, shape, dt=F32):
        kind = "ExternalOutput" if name in taps else "Internal"
        return nc.dram_tensor(name, shape, dt, kind=kind).ap()

    x_in = din("x", [SEQ, D])
    cT = din("cT", [128, 16])
    ada_w = din("ada_w", [DEPTH, D, 6 * D])
    ada_bT = din("ada_bT", [128, DEPTH * 96])
    gT = din("gT", [128, DEPTH * 32])
    w_in = din("w_in", [DEPTH, D, IN_WIDTH])
    og = din("og", [DEPTH, D])
    w_out = din("w_out", [DEPTH, D, D])
    wq = din("peer_wq", [DEPTH, D, D])
    skT = din("skT", [DEPTH, 16, 128, 128])
    uT = din("uT", [1, 128, 128] if peer_dummy else [nlayers, D, NKEYS * NKEYS])
    pv = din("pv", [1, 128, 128] if peer_dummy else [nlayers, NKEYS * NKEYS, D])
    fg = din("fg", [1, D])
    ident_d = din("ident", [128, 128])
    cs128 = din("cs128", [128, 2, SEQ])
    cs64 = din("cs64", [128, 2, SEQ])
    out_d = nc.dram_tensor("out", [SEQ, D], F32, kind="ExternalOutput").ap()

    xs = dscr("xs", [SEQ, D])
    hT_d = dscr("hT", [128, 16, SEQ], BF16)
    modrow = dscr("modrow", [DEPTH * 96, 128])
    qaT = dscr("qaT", [6, 128, SEQ], BF16)
    kaT = dscr("kaT", [128, SEQ], BF16)
    iqT = dscr("iqT", [8, 128, SEQ], BF16)
    ikT = dscr("ikT", [128, SEQ], BF16)
    qbT = dscr("qbT", [5, 128, SEQ], BF16)
    kbT = dscr("kbT", [5, 128, SEQ], BF16)
    qcT = dscr("qcT", [5, 128, SEQ], BF16)
    kcT = dscr("kcT", [5, 128, SEQ], BF16)
    vtok = dscr("vtok", [SEQ, 1408], BF16)
    iw_d = dscr("iw", [SEQ, 16])
    ynT = dscr("ynT", [16, 128, SEQ], BF16)
    ps_d = dscr("peer_s", [SEQ, 2048])
    s0r_d = dscr("peer_s0r", [SEQ, 128, 8])
    pst_d = dscr("peer_st", [SEQ, 16])

    es = contextlib.ExitStack()
    with es:
        S = Sched(nc, es)

        sbn = [0]

        def sb(stack, name, shape, dt=F32):
            sbn[0] += 1
            return stack.enter_context(nc.sbuf_tensor("s%d_%s" % (sbn[0], name), shape, dt))

        psA = es.enter_context(nc.psum_tensor("psA", [128, 2048], F32))
        psB = es.enter_context(nc.psum_tensor("psB", [128, 2048], F32))

        def bankA(i):
            return psA[:, i * 512:(i + 1) * 512]

        def bankB(i):
            return psB[:, i * 512:(i + 1) * 512]

        negreg = nc.gpsimd.to_reg(NEG)
        ident = sb(es, "ident", [128, 128])
        identb = sb(es, "identb", [128, 128], BF16)
        modT = sb(es, "modT", [128, DEPTH * 96])
        scs = sb(es, "scs", [128, DEPTH * 32])
        gsb = sb(es, "gsb", [128, DEPTH * 32])
        S.dma("sp", ident[:], ident_d[:, :], w=["ident"])
        S.op("dve", lambda e: e.tensor_copy(out=identb[:], in_=ident[:]), r=["ident"], w=["identb"])
        S.dma("sp", gsb[:], gT[:, :], w=["gsb"])

        with contextlib.ExitStack() as ph:
            cT_sb = sb(ph, "cT_sb", [128, 16])
            cond = sb(ph, "cond", [128, 16])
            abT = sb(ph, "abT", [128, DEPTH * 96])
            wbuf = [sb(ph, "adaw%d" % i, [128, 16, 512]) for i in range(2)]
            S.dma("sp", cT_sb[:], cT[:, :], w=["cT"])
            S.dma("sp", abT[:], ada_bT[:, :], w=["abT"])
            S.op("act", lambda e: e.activation(out=cond[:], in_=cT_sb[:], func=AF.Silu), r=["cT"], w=["cond"])
            gi = 0
            for l in range(nlayers):
                awl = ada_w[l].rearrange("(kc p) n -> p kc n", p=128)
                for g in range(24):
                    wb = wbuf[gi % 2]
                    key = "adaw%d" % (gi % 2)
                    S.dma("sp" if gi % 2 == 0 else "act", wb[:], awl[:, :, g * 512:(g + 1) * 512], w=[key])
                    for j in range(4):
                        col = l * 96 + g * 4 + j
                        for kc in range(16):
                            S.op("pe", lambda e, wb=wb, j=j, kc=kc, col=col: e.matmul(
                                psA[:, col:col + 1], lhsT=wb[:, kc, j * 128:(j + 1) * 128],
                                rhs=cond[:, kc:kc + 1], start=(kc == 0), stop=(kc == 15)),
                                r=[key, "cond"], w=["A0"])
                    gi += 1
            ncol = nlayers * 96
            S.op("dve", lambda e: e.tensor_tensor(out=modT[:, 0:ncol], in0=psA[:, 0:ncol], in1=abT[:, 0:ncol], op=ALU.add),
                 r=["A0", "abT"], w=["modT"])
            for l in range(nlayers):
                for which in range(2):
                    src = modT[:, l * 96 + 16 + which * 48: l * 96 + 32 + which * 48]
                    dst = scs[:, l * 32 + which * 16: l * 32 + which * 16 + 16]
                    gsl = gsb[:, l * 32 + which * 16: l * 32 + which * 16 + 16]
                    S.op("dve", lambda e, src=src, dst=dst: e.tensor_scalar(out=dst, in0=src, scalar1=1.0, scalar2=None, op0=ALU.add),
                         r=["modT"], w=["scs"])
                    S.op("dve", lambda e, dst=dst, gsl=gsl: e.tensor_tensor(out=dst, in0=dst, in1=gsl, op=ALU.mult),
                         r=["scs", "gsb"], w=["scs"])
            mr = sb(ph, "mr", [128, 2, 128])
            S.op("pe", lambda e: e.transpose(out=psA[:, 512:640], in_=modT[:, 0:128], identity=ident[:]), r=["modT", "ident"], w=["A1"])
            S.op("dve", lambda e: e.tensor_copy(out=mr[:, 0, :], in_=psA[:, 512:640]), r=["A1"], w=["mr"])
            if ncol > 128:
                S.op("pe", lambda e: e.transpose(out=psA[0:64, 1024:1152], in_=modT[:, 128:192], identity=ident[:]), r=["modT", "ident"], w=["A2"])
                S.op("dve", lambda e: e.tensor_copy(out=mr[0:64, 1, :], in_=psA[0:64, 1024:1152]), r=["A2"], w=["mr"])
            S.dma("sp", modrow[0:min(ncol, 128), :], mr[0:min(ncol, 128), 0, :], r=["mr"], w=["D:modrow"])
            if ncol > 128:
                S.dma("sp", modrow[128:192, :], mr[0:64, 1, :], r=["mr"], w=["D:modrow"])

        def gate_row(l, which):
            r0 = l * 96 + 32 + which * 48
            return modrow[r0:r0 + 16, :].rearrange("(o a) b -> o (a b)", o=1)

        def norm_phase(tag, xsrc, l, which):
            sc = scs[:, l * 32 + which * 16: l * 32 + which * 16 + 16]
            shb = l * 96 + which * 48
            sh = modT[:, shb:shb + 16]
            S.barrier()
            with contextlib.ExitStack() as ph:
                xt = [sb(ph, "%sx%d" % (tag, i), [128, 4, D]) for i in range(2)]
                junk = sb(ph, tag + "junk", [128, D])
                ss = sb(ph, tag + "ss", [128, 4])
                rstd = sb(ph, tag + "rstd", [128, 4])
                hst = [sb(ph, "%shst%d" % (tag, i), [128, 16, 512], BF16) for i in range(2)]
                for tg in range(4):
                    xb = xt[tg % 2]
                    xk = "%sx%d" % (tag, tg % 2)
                    hk = "%shst%d" % (tag, tg % 2)
                    S.dma("sp", xb[:], xsrc[tg * 512:(tg + 1) * 512, :].rearrange("(i p) d -> p i d", p=128),
                          r=["D:xs"], w=[xk])
                    for i in range(4):
                        S.op("act", lambda e, xb=xb, i=i: e.activation(out=junk[:], in_=xb[:, i, :], func=AF.Square,
                                                                       accum_out=ss[:, i:i + 1]),
                             r=[xk], w=[tag + "junk", tag + "ss"])
                    S.op("dve", lambda e: e.tensor_scalar(out=rstd[:], in0=ss[:], scalar1=1.0 / D, scalar2=EPS,
                                                          op0=ALU.mult, op1=ALU.add), r=[tag + "ss"], w=[tag + "rstd"])
                    S.op("act", lambda e: e.activation(out=rstd[:], in_=rstd[:], func=AF.Sqrt), r=[tag + "rstd"], w=[tag + "rstd"])
                    S.op("dve", lambda e: e.reciprocal(out=rstd[:], in_=rstd[:]), r=[tag + "rstd"], w=[tag + "rstd"])
                    for i in range(4):
                        S.op("pool" if i % 2 else "dve", lambda e, xb=xb, i=i: e.tensor_scalar(
                            out=xb[:, i, :], in0=xb[:, i, :], scalar1=rstd[:, i:i + 1], scalar2=None, op0=ALU.mult),
                            r=[xk, tag + "rstd"], w=[xk])
                    hs = hst[tg % 2]
                    for dc in range(16):
                        bk = dc % 4
                        for i in range(4):
                            S.op("pe", lambda e, xb=xb, i=i, dc=dc, bk=bk: e.transpose(
                                out=psA[:, bk * 512 + i * 128: bk * 512 + (i + 1) * 128],
                                in_=xb[:, i, dc * 128:(dc + 1) * 128], identity=ident[:]),
                                r=[xk, "ident"], w=["A%d" % bk])
                        S.op("act", lambda e, hs=hs, dc=dc, bk=bk: e.activation(
                            out=hs[:, dc, :], in_=bankA(bk), func=AF.Identity,
                            scale=sc[:, dc:dc + 1], bias=sh[:, dc:dc + 1]),
                            r=["A%d" % bk, "scs", "modT"], w=[hk])
                    S.dma("sp", hT_d[:, :, tg * 512:(tg + 1) * 512], hs[:], r=[hk], w=["D:hT"])

        def proj_phase(l):
            wl = w_in[l].rearrange("(dc p) n -> p dc n", p=128)
            S.barrier()
            with contextlib.ExitStack() as ph:
                hT = sb(ph, "hT", [128, 16, SEQ], BF16)
                for q in range(4):
                    S.dma("sp" if q % 2 == 0 else "act", hT[:, q * 4:(q + 1) * 4, :], hT_d[:, q * 4:(q + 1) * 4, :],
                          r=["D:hT"], w=["hT%d" % q])
                hkeys = ["hT%d" % q for q in range(4)]
                c128 = sb(ph, "c128", [128, 2, SEQ])
                c64 = sb(ph, "c64", [128, 2, SEQ])
                S.dma("sp", c128[:], cs128[:, :, :], w=["c128"])
                S.dma("act", c64[:], cs64[:, :, :], w=["c64"])
                wts = [sb(ph, "wt%d" % i, [128, 16, 128], BF16) for i in range(2)]
                wss = [sb(ph, "ws%d" % i, [128, 16, 128], BF16) for i in range(2)]
                stg = [sb(ph, "stg%d" % i, [128, SEQ], BF16) for i in range(2)]
                t1 = sb(ph, "rt1", [128, 512])
                t2 = sb(ph, "rt2", [128, 512])

                chunks = []
                for h in range(6):
                    chunks.append((qaT[h], [(0, C_QA + h * 128, 128)], 128))
                chunks.append((kaT, [(0, C_KA, 128)], 128))
                for c in range(8):
                    chunks.append((iqT[c], [(0, C_IQ + c * 128, 128)], 64))
                chunks.append((ikT, [(0, C_IK, 64), (64, C_IK, 64)], 64))
                for h in range(5):
                    chunks.append((qbT[h], [(0, C_QB + h * 128, 128)], None))
                    chunks.append((kbT[h], [(0, C_KB + h * 128, 128)], None))
                for h in range(5):
                    chunks.append((qcT[h], [(0, C_QC + h * 128, 128)], 128))
                    chunks.append((kcT[h], [(0, C_KC + h * 128, 128)], 128))

                for ci, (dst, pieces, rope) in enumerate(chunks):
                    wt = wts[ci % 2]
                    ws = wss[ci % 2]
                    wk = "wt%d" % (ci % 2)
                    wsk = "ws%d" % (ci % 2)
                    st = stg[ci % 2]
                    sk = "stg%d" % (ci % 2)
                    for (dc0, sc0, wd) in pieces:
                        S.dma("pool", wt[:, :, dc0:dc0 + wd], wl[:, :, sc0:sc0 + wd], w=[wk])
                    if rope is not None:
                        half = rope // 2
                        for (dc0, sc0, wd) in pieces:
                            for b0 in range(0, wd, rope):
                                S.dma("pool", ws[:, :, dc0 + b0:dc0 + b0 + half],
                                      wl[:, :, sc0 + b0 + half:sc0 + b0 + rope], w=[wsk])
                                S.dma("pool", ws[:, :, dc0 + b0 + half:dc0 + b0 + rope],
                                      wl[:, :, sc0 + b0:sc0 + b0 + half], w=[wsk])
                    tab = c128 if rope == 128 else c64
                    tabk = "c128" if rope == 128 else "c64"
                    for tq in range(4):
                        ba = tq % 2
                        for dc in range(16):
                            S.op("pe", lambda e, wt=wt, dc=dc, tq=tq, ba=ba: e.matmul(
                                bankA(ba), lhsT=wt[:, dc, :], rhs=hT[:, dc, tq * 512:(tq + 1) * 512],
                                start=(dc == 0), stop=(dc == 15)), r=[wk, hkeys[dc // 4]], w=["A%d" % ba])
                        if rope is None:
                            S.op("act", lambda e, st=st, tq=tq, ba=ba: e.copy(out=st[:, tq * 512:(tq + 1) * 512], in_=bankA(ba)),
                                 r=["A%d" % ba], w=[sk])
                        else:
                            for dc in range(16):
                                S.op("pe", lambda e, ws=ws, dc=dc, tq=tq, ba=ba: e.matmul(
                                    bankB(ba), lhsT=ws[:, dc, :], rhs=hT[:, dc, tq * 512:(tq + 1) * 512],
                                    start=(dc == 0), stop=(dc == 15)), r=[wsk, hkeys[dc // 4]], w=["B%d" % ba])
                            S.op("dve", lambda e, tq=tq, ba=ba, tab=tab: e.tensor_tensor(
                                out=t1[:], in0=bankA(ba), in1=tab[:, 0, tq * 512:(tq + 1) * 512], op=ALU.mult),
                                r=["A%d" % ba, tabk], w=["rt1"])
                            S.op("dve", lambda e, tq=tq, ba=ba, tab=tab: e.tensor_tensor(
                                out=t2[:], in0=bankB(ba), in1=tab[:, 1, tq * 512:(tq + 1) * 512], op=ALU.mult),
                                r=["B%d" % ba, tabk], w=["rt2"])
                            S.op("pool", lambda e, st=st, tq=tq: e.tensor_tensor(
                                out=st[:, tq * 512:(tq + 1) * 512], in0=t1[:], in1=t2[:], op=ALU.add),
                                r=["rt1", "rt2"], w=[sk])
                    S.dma("sp", dst, st[:], r=[sk], w=["D:qk"])

                wv = sb(ph, "wv", [128, 16, 1424], BF16)
                for (dc0, sc0, wd) in [(0, C_VA, 128), (128, C_VB, 640), (768, C_VC, 640), (1408, C_IW, 16)]:
                    S.dma("pool", wv[:, :, dc0:dc0 + wd], wl[:, :, sc0:sc0 + wd], w=["wv"])
                vst = [sb(ph, "vst%d" % i, [128, 1408], BF16) for i in range(2)]
                iwst = sb(ph, "iwst", [128, NTT, 16])
                for tt in range(NTT):
                    vs = vst[tt % 2]
                    vk = "vst%d" % (tt % 2)
                    for nb, (n0, n1) in enumerate([(0, 512), (512, 1024), (1024, 1424)]):
                        for dc in range(16):
                            S.op("pe", lambda e, tt=tt, dc=dc, nb=nb, n0=n0, n1=n1: e.matmul(
                                psB[:, nb * 512: nb * 512 + (n1 - n0)], lhsT=hT[:, dc, tt * 128:(tt + 1) * 128],
                                rhs=wv[:, dc, n0:n1], start=(dc == 0), stop=(dc == 15)),
                                r=["wv", hkeys[dc // 4]], w=["B%d" % nb])
                    S.op("act", lambda e, vs=vs: e.copy(out=vs[:, 0:1024], in_=psB[:, 0:1024]), r=["B0", "B1"], w=[vk])
                    S.op("dve", lambda e, vs=vs: e.tensor_copy(out=vs[:, 1024:1408], in_=psB[:, 1024:1408]), r=["B2"], w=[vk])
                    S.op("dve", lambda e, tt=tt: e.tensor_scalar(out=iwst[:, tt, :], in0=psB[:, 1408:1424], scalar1=IDX_SCALE,
                                                                 scalar2=None, op0=ALU.mult), r=["B2"], w=["iwst"])
                    S.dma("sp", vtok[tt * 128:(tt + 1) * 128, :], vs[:], r=[vk], w=["D:vtok"])
                S.dma("sp", iw_d.rearrange("(tt p) h -> p tt h", p=128), iwst[:], r=["iwst"], w=["D:iw"])

        def attn_phase(l):
            S.barrier()
            with contextlib.ExitStack() as ph:
                ogb = sb(ph, "ogb", [128, D])
                S.dma("sp", ogb[:], og[l:l + 1, :].partition_broadcast(128), w=["ogb"])
                qT = [sb(ph, "qT%d" % i, [128, SEQ], BF16) for i in range(2)]
                kT = [sb(ph, "kT%d" % i, [128, SEQ], BF16) for i in range(2)]
                vv = [sb(ph, "vv%d" % i, [128, NTT, 128], BF16) for i in range(2)]
                ynst = [sb(ph, "ynst%d" % i, [128, SEQ], BF16) for i in range(2)]
                f1 = sb(ph, "f1", [128, SEQ + 1])
                f2 = sb(ph, "f2", [128, SEQ + 1])
                f3 = sb(ph, "f3", [128, SEQ + 1])
                pb = sb(ph, "pb", [128, SEQ], BF16)
                aT = sb(ph, "aT", [128, NTT, 128], BF16)
                sm = sb(ph, "sm", [128, 64])
                ysb = sb(ph, "ysb", [128, 128])
                ynb = sb(ph, "ynb", [128, 128], BF16)
                jk = sb(ph, "jk", [128, 128])
                m8 = sb(ph, "m8", [128, 8])
                pen16 = sb(ph, "pen16", [128, 16])
                gs8 = sb(ph, "gs8", [128, 8])
                S.op("dve", lambda e: e.memset(f2[:, 0:1], 0.0), w=["f2"])
                psBb = psB[:].bitcast(BF16)

                def load_head(slot, qsrc, ksrc, vcol, ksame=False):
                    S.dma("sp", qT[slot][:], qsrc, r=["D:qk"], w=["qT%d" % slot])
                    if not ksame:
                        S.dma("act", kT[slot][:], ksrc, r=["D:qk"], w=["kT%d" % slot])
                        S.dma("sp", vv[slot][:], vtok[:, vcol:vcol + 128].rearrange("(st p) n -> p st n", p=128),
                              r=["D:vtok"], w=["vv%d" % slot])

                def scores(slot, kslot, qi, W):
                    nch = (W + 511) // 512
                    for c in range(nch):
                        wc = min(512, W - c * 512)
                        S.op("pe", lambda e, c=c, wc=wc: e.matmul(
                            psA[:, c * 512:c * 512 + wc], lhsT=qT[slot][:, qi * 128:(qi + 1) * 128],
                            rhs=kT[kslot][:, c * 512:c * 512 + wc], start=True, stop=True),
                            r=["qT%d" % slot, "kT%d" % kslot], w=["A%d" % c])
                    return ["A%d" % c for c in range(nch)]

                def pv_and_finish(kslot, qi, W, head, dst, dkey, rinv):
                    nk = W // 128
                    for sc in range(nk):
                        bk = 0 if sc < 8 else 1
                        off = bk * 1024 + (sc % 8) * 128
                        S.op("pe", lambda e, sc=sc, off=off: e.transpose(
                            out=psBb[:, off:off + 128], in_=pb[:, sc * 128:(sc + 1) * 128], identity=identb[:]),
                            r=["pb", "identb"], w=["B%d" % bk])
                    n0 = min(nk, 8)
                    S.op("act", lambda e: e.copy(out=aT[:, 0:n0, :].rearrange("p a b -> p (a b)"), in_=psBb[:, 0:n0 * 128]),
                         r=["B0"], w=["aT"])
                    if nk > 8:
                        S.op("dve", lambda e: e.tensor_copy(out=aT[:, 8:nk, :].rearrange("p a b -> p (a b)"),
                                                            in_=psBb[:, 1024:1024 + (nk - 8) * 128]), r=["B1"], w=["aT"])
                    for sc in range(nk):
                        S.op("pe", lambda e, sc=sc: e.matmul(psB[:, 1024:1152], lhsT=aT[:, sc, :], rhs=vv[kslot][:, sc, :],
                                                             start=(sc == 0), stop=(sc == nk - 1)),
                             r=["aT", "vv%d" % kslot], w=["B2"])
                    if rinv is None:
                        S.op("act", lambda e: e.copy(out=ysb[:], in_=psB[:, 1024:1152]), r=["B2"], w=["ysb"])
                    else:
                        S.op("dve", lambda e: e.tensor_scalar(out=ysb[:], in0=psB[:, 1024:1152], scalar1=rinv, scalar2=None,
                                                              op0=ALU.mult), r=["B2", "sm"], w=["ysb"])
                    S.op("act", lambda e: e.activation(out=jk[:], in_=ysb[:], func=AF.Square, accum_out=sm[:, 8:9]),
                         r=["ysb"], w=["jk", "sm"])
                    S.op("dve", lambda e: e.tensor_scalar(out=sm[:, 9:10], in0=sm[:, 8:9], scalar1=1.0 / HD, scalar2=EPS,
                                                          op0=ALU.mult, op1=ALU.add), r=["sm"], w=["sm"])
                    S.op("act", lambda e: e.activation(out=sm[:, 9:10], in_=sm[:, 9:10], func=AF.Ln), r=["sm"], w=["sm"])
                    S.op("act", lambda e: e.activation(out=sm[:, 10:11], in_=sm[:, 9:10], func=AF.Exp, scale=-0.5), r=["sm"], w=["sm"])
                    S.op("dve", lambda e: e.scalar_tensor_tensor(out=ynb[:], in0=ysb[:], scalar=sm[:, 10:11], in1=ogb[:, head * 128:(head + 1) * 128],
                                                                 op0=ALU.mult, op1=ALU.mult), r=["ysb", "sm", "ogb"], w=["ynb"])
                    S.op("pe", lambda e: e.transpose(out=psBb[:, 3072:3200], in_=ynb[:], identity=identb[:]),
                         r=["ynb", "identb"], w=["B3"])
                    S.op("act", lambda e: e.copy(out=dst[:, qi * 128:(qi + 1) * 128], in_=psBb[:, 3072:3200]),
                         r=["B3"], w=[dkey])

                def softmax_rows(W, zkeys, pen, penkeys):
                    S.op("dve", lambda e: e.scalar_tensor_tensor(out=f1[:, 0:W], in0=psA[:, 0:W], scalar=ATT_SCALE, in1=pen,
                                                                 op0=ALU.mult, op1=ALU.add), r=zkeys + penkeys, w=["f1"])

                def softmax_tail(W):
                    S.op("dve", lambda e: e.reduce_max(out=sm[:, 0:1], in_=f1[:, 0:W], axis=AX.X), r=["f1"], w=["sm"])
                    S.op("dve", lambda e: e.tensor_scalar(out=sm[:, 2:3], in0=sm[:, 0:1], scalar1=-1.0, scalar2=None, op0=ALU.mult),
                         r=["sm"], w=["sm"])
                    S.op("act", lambda e: e.activation(out=pb[:, 0:W], in_=f1[:, 0:W], func=AF.Exp, bias=sm[:, 2:3],
                                                       accum_out=sm[:, 1:2]), r=["f1", "sm"], w=["pb", "sm"])
                    S.op("dve", lambda e: e.reciprocal(out=sm[:, 3:4], in_=sm[:, 1:2]), r=["sm"], w=["sm"])

                hcount = [0]

                def next_slot():
                    s = hcount[0] % 2
                    hcount[0] += 1
                    return s

                for h in range(5):
                    slot = next_slot()
                    load_head(slot, qbT[h], kbT[h], 128 + h * 128)
                    for qi in range(NTT):
                        W = (qi + 1) * 128
                        zk = scores(slot, slot, qi, W)
                        S.op("act", lambda e, W=W: e.activation(out=f1[:, 0:W], in_=psA[:, 0:W], func=AF.Exp, scale=ATT_SCALE),
                             r=zk, w=["f1"])
                        S.op("act", lambda e, W=W: e.activation(out=f1[:, 0:W], in_=f1[:, 0:W], func=AF.Ln, bias=1.0),
                             r=["f1"], w=["f1"])
                        S.op("pool", lambda e, W=W: e.affine_select(out=f1[:, W - 128:W], in_=f1[:, W - 128:W], pattern=[[-1, 128]],
                                                                    base=0, channel_multiplier=1, compare_op=ALU.is_gt, fill=0.0),
                             r=["f1"], w=["f1"])
                        S.op("dve", lambda e, W=W: e.tensor_tensor_scan(out=f2[:, 1:W + 1], data0=f1[:, 0:W], data1=f1[:, 0:W],
                                                                        initial=0.0, op0=ALU.add, op1=ALU.bypass),
                             r=["f1"], w=["f2"])
                        S.op("dve", lambda e, W=W: e.tensor_scalar(out=sm[:, 4:5], in0=f2[:, W:W + 1], scalar1=-1.0, scalar2=None,
                                                                   op0=ALU.mult), r=["f2"], w=["sm"])
                        S.op("dve", lambda e, W=W: e.scalar_tensor_tensor(out=f3[:, 0:W], in0=psA[:, 0:W], scalar=ATT_SCALE,
                                                                          in1=f2[:, 0:W], op0=ALU.mult, op1=ALU.add),
                             r=zk + ["f2"], w=["f3"])
                        S.op("act", lambda e, W=W: e.activation(out=pb[:, 0:W], in_=f3[:, 0:W], func=AF.Exp, bias=sm[:, 4:5]),
                             r=["f3", "sm"], w=["pb"])
                        S.op("pool", lambda e, W=W: e.affine_select(out=pb[:, W - 128:W], in_=pb[:, W - 128:W], pattern=[[-1, 128]],
                                                                    base=0, channel_multiplier=1, compare_op=ALU.is_gt, fill=0.0),
                             r=["pb"], w=["pb"])
                        pv_and_finish(slot, qi, W, 6 + h, ynst[slot], 'ynst%d' % slot, None)
                    S.dma("sp", ynT[6 + h], ynst[slot][:], r=["ynst%d" % slot], w=["D:ynT"])
                if stop == "attnB":
                    return

                for h in range(5):
                    slot = next_slot()
                    load_head(slot, qcT[h], kcT[h], 768 + h * 128)
                    S.op("dve", lambda e, slot=slot: e.tensor_reduce(out=gs8[:], in_=kT[slot][:].rearrange("p (n s) -> p n s", s=256),
                                                                     op=ALU.add, axis=AX.X), r=["kT%d" % slot], w=["gs8"])
                    kmb = sb(ph, "kmb%d" % h, [128, 8], BF16)
                    S.op("dve", lambda e, kmb=kmb: e.tensor_scalar(out=kmb[:], in0=gs8[:], scalar1=1.0 / 256, scalar2=None, op0=ALU.mult),
                         r=["gs8"], w=["kmb"])
                    for qi in range(NTT):
                        W = (qi + 1) * 128
                        own = qi // 2
                        zk = scores(slot, slot, qi, W)
                        if own > 3:
                            S.op("pe", lambda e, kmb=kmb, qi=qi: e.matmul(psB[:, 1536:1544], lhsT=qT[slot][:, qi * 128:(qi + 1) * 128],
                                                                          rhs=kmb[:], start=True, stop=True),
                                 r=["qT%d" % slot, "kmb"], w=["B3"])
                            S.op("dve", lambda e: e.memset(gs8[:], NEG), w=["gs8"])
                            S.op("dve", lambda e, own=own: e.tensor_copy(out=gs8[:, 0:own], in_=psB[:, 1536:1536 + own]),
                                 r=["B3"], w=["gs8"])
                            S.op("dve", lambda e: e.max(out=m8[:], in_=gs8[:]), r=["gs8"], w=["m8"])
                            S.op("dve", lambda e: e.memset(pen16[:], 0.0), w=["pen16"])
                            S.op("dve", lambda e, own=own: e.tensor_scalar(
                                out=pen16[:, 0:2 * own].rearrange("p (n two) -> p n two", two=2),
                                in0=gs8[:, 0:own].unsqueeze(2).to_broadcast([128, own, 2]),
                                scalar1=m8[:, 2:3], scalar2=NEG, op0=ALU.is_lt, op1=ALU.mult),
                                r=["gs8", "m8"], w=["pen16"])
                        else:
                            S.op("dve", lambda e: e.memset(pen16[:], 0.0), w=["pen16"])
                        nk = W // 128
                        S.op("dve", lambda e, W=W, nk=nk: e.scalar_tensor_tensor(
                            out=f1[:, 0:W].rearrange("p (n s) -> p n s", s=128),
                            in0=psA[:, 0:W].rearrange("p (n s) -> p n s", s=128), scalar=ATT_SCALE,
                            in1=pen16[:, 0:nk].unsqueeze(2).to_broadcast([128, nk, 128]),
                            op0=ALU.mult, op1=ALU.add), r=zk + ["pen16"], w=["f1"])
                        S.op("pool", lambda e, W=W: e.affine_select(out=f1[:, W - 128:W], in_=f1[:, W - 128:W], pattern=[[-1, 128]],
                                                                    base=0, channel_multiplier=1, compare_op=ALU.is_ge, fill=negreg),
                             r=["f1"], w=["f1"])
                        softmax_tail(W)
                        pv_and_finish(slot, qi, W, 11 + h, ynst[slot], 'ynst%d' % slot, sm[:, 3:4])
                    S.dma("sp", ynT[11 + h], ynst[slot][:], r=["ynst%d" % slot], w=["D:ynT"])
                if stop == "attnC":
                    return

                iq = sb(ph, "iq", [128, 8, SEQ], BF16)
                ik2 = sb(ph, "ik2", [128, SEQ], BF16)
                iws = sb(ph, "iws", [128, NTT, 16])
                qa = sb(ph, "qa", [128, 6, SEQ], BF16)
                pen = sb(ph, "pen", [128, SEQ])
                ynsta = sb(ph, "ynsta", [128, 6, SEQ], BF16)
                for c in range(8):
                    S.dma("sp" if c % 2 else "act", iq[:, c, :], iqT[c], r=["D:qk"], w=["iq"])
                for hh in range(6):
                    S.dma("sp" if hh % 2 else "act", qa[:, hh, :], qaT[hh], r=["D:qk"], w=["qa"])
                S.dma("sp", ik2[:], ikT, r=["D:qk"], w=["ik2"])
                S.dma("sp", iws[:], iw_d.rearrange("(tt p) h -> p tt h", p=128), r=["D:iw"], w=["iws"])
                S.dma("act", kT[0][:], kaT, r=["D:qk"], w=["kT0"])
                S.dma("sp", vv[0][:], vtok[:, 0:128].rearrange("(st p) n -> p st n", p=128), r=["D:vtok"], w=["vv0"])
                for qi in range(NTT):
                    W = (qi + 1) * 128
                    nch = (W + 511) // 512
                    for hh in range(16):
                        c, half = hh // 2, hh % 2
                        r0 = 64 * half
                        for ch in range(nch):
                            wc = min(512, W - ch * 512)
                            S.op("pe", lambda e, c=c, r0=r0, ch=ch, wc=wc, qi=qi: e.matmul(
                                psB[:, ch * 512:ch * 512 + wc], lhsT=iq[r0:r0 + 64, c, qi * 128:(qi + 1) * 128],
                                rhs=ik2[r0:r0 + 64, ch * 512:ch * 512 + wc], start=True, stop=True),
                                r=["iq", "ik2"], w=["B%d" % ch])
                        bks = ["B%d" % ch for ch in range(nch)]
                        S.op("act", lambda e, W=W: e.activation(out=f3[:, 0:W], in_=psB[:, 0:W], func=AF.Relu), r=bks, w=["f3"])
                        if hh == 0:
                            S.op("dve", lambda e, W=W, qi=qi: e.tensor_scalar(out=f2[:, 0:W], in0=f3[:, 0:W], scalar1=iws[:, qi, 0:1],
                                                                              scalar2=None, op0=ALU.mult), r=["f3", "iws"], w=["f2"])
                        else:
                            S.op("dve", lambda e, W=W, qi=qi, hh=hh: e.scalar_tensor_tensor(
                                out=f2[:, 0:W], in0=f3[:, 0:W], scalar=iws[:, qi, hh:hh + 1], in1=f2[:, 0:W],
                                op0=ALU.mult, op1=ALU.add), r=["f3", "iws", "f2"], w=["f2"])
                    S.op("pool", lambda e, W=W: e.affine_select(out=f2[:, W - 128:W], in_=f2[:, W - 128:W], pattern=[[-1, 128]],
                                                                base=0, channel_multiplier=1, compare_op=ALU.is_ge, fill=negreg),
                         r=["f2"], w=["f2"])
                    if W > DSA_TOPK:
                        cur = f2
                        curk = "f2"
                        for rnd in range(DSA_TOPK // 8):
                            S.op("dve", lambda e, cur=cur, W=W: e.max(out=m8[:], in_=cur[:, 0:W]), r=[curk], w=["m8"])
                            if rnd < DSA_TOPK // 8 - 1:
                                S.op("dve", lambda e, cur=cur, W=W: e.match_replace(out=f3[:, 0:W], in_to_replace=m8[:],
                                                                                    in_values=cur[:, 0:W], imm_value=NEG),
                                     r=[curk, "m8"], w=["f3"])
                                cur = f3
                                curk = "f3"
                        S.op("dve", lambda e, W=W: e.tensor_scalar(out=pen[:, 0:W], in0=f2[:, 0:W], scalar1=m8[:, 7:8], scalar2=NEG,
                                                                   op0=ALU.is_lt, op1=ALU.mult), r=["f2", "m8"], w=["pen"])
                    else:
                        S.op("dve", lambda e, W=W: e.tensor_scalar(out=pen[:, 0:W], in0=f2[:, 0:W], scalar1=-1.0e29, scalar2=NEG,
                                                                   op0=ALU.is_lt, op1=ALU.mult), r=["f2"], w=["pen"])
                    for hh in range(6):
                        for ch in range(nch):
                            wc = min(512, W - ch * 512)
                            S.op("pe", lambda e, hh=hh, ch=ch, wc=wc, qi=qi: e.matmul(
                                psA[:, ch * 512:ch * 512 + wc], lhsT=qa[:, hh, qi * 128:(qi + 1) * 128],
                                rhs=kT[0][:, ch * 512:ch * 512 + wc], start=True, stop=True),
                                r=["qa", "kT0"], w=["A%d" % ch])
                        zk = ["A%d" % ch for ch in range(nch)]
                        softmax_rows(W, zk, pen[:, 0:W], ["pen"])
                        softmax_tail(W)
                        pv_and_finish(0, qi, W, hh, ynsta[:, hh, :], "ynsta", sm[:, 3:4])
                for hh in range(6):
                    S.dma("sp", ynT[hh], ynsta[:, hh, :], r=["ynsta"], w=["D:ynT"])

        def outproj_phase(l):
            S.barrier()
            with contextlib.ExitStack() as ph:
                yT = sb(ph, "yT", [128, 16, SEQ], BF16)
                wo = sb(ph, "wo", [128, 16, D], BF16)
                g1b = sb(ph, "g1b", [128, D])
                xt = [sb(ph, "ox%d" % i, [128, D]) for i in range(2)]
                tmp = sb(ph, "otmp", [128, D])
                wol = w_out[l].rearrange("(hc p) d -> p hc d", p=128)
                for q in range(4):
                    S.dma("sp" if q % 2 else "act", yT[:, q * 4:(q + 1) * 4, :], ynT[q * 4:(q + 1) * 4].rearrange("h p t -> p h t"),
                          r=["D:ynT"], w=["yT%d" % q])
                    S.dma("pool", wo[:, q * 4:(q + 1) * 4, :], wol[:, q * 4:(q + 1) * 4, :], w=["wo%d" % q])
                S.dma("sp", g1b[:], gate_row(l, 0).partition_broadcast(128), r=["D:modrow"], w=["g1b"])
                xsrc = x_in if l == 0 else xs
                for tt in range(NTT):
                    xb = xt[tt % 2]
                    xk = "ox%d" % (tt % 2)
                    S.dma("act", xb[:], xsrc[tt * 128:(tt + 1) * 128, :], r=["D:xs"], w=[xk])
                    for dq in range(4):
                        for hc in range(16):
                            S.op("pe", lambda e, tt=tt, dq=dq, hc=hc: e.matmul(
                                bankA(dq), lhsT=yT[:, hc, tt * 128:(tt + 1) * 128], rhs=wo[:, hc, dq * 512:(dq + 1) * 512],
                                start=(hc == 0), stop=(hc == 15)), r=["yT%d" % (hc // 4), "wo%d" % (hc // 4)], w=["A%d" % dq])
                    S.op("dve", lambda e: e.tensor_tensor(out=tmp[:], in0=psA[:, 0:D], in1=g1b[:], op=ALU.mult),
                         r=["A0", "A1", "A2", "A3", "g1b"], w=["otmp"])
                    S.op("pool", lambda e, xb=xb: e.tensor_tensor(out=xb[:], in0=xb[:], in1=tmp[:], op=ALU.add),
                         r=[xk, "otmp"], w=[xk])
                    S.dma("sp", xs[tt * 128:(tt + 1) * 128, :], xb[:], r=[xk], w=["D:xs"])

        def peerA_phase(l):
            S.barrier()
            with contextlib.ExitStack() as ph:
                hT = sb(ph, "phT", [128, 16, SEQ], BF16)
                wqb = sb(ph, "wqb", [128, 16, D], BF16)
                skb = sb(ph, "skb", [128, 16, 128], BF16)
                qTg = [sb(ph, "qTg%d" % i, [128, SEQ], BF16) for i in range(2)]
                sst = [sb(ph, "sst%d" % i, [128, NTT, 128]) for i in range(2)]
                wql = wq[l].rearrange("(dc p) n -> p dc n", p=128)
                for q in range(4):
                    S.dma("sp" if q % 2 else "act", hT[:, q * 4:(q + 1) * 4, :], hT_d[:, q * 4:(q + 1) * 4, :], r=["D:hT"], w=["phT%d" % q])
                    S.dma("pool", wqb[:, q * 4:(q + 1) * 4, :], wql[:, q * 4:(q + 1) * 4, :], w=["wqb%d" % q])
                S.dma("pool", skb[:], skT[l].rearrange("g e n -> e g n"), w=["skb"])
                ps_v = ps_d.rearrange("(tt p) (g n) -> p tt g n", p=128, n=128)
                for g in range(16):
                    qg = qTg[g % 2]
                    qk = "qTg%d" % (g % 2)
                    ss_ = sst[g % 2]
                    ssk = "sst%d" % (g % 2)
                    for tq in range(4):
                        ba = tq % 2
                        for dc in range(16):
                            S.op("pe", lambda e, g=g, tq=tq, dc=dc, ba=ba: e.matmul(
                                bankA(ba), lhsT=wqb[:, dc, g * 128:(g + 1) * 128], rhs=hT[:, dc, tq * 512:(tq + 1) * 512],
                                start=(dc == 0), stop=(dc == 15)), r=["wqb%d" % (dc // 4), "phT%d" % (dc // 4)], w=["A%d" % ba])
                        S.op("act", lambda e, qg=qg, tq=tq, ba=ba: e.copy(out=qg[:, tq * 512:(tq + 1) * 512], in_=bankA(ba)),
                             r=["A%d" % ba], w=[qk])
                    for tt in range(NTT):
                        S.op("pe", lambda e, qg=qg, g=g, tt=tt: e.matmul(
                            psB[:, tt * 128:(tt + 1) * 128], lhsT=qg[:, tt * 128:(tt + 1) * 128], rhs=skb[:, g, :],
                            start=True, stop=True), r=[qk, "skb"], w=["B%d" % (tt // 4)])
                    S.op("dve", lambda e, ss_=ss_: e.tensor_copy(out=ss_[:].rearrange("p a b -> p (a b)"), in_=psB[:, 0:2048]),
                         r=["B0", "B1", "B2", "B3"], w=[ssk])
                    S.dma("sp", ps_v[:, :, g, :], ss_[:], r=[ssk], w=["D:ps"])
            S.barrier()
            with contextlib.ExitStack() as ph:
                sall = [sb(ph, "sall%d" % i, [128, 16, 128]) for i in range(2)]
                wk_ = sb(ph, "pwk", [128, 16, 128])
                sv = sb(ph, "psv", [128, 16, 16])
                cand = sb(ph, "pcand", [128, 8, 256])
                cw = sb(ph, "pcw", [128, 8, 256])
                c16 = sb(ph, "pc16", [128, 8, 16])
                e16 = sb(ph, "pe16", [128, 8, 16])
                zz = sb(ph, "pzz", [128, 8])
                stt = sb(ph, "pstt", [128, NTT, 16])
                s0r = [sb(ph, "ps0r%d" % i, [128, 128, 8]) for i in range(2)]
                for tt in range(NTT):
                    sa = sall[tt % 2]
                    sak = "sall%d" % (tt % 2)
                    S.dma("sp", sa[:], ps_d[tt * 128:(tt + 1) * 128, :].rearrange("p (g n) -> p g n", g=16), r=["D:ps"], w=[sak])
                    for g in range(16):
                        S.op("dve", lambda e, sa=sa, g=g: e.max(out=sv[:, g, 0:8], in_=sa[:, g, :]), r=[sak], w=["psv"])
                        S.op("dve", lambda e, sa=sa, g=g: e.match_replace(out=wk_[:, g, :], in_to_replace=sv[:, g, 0:8],
                                                                          in_values=sa[:, g, :], imm_value=NEG),
                             r=[sak, "psv"], w=["pwk"])
                        S.op("dve", lambda e, g=g: e.max(out=sv[:, g, 8:16], in_=wk_[:, g, :]), r=["pwk"], w=["psv"])
                    sv4 = sv[:].rearrange("p (pp c) k -> p pp c k", c=2)
                    S.op("dve", lambda e, sv4=sv4: e.tensor_tensor(
                        out=cand[:].rearrange("p a (k l) -> p a k l", l=16),
                        in0=sv4[:, :, 0, :].unsqueeze(3).to_broadcast([128, 8, 16, 16]),
                        in1=sv4[:, :, 1, :].unsqueeze(2).to_broadcast([128, 8, 16, 16]), op=ALU.add),
                        r=["psv"], w=["pcand"])
                    for p in range(8):
                        S.op("dve", lambda e, p=p: e.max(out=c16[:, p, 0:8], in_=cand[:, p, :]), r=["pcand"], w=["pc16"])
                        S.op("dve", lambda e, p=p: e.match_replace(out=cw[:, p, :], in_to_replace=c16[:, p, 0:8],
                                                                   in_values=cand[:, p, :], imm_value=NEG),
                             r=["pcand", "pc16"], w=["pcw"])
                        S.op("dve", lambda e, p=p: e.max(out=c16[:, p, 8:16], in_=cw[:, p, :]), r=["pcw"], w=["pc16"])
                    S.op("dve", lambda e: e.tensor_tensor(out=e16[:], in0=c16[:], in1=c16[:, :, 0:1].to_broadcast([128, 8, 16]),
                                                          op=ALU.subtract), r=["pc16"], w=["pe16"])
                    S.op("act", lambda e: e.activation(out=e16[:], in_=e16[:], func=AF.Exp), r=["pe16"], w=["pe16"])
                    S.op("dve", lambda e: e.tensor_reduce(out=zz[:], in_=e16[:], op=ALU.add, axis=AX.X), r=["pe16"], w=["pzz"])
                    S.op("act", lambda e: e.activation(out=zz[:], in_=zz[:], func=AF.Ln), r=["pzz"], w=["pzz"])
                    S.op("dve", lambda e, tt=tt: e.tensor_copy(out=stt[:, tt, 0:8], in_=c16[:, :, 15]), r=["pc16"], w=["pstt"])
                    S.op("dve", lambda e, tt=tt: e.tensor_tensor(out=stt[:, tt, 8:16], in0=zz[:], in1=c16[:, :, 0], op=ALU.add),
                         r=["pc16", "pzz"], w=["pstt"])
                    S.op("dve", lambda e, tt=tt: e.tensor_scalar(out=stt[:, tt, 8:16], in0=stt[:, tt, 8:16], scalar1=-1.0, scalar2=None,
                                                                 op0=ALU.mult), r=["pstt"], w=["pstt"])
                    s0 = s0r[tt % 2]
                    s0k = "ps0r%d" % (tt % 2)
                    S.op("pool", lambda e, sa=sa, s0=s0: e.tensor_copy(
                        out=s0[:], in_=sa[:].rearrange("p (pp c) n -> p c n pp", c=2)[:, 0]), r=[sak], w=[s0k])
                    S.dma("sp", s0r_d[tt * 128:(tt + 1) * 128, :, :], s0[:], r=[s0k], w=["D:s0r"])
                S.dma("sp", pst_d.rearrange("(tt p) k -> p tt k", p=128), stt[:], r=["pstt"], w=["D:pst"])

        def peerB_phase(l):
            S.barrier()
            NTG = SEQ // PEER_TG
            TPG = PEER_TG // 128
            NIG = 128 // PEER_IG
            NE2 = PEER_IG // PEER_EG
            with contextlib.ExitStack() as ph:
                g2b = sb(ph, "g2b", [128, D])
                S.dma("sp", g2b[:], gate_row(l, 1).partition_broadcast(128), r=["D:modrow"], w=["g2b"])
                hTg = sb(ph, "hTg", [128, 16, PEER_TG], BF16)
                s1g = sb(ph, "s1g", [128, TPG, 8, 128])
                s0gs = [sb(ph, "s0g%d" % i, [128, TPG, PEER_IG, 8]) for i in range(2)]
                stg_ = sb(ph, "pstg", [128, TPG, 16])
                outacc = sb(ph, "outacc", [128, TPG, D])
                WTs = [sb(ph, "WT%d" % i, [128, PEER_IG, PEER_TG], BF16) for i in range(2)]
                vbuf = [sb(ph, "pv%d" % i, [128, PEER_IG, 128]) for i in range(2)]
                ebuf = [sb(ph, "pe%d" % i, [128, PEER_IG, 128], BF16) for i in range(2)]
                w8 = sb(ph, "pw8", [128, 8, PEER_IG, 128], BF16)
                uTg = [sb(ph, "uTg%d" % i, [128, 16, PEER_EG * 128], BF16) for i in range(2)]
                vg = [sb(ph, "vg%d" % i, [128, PEER_EG, D], BF16) for i in range(2)]
                gl1 = sb(ph, "gl", [128, PEER_EG, PEER_TG])
                gl = [gl1, gl1]
                G = [sb(ph, "G%d" % i, [128, PEER_EG, PEER_TG], BF16) for i in range(2)]
                uTl = uT[l].rearrange("(dc p) e -> p dc e", p=128)
                pvl = pv[l].rearrange("(c p) d -> p c d", p=128)
                ps_v = ps_d.rearrange("(tt p) (pp c n) -> p tt pp c n", p=128, c=2, n=128)
                cnt = {"w": 0}

                def s0_load(tg, ig):
                    t0_ = tg * PEER_TG
                    S.dma("sp", s0gs[ig % 2][:], s0r_d[t0_:t0_ + PEER_TG, ig * PEER_IG:(ig + 1) * PEER_IG, :].rearrange("(tt p) i pp -> p tt i pp", p=128),
                          r=["D:s0r"], w=["s0g%d" % (ig % 2)])

                def wb_p(ig, tt, p):
                    s0g = s0gs[ig % 2]
                    b = cnt["w"] % 2
                    cnt["w"] += 1
                    vb, eb = vbuf[b], ebuf[b]
                    HP = PEER_IG // 2
                    S.op("pool", lambda e: e.tensor_tensor(
                        out=vb[:, 0:HP, :], in0=s1g[:, tt, p, :].unsqueeze(1).to_broadcast([128, HP, 128]),
                        in1=s0g[:, tt, 0:HP, p].unsqueeze(2).to_broadcast([128, HP, 128]), op=ALU.add),
                        r=["s1g", "s0g%d" % (ig % 2)], w=["pv%d" % b])
                    for i in range(HP, PEER_IG):
                        S.op("act", lambda e, i=i: e.activation(out=vb[:, i, :], in_=s1g[:, tt, p, :], func=AF.Identity,
                                                                bias=s0g[:, tt, i, p:p + 1]),
                             r=["s1g", "s0g%d" % (ig % 2)], w=["pva%d_%d" % (b, i)])
                    vkeys = ["pv%d" % b] + ["pva%d_%d" % (b, i) for i in range(HP, PEER_IG)]
                    S.op("act", lambda e: e.activation(
                        out=eb[:], in_=vb[:], func=AF.Exp, bias=stg_[:, tt, 8 + p:9 + p]),
                        r=vkeys + ["pstg"], w=["pe%d" % b])
                    S.op("dve", lambda e: e.scalar_tensor_tensor(
                        out=w8[:, p], in0=vb[:], scalar=stg_[:, tt, p:p + 1], in1=eb[:], op0=ALU.is_ge, op1=ALU.mult),
                        r=vkeys + ["pe%d" % b, "pstg"], w=["pw%d" % p])
                    for i in range(PEER_IG):
                        S.op("pe", lambda e, i=i: e.matmul(
                            psB[:, i * 128:(i + 1) * 128], lhsT=w8[:, p, i, :], rhs=identb[:],
                            start=(p == 0 and i % 4 == 0), stop=(p == 7)), r=["pw%d" % p, "identb"], w=["B%d" % (i // 4)])

                def wb_pe(ig, tt):
                    WT = WTs[ig % 2]
                    S.op("act", lambda e: e.copy(out=WT[:, :, tt * 128:(tt + 1) * 128],
                                                 in_=psB[:, 0:1024].rearrange("p (i t) -> p i t", t=128)),
                         r=["B0", "B1"], w=["WT%d_%d" % (ig % 2, tt)])

                def load_u(n):
                    ub = n % 2
                    S.dma("pool", uTg[ub][:], uTl[:, :, n * PEER_EG * 128:(n + 1) * PEER_EG * 128], w=["uTg%d" % ub])

                def load_v(n):
                    ub = n % 2
                    S.dma("pool", vg[ub][:], pvl[:, n * PEER_EG:(n + 1) * PEER_EG, :], w=["vg%d" % ub])

                def ao_act(n, ec):
                    ub = n % 2
                    for dc in range(16):
                        S.op("pe", lambda e, dc=dc: e.matmul(
                            psA[:, ec * 512:ec * 512 + PEER_TG], lhsT=uTg[ub][:, dc, ec * 128:(ec + 1) * 128], rhs=hTg[:, dc, :],
                            start=(dc == 0), stop=(dc == 15)), r=["uTg%d" % ub, "hTg"], w=["A%d" % ec])

                def ao_gelu(n):
                    for ec in range(PEER_EG):
                        S.op("act", lambda e, ec=ec: e.activation(out=gl1[:, ec, :], in_=psA[:, ec * 512:ec * 512 + PEER_TG], func=AF.Gelu),
                             r=["A%d" % ec], w=["gl_%d" % ec])

                def ao_gmult(n):
                    ub = n % 2
                    ig, eg2 = n // NE2, n % NE2
                    WT = WTs[ig % 2]
                    wkeys = ["WT%d_%d" % (ig % 2, tt) for tt in range(TPG)]
                    for ec in range(PEER_EG):
                        il = eg2 * PEER_EG + ec
                        S.op("dve", lambda e, ec=ec, il=il: e.tensor_tensor(
                            out=G[ub][:, ec, :], in0=gl1[:, ec, :], in1=WT[:, il, :], op=ALU.mult),
                            r=["gl_%d" % ec] + wkeys, w=["G%d" % ub])

                def ao_outacc(n, j):
                    ub = n % 2
                    tt, dq = j // 4, j % 4
                    bk = 2 + (j % 2)
                    for ec in range(PEER_EG):
                        S.op("pe", lambda e, ec=ec: e.matmul(
                            bankB(bk), lhsT=G[ub][:, ec, tt * 128:(tt + 1) * 128], rhs=vg[ub][:, ec, dq * 512:(dq + 1) * 512],
                            start=(ec == 0), stop=(ec == PEER_EG - 1)), r=["G%d" % ub, "vg%d" % ub], w=["B%d" % bk])
                    ok = "outacc%d_%d" % (tt, dq // 2)
                    dst = outacc[:, tt, dq * 512:(dq + 1) * 512]
                    if n == 0:
                        S.op("dve", lambda e: e.tensor_copy(out=dst, in_=bankB(bk)), r=["B%d" % bk], w=[ok])
                    else:
                        S.op("dve", lambda e: e.tensor_tensor(out=dst, in0=bankB(bk), in1=dst, op=ALU.add), r=["B%d" % bk, ok], w=[ok])

                NSTEP = NIG * NE2
                WPS = TPG // NE2
                NOUT = TPG * 4
                assert TPG % NE2 == 0 and PEER_EG == 4
                for tg in range(NTG):
                    t0 = tg * PEER_TG
                    S.dma("sp", hTg[:], hT_d[:, :, t0:t0 + PEER_TG], r=["D:hT"], w=["hTg"])
                    for tt in range(TPG):
                        S.dma("act", s1g[:, tt], ps_v[:, tg * TPG + tt, :, 1, :], r=["D:ps"], w=["s1g"])
                    S.dma("act", stg_[:], pst_d[t0:t0 + PEER_TG, :].rearrange("(tt p) k -> p tt k", p=128), r=["D:pst"], w=["pstg"])
                    s0_load(tg, 0)
                    s0_load(tg, 1)
                    load_u(0)
                    load_v(0)
                    for tt in range(TPG):
                        for p in range(8):
                            wb_p(0, tt, p)
                        wb_pe(0, tt)
                    for n in range(NSTEP + 1):
                        ig, k = n // NE2, n % NE2
                        if n < NSTEP:
                            if n + 1 < NSTEP:
                                load_u(n + 1)
                            if n >= 1:
                                load_v(n)
                        for wt in range(WPS):
                            if n < NSTEP:
                                for ec in range(wt * PEER_EG // WPS, (wt + 1) * PEER_EG // WPS):
                                    ao_act(n, ec)
                            if n < NSTEP and ig + 1 < NIG:
                                for p in range(8):
                                    wb_p(ig + 1, k * WPS + wt, p)
                                wb_pe(ig + 1, k * WPS + wt)
                            if n >= 1:
                                for sl in range(wt * NOUT // WPS, (wt + 1) * NOUT // WPS):
                                    ao_outacc(n - 1, sl)
                        if n < NSTEP:
                            ao_gelu(n)
                            if k == NE2 - 1 and ig + 2 < NIG:
                                s0_load(tg, ig + 2)
                            ao_gmult(n)
                    for tt in range(TPG):
                        r0 = t0 + tt * 128
                        for hf in range(2):
                            c0 = hf * 1024
                            xh = vbuf[0][:].rearrange("p a b -> p (a b)")
                            th = vbuf[1][:].rearrange("p a b -> p (a b)")
                            S.dma("sp", xh, xs[r0:r0 + 128, c0:c0 + 1024], r=["D:xs"], w=["pv0"] + ["pva0_%d" % i for i in range(PEER_IG // 2, PEER_IG)])
                            S.op("dve", lambda e, tt=tt, c0=c0, th=th: e.tensor_tensor(out=th, in0=outacc[:, tt, c0:c0 + 1024], in1=g2b[:, c0:c0 + 1024], op=ALU.mult),
                                 r=["outacc%d_%d" % (tt, hf), "g2b"], w=["pv1"] + ["pva1_%d" % i for i in range(PEER_IG // 2, PEER_IG)])
                            S.op("pool", lambda e, xh=xh, th=th: e.tensor_tensor(out=xh, in0=xh, in1=th, op=ALU.add), r=["pv0", "pv1"], w=["pv0"])
                            S.dma("sp", xs[r0:r0 + 128, c0:c0 + 1024], xh, r=["pv0"], w=["D:xs"])

        def final_phase():
            S.barrier()
            with contextlib.ExitStack() as ph:
                fgb = sb(ph, "fgb", [128, D])
                S.dma("sp", fgb[:], fg.partition_broadcast(128), w=["fgb"])
                xt = [sb(ph, "fx%d" % i, [128, D]) for i in range(2)]
                junk = sb(ph, "fjunk", [128, D])
                st = sb(ph, "fst", [128, 4])
                for tt in range(NTT):
                    xb = xt[tt % 2]
                    xk = "fx%d" % (tt % 2)
                    S.dma("sp" if tt % 2 else "act", xb[:], xs[tt * 128:(tt + 1) * 128, :], r=["D:xs"], w=[xk])
                    S.op("act", lambda e, xb=xb: e.activation(out=junk[:], in_=xb[:], func=AF.Square, accum_out=st[:, 0:1]),
                         r=[xk], w=["fjunk", "fst"])
                    S.op("dve", lambda e: e.tensor_scalar(out=st[:, 1:2], in0=st[:, 0:1], scalar1=1.0 / D, scalar2=EPS, op0=ALU.mult, op1=ALU.add),
                         r=["fst"], w=["fst"])
                    S.op("act", lambda e: e.activation(out=st[:, 1:2], in_=st[:, 1:2], func=AF.Sqrt), r=["fst"], w=["fst"])
                    S.op("dve", lambda e: e.reciprocal(out=st[:, 2:3], in_=st[:, 1:2]), r=["fst"], w=["fst"])
                    S.op("dve", lambda e, xb=xb: e.scalar_tensor_tensor(out=xb[:], in0=xb[:], scalar=st[:, 2:3], in1=fgb[:],
                                                                        op0=ALU.mult, op1=ALU.mult), r=[xk, "fst", "fgb"], w=[xk])
                    S.dma("sp", out_d[tt * 128:(tt + 1) * 128, :], xb[:], r=[xk], w=["D:out"])

        def run_layers():
            for l in range(nlayers):
                norm_phase("n1_%d" % l, x_in if l == 0 else xs, l, 0)
                if stop == "norm1":
                    return
                proj_phase(l)
                if stop == "proj":
                    return
                attn_phase(l)
                if stop in ("attnB", "attnC", "attn"):
                    return
                outproj_phase(l)
                if stop == "outproj":
                    return
                norm_phase("n2_%d" % l, xs, l, 1)
                peerA_phase(l)
                if stop == "peerA":
                    return
                peerB_phase(l)
                if stop == "peerB":
                    return
            final_phase()

        run_layers()
        S.finish("sp")
    return nc


def _rope_tables():
    def tab(hd):
        half = hd // 2
        inv = (10000.0 ** (-np.arange(half, dtype=np.float32) / half)).astype(np.float32)
        ang = np.arange(SEQ, dtype=np.float32)[:, None] * inv[None, :]
        cos = np.cos(ang).astype(np.float32).T
        sin = np.sin(ang).astype(np.float32).T
        cos_f = np.concatenate([cos, cos], axis=0)
        sin_s = np.concatenate([-sin, sin], axis=0)
        reps = 128 // hd
        out = np.stack([np.tile(cos_f, (reps, 1)), np.tile(sin_s, (reps, 1))], axis=1)
        return np.ascontiguousarray(out, dtype=np.float32)
    return tab(128), tab(64)


def prep_inputs(x, c, ada_w, ada_b, norm1_g, norm2_g, w_in, out_norm_g, w_out,
                peer_wq, peer_subkeys, peer_u, peer_v, final_g):
    f = lambda a: np.ascontiguousarray(np.asarray(a), dtype=np.float32)
    x, c = f(x), f(c)
    cs128, cs64 = _rope_tables()
    shared = {
        "ada_w": f(ada_w),
        "ada_bT": f(np.asarray(ada_b).reshape(DEPTH, 96, 128).transpose(2, 0, 1).reshape(128, DEPTH * 96)),
        "gT": f(np.stack([np.asarray(norm1_g).reshape(DEPTH, 16, 128), np.asarray(norm2_g).reshape(DEPTH, 16, 128)], axis=1)
                .transpose(3, 0, 1, 2).reshape(128, DEPTH * 32)),
        "w_in": f(w_in),
        "og": f(out_norm_g),
        "w_out": f(w_out),
        "peer_wq": f(peer_wq),
        "skT": f(np.asarray(peer_subkeys).reshape(DEPTH, 16, 128, 128).transpose(0, 1, 3, 2)),
        "uT": f(np.asarray(peer_u).transpose(0, 2, 1)),
        "pv": f(peer_v),
        "fg": f(np.asarray(final_g).reshape(1, D)),
        "ident": np.eye(128, dtype=np.float32),
        "cs128": cs128,
        "cs64": cs64,
    }
    maps = []
    for b in range(x.shape[0]):
        m = dict(shared)
        m["x"] = x[b]
        m["cT"] = f(c[b].reshape(16, 128).T)
        maps.append(m)
    return maps


def kernel(**inputs):
    maps = prep_inputs(**inputs)
    nc = build()
    res = run_bass_kernel_spmd(nc, maps, core_ids=list(range(len(maps))))
    return np.stack([r["out"] for r in res.results], axis=0).astype(np.float32)
```

```python
import contextlib
import numpy as np
import concourse.bass as bass
import concourse.mybir as mybir
from concourse.bass_utils import run_bass_kernel_spmd

F32 = mybir.dt.float32
BF16 = mybir.dt.bfloat16
AF = mybir.ActivationFunctionType
ALU = mybir.AluOpType
AX = mybir.AxisListType

SEQ = 2048
D = 2048
NTT = SEQ // 128
DEPTH = 2
HD = 128
EPS = 1e-6
NEG = -1.0e30
IN_WIDTH = 5968
C_QA, C_KA, C_VA, C_IQ, C_IK, C_IW = 0, 768, 896, 1024, 2048, 2112
C_QB, C_KB, C_VB, C_QC, C_KC, C_VC = 2128, 2768, 3408, 4048, 4688, 5328
ATT_SCALE = HD ** -0.5
IDX_SCALE = (64 ** -0.5) * (16 ** -0.5)
DSA_TOPK = 256
NKEYS = 128
PEER_TG = 512
PEER_EG = 4
PEER_IG = 8


class Sched:
    NDMA = 40
    NHW = 24

    def __init__(self, nc, es):
        self.nc = nc
        self.engs = {"pe": nc.tensor, "act": nc.scalar, "dve": nc.vector,
                     "pool": nc.gpsimd, "sp": nc.sync}
        self.sem = {k: es.enter_context(nc.semaphore("sem_" + k)) for k in self.engs}
        self.cnt = {k: 0 for k in self.engs}
        self.dsem = [es.enter_context(nc.semaphore("dsem%d" % i)) for i in range(self.NDMA)]
        self.dcnt = [0] * self.NDMA
        self.dnext = 0
        self.dnext_sw = 0
        self.known = {k: {} for k in self.engs}
        self.res = {}
        self.ninstr = 0

    def _semobj(self, key):
        return self.sem[key] if isinstance(key, str) else self.dsem[key]

    def _wait(self, eng, key, val):
        if val <= 0:
            return
        kn = self.known[eng]
        if kn.get(key, 0) >= val:
            return
        self.engs[eng].wait_ge(self._semobj(key), val)
        kn[key] = val

    def _deps(self, r, w):
        deps = {}

        def add(k, v):
            if deps.get(k, 0) < v:
                deps[k] = v
        for key in r:
            st = self.res.get(key)
            if st is not None and st["w"] is not None:
                add(*st["w"])
        for key in w:
            st = self.res.get(key)
            if st is not None:
                if st["w"] is not None:
                    add(*st["w"])
                for k, v in st["r"].items():
                    add(k, v)
        return deps

    def _commit(self, r, w, stamp):
        k, v = stamp
        for key in r:
            st = self.res.setdefault(key, {"w": None, "r": {}})
            if st["r"].get(k, 0) < v:
                st["r"][k] = v
        for key in w:
            self.res[key] = {"w": stamp, "r": {}}

    def op(self, eng, fn, r=(), w=()):
        for k, v in self._deps(r, w).items():
            if eng == "pe" and k == "pe":
                continue
            self._wait(eng, k, v)
        ins = fn(self.engs[eng])
        self.cnt[eng] += 1
        ins.then_inc(self.sem[eng], 1)
        self._commit(r, w, (eng, self.cnt[eng]))
        self.ninstr += 1
        return ins

    def dma(self, eng, out, in_, r=(), w=(), **kw):
        for k, v in self._deps(r, w).items():
            self._wait(eng, k, v)
        if eng == "pool":
            i = self.NHW + self.dnext_sw
            self.dnext_sw = (self.dnext_sw + 1) % (self.NDMA - self.NHW)
        else:
            i = self.dnext
            self.dnext = (self.dnext + 1) % self.NHW
        self._wait(eng, i, self.dcnt[i])
        ins = self.engs[eng].dma_start(out=out, in_=in_, **kw)
        self.dcnt[i] += 16
        ins.then_inc(self.dsem[i], 16)
        self._commit(r, w, (i, self.dcnt[i]))
        self.ninstr += 1
        return ins

    def barrier(self):
        for eng in self.engs:
            for k in self.engs:
                if k != eng:
                    self._wait(eng, k, self.cnt[k])
            for i in range(self.NDMA):
                self._wait(eng, i, self.dcnt[i])

    def finish(self, eng="sp"):
        for k in self.engs:
            self._wait(eng, k, self.cnt[k])
        for i in range(self.NDMA):
            self._wait(eng, i, self.dcnt[i])


def build(nlayers=DEPTH, taps=(), stop=None, peer_dummy=False):
    nc = bass.Bass("TRN2", target_bir_lowering=False)

    def din(name, shape, dt=F32):
        return nc.dram_tensor(name, shape, dt, kind="ExternalInput").ap()

    def dscr(name, shape, dt=F32):
        kind = "ExternalOutput" if name in taps else "Internal"
        return nc.dram_tensor(name, shape, dt, kind=kind).ap()

    x_in = din("x", [SEQ, D])
    cT = din("cT", [128, 16])
    ada_w = din("ada_w", [DEPTH, D, 6 * D])
    ada_bT = din("ada_bT", [128, DEPTH * 96])
    gT = din("gT", [128, DEPTH * 32])
    w_in = din("w_in", [DEPTH, D, IN_WIDTH])
    og = din("og", [DEPTH, D])
    w_out = din("w_out", [DEPTH, D, D])
    wq = din("peer_wq", [DEPTH, D, D])
    skT = din("skT", [DEPTH, 16, 128, 128])
    uT = din("uT", [1, 128, 128] if peer_dummy else [nlayers, D, NKEYS * NKEYS])
    pv = din("pv", [1, 128, 128] if peer_dummy else [nlayers, NKEYS * NKEYS, D])
    fg = din("fg", [1, D])
    ident_d = din("ident", [128, 128])
    cs128 = din("cs128", [128, 2, SEQ])
    cs64 = din("cs64", [128, 2, SEQ])
    out_d = nc.dram_tensor("out", [SEQ, D], F32, kind="ExternalOutput").ap()

    xs = dscr("xs", [SEQ, D])
    hT_d = dscr("hT", [128, 16, SEQ], BF16)
    modrow = dscr("modrow", [DEPTH * 96, 128])
    qaT = dscr("qaT", [6, 128, SEQ], BF16)
    kaT = dscr("kaT", [128, SEQ], BF16)
    iqT = dscr("iqT", [8, 128, SEQ], BF16)
    ikT = dscr("ikT", [128, SEQ], BF16)
    qbT = dscr("qbT", [5, 128, SEQ], BF16)
    kbT = dscr("kbT", [5, 128, SEQ], BF16)
    qcT = dscr("qcT", [5, 128, SEQ], BF16)
    kcT = dscr("kcT", [5, 128, SEQ], BF16)
    vtok = dscr("vtok", [SEQ, 1408], BF16)
    iw_d = dscr("iw", [SEQ, 16])
    ynT = dscr("ynT", [16, 128, SEQ], BF16)
    ps_d = dscr("peer_s", [SEQ, 2048])
    s0r_d = dscr("peer_s0r", [SEQ, 128, 8])
    pst_d = dscr("peer_st", [SEQ, 16])

    es = contextlib.ExitStack()
    with es:
        S = Sched(nc, es)

        sbn = [0]

        def sb(stack, name, shape, dt=F32):
            sbn[0] += 1
            return stack.enter_context(nc.sbuf_tensor("s%d_%s" % (sbn[0], name), shape, dt))

        psA = es.enter_context(nc.psum_tensor("psA", [128, 2048], F32))
        psB = es.enter_context(nc.psum_tensor("psB", [128, 2048], F32))

        def bankA(i):
            return psA[:, i * 512:(i + 1) * 512]

        def bankB(i):
            return psB[:, i * 512:(i + 1) * 512]

        negreg = nc.gpsimd.to_reg(NEG)
        ident = sb(es, "ident", [128, 128])
        identb = sb(es, "identb", [128, 128], BF16)
        modT = sb(es, "modT", [128, DEPTH * 96])
        scs = sb(es, "scs", [128, DEPTH * 32])
        gsb = sb(es, "gsb", [128, DEPTH * 32])
        S.dma("sp", ident[:], ident_d[:, :], w=["ident"])
        S.op("dve", lambda e: e.tensor_copy(out=identb[:], in_=ident[:]), r=["ident"], w=["identb"])
        S.dma("sp", gsb[:], gT[:, :], w=["gsb"])

        with contextlib.ExitStack() as ph:
            cT_sb = sb(ph, "cT_sb", [128, 16])
            cond = sb(ph, "cond", [128, 16])
            abT = sb(ph, "abT", [128, DEPTH * 96])
            wbuf = [sb(ph, "adaw%d" % i, [128, 16, 512]) for i in range(2)]
            S.dma("sp", cT_sb[:], cT[:, :], w=["cT"])
            S.dma("sp", abT[:], ada_bT[:, :], w=["abT"])
            S.op("act", lambda e: e.activation(out=cond[:], in_=cT_sb[:], func=AF.Silu), r=["cT"], w=["cond"])
            gi = 0
            for l in range(nlayers):
                awl = ada_w[l].rearrange("(kc p) n -> p kc n", p=128)
                for g in range(24):
                    wb = wbuf[gi % 2]
                    key = "adaw%d" % (gi % 2)
                    S.dma("sp" if gi % 2 == 0 else "act", wb[:], awl[:, :, g * 512:(g + 1) * 512], w=[key])
                    for j in range(4):
                        col = l * 96 + g * 4 + j
                        for kc in range(16):
                            S.op("pe", lambda e, wb=wb, j=j, kc=kc, col=col: e.matmul(
                                psA[:, col:col + 1], lhsT=wb[:, kc, j * 128:(j + 1) * 128],
                                rhs=cond[:, kc:kc + 1], start=(kc == 0), stop=(kc == 15)),
                                r=[key, "cond"], w=["A0"])
                    gi += 1
            ncol = nlayers * 96
            S.op("dve", lambda e: e.tensor_tensor(out=modT[:, 0:ncol], in0=psA[:, 0:ncol], in1=abT[:, 0:ncol], op=ALU.add),
                 r=["A0", "abT"], w=["modT"])
            for l in range(nlayers):
                for which in range(2):
                    src = modT[:, l * 96 + 16 + which * 48: l * 96 + 32 + which * 48]
                    dst = scs[:, l * 32 + which * 16: l * 32 + which * 16 + 16]
                    gsl = gsb[:, l * 32 + which * 16: l * 32 + which * 16 + 16]
                    S.op("dve", lambda e, src=src, dst=dst: e.tensor_scalar(out=dst, in0=src, scalar1=1.0, scalar2=None, op0=ALU.add),
                         r=["modT"], w=["scs"])
                    S.op("dve", lambda e, dst=dst, gsl=gsl: e.tensor_tensor(out=dst, in0=dst, in1=gsl, op=ALU.mult),
                         r=["scs", "gsb"], w=["scs"])
            mr = sb(ph, "mr", [128, 2, 128])
            S.op("pe", lambda e: e.transpose(out=psA[:, 512:640], in_=modT[:, 0:128], identity=ident[:]), r=["modT", "ident"], w=["A1"])
            S.op("dve", lambda e: e.tensor_copy(out=mr[:, 0, :], in_=psA[:, 512:640]), r=["A1"], w=["mr"])
            if ncol > 128:
                S.op("pe", lambda e: e.transpose(out=psA[0:64, 1024:1152], in_=modT[:, 128:192], identity=ident[:]), r=["modT", "ident"], w=["A2"])
                S.op("dve", lambda e: e.tensor_copy(out=mr[0:64, 1, :], in_=psA[0:64, 1024:1152]), r=["A2"], w=["mr"])
            S.dma("sp", modrow[0:min(ncol, 128), :], mr[0:min(ncol, 128), 0, :], r=["mr"], w=["D:modrow"])
            if ncol > 128:
                S.dma("sp", modrow[128:192, :], mr[0:64, 1, :], r=["mr"], w=["D:modrow"])

        def gate_row(l, which):
            r0 = l * 96 + 32 + which * 48
            return modrow[r0:r0 + 16, :].rearrange("(o a) b -> o (a b)", o=1)

        def norm_phase(tag, xsrc, l, which):
            sc = scs[:, l * 32 + which * 16: l * 32 + which * 16 + 16]
            shb = l * 96 + which * 48
            sh = modT[:, shb:shb + 16]
            S.barrier()
            with contextlib.ExitStack() as ph:
                xt = [sb(ph, "%sx%d" % (tag, i), [128, 4, D]) for i in range(2)]
                junk = sb(ph, tag + "junk", [128, D])
                ss = sb(ph, tag + "ss", [128, 4])
                rstd = sb(ph, tag + "rstd", [128, 4])
                hst = [sb(ph, "%shst%d" % (tag, i), [128, 16, 512], BF16) for i in range(2)]
                for tg in range(4):
                    xb = xt[tg % 2]
                    xk = "%sx%d" % (tag, tg % 2)
                    hk = "%shst%d" % (tag, tg % 2)
                    S.dma("sp", xb[:], xsrc[tg * 512:(tg + 1) * 512, :].rearrange("(i p) d -> p i d", p=128),
                          r=["D:xs"], w=[xk])
                    for i in range(4):
                        S.op("act", lambda e, xb=xb, i=i: e.activation(out=junk[:], in_=xb[:, i, :], func=AF.Square,
                                                                       accum_out=ss[:, i:i + 1]),
                             r=[xk], w=[tag + "junk", tag + "ss"])
                    S.op("dve", lambda e: e.tensor_scalar(out=rstd[:], in0=ss[:], scalar1=1.0 / D, scalar2=EPS,
                                                          op0=ALU.mult, op1=ALU.add), r=[tag + "ss"], w=[tag + "rstd"])
                    S.op("act", lambda e: e.activation(out=rstd[:], in_=rstd[:], func=AF.Sqrt), r=[tag + "rstd"], w=[tag + "rstd"])
                    S.op("dve", lambda e: e.reciprocal(out=rstd[:], in_=rstd[:]), r=[tag + "rstd"], w=[tag + "rstd"])
                    for i in range(4):
                        if i % 2:
                            S.op("act", lambda e, xb=xb, i=i: e.activation(out=xb[:, i, :], in_=xb[:, i, :], func=AF.Copy,
                                                                           scale=rstd[:, i:i + 1]), r=[xk, tag + "rstd"], w=[xk])
                        else:
                            S.op("dve", lambda e, xb=xb, i=i: e.tensor_scalar(
                                out=xb[:, i, :], in0=xb[:, i, :], scalar1=rstd[:, i:i + 1], scalar2=None, op0=ALU.mult),
                                r=[xk, tag + "rstd"], w=[xk])
                    hs = hst[tg % 2]
                    for dc in range(16):
                        bk = dc % 4
                        for i in range(4):
                            S.op("pe", lambda e, xb=xb, i=i, dc=dc, bk=bk: e.transpose(
                                out=psA[:, bk * 512 + i * 128: bk * 512 + (i + 1) * 128],
                                in_=xb[:, i, dc * 128:(dc + 1) * 128], identity=ident[:]),
                                r=[xk, "ident"], w=["A%d" % bk])
                        S.op("act", lambda e, hs=hs, dc=dc, bk=bk: e.activation(
                            out=hs[:, dc, :], in_=bankA(bk), func=AF.Identity,
                            scale=sc[:, dc:dc + 1], bias=sh[:, dc:dc + 1]),
                            r=["A%d" % bk, "scs", "modT"], w=[hk])
                    S.dma("sp", hT_d[:, :, tg * 512:(tg + 1) * 512], hs[:], r=[hk], w=["D:hT"])

        def proj_phase(l):
            wl = w_in[l].rearrange("(dc p) n -> p dc n", p=128)
            S.barrier()
            with contextlib.ExitStack() as ph:
                hT = sb(ph, "hT", [128, 16, SEQ], BF16)
                for q in range(4):
                    S.dma("sp" if q % 2 == 0 else "act", hT[:, q * 4:(q + 1) * 4, :], hT_d[:, q * 4:(q + 1) * 4, :],
                          r=["D:hT"], w=["hT%d" % q])
                hkeys = ["hT%d" % q for q in range(4)]
                c128 = sb(ph, "c128", [128, 2, SEQ])
                c64 = sb(ph, "c64", [128, 2, SEQ])
                S.dma("sp", c128[:], cs128[:, :, :], w=["c128"])
                S.dma("act", c64[:], cs64[:, :, :], w=["c64"])
                wts = [sb(ph, "wt%d" % i, [128, 16, 128], BF16) for i in range(2)]
                wss = [sb(ph, "ws%d" % i, [128, 16, 128], BF16) for i in range(2)]
                stg = [sb(ph, "stg%d" % i, [128, SEQ], BF16) for i in range(2)]
                t1 = sb(ph, "rt1", [128, 512])
                t2 = sb(ph, "rt2", [128, 512])

                chunks = []
                for h in range(6):
                    chunks.append((qaT[h], [(0, C_QA + h * 128, 128)], 128))
                chunks.append((kaT, [(0, C_KA, 128)], 128))
                for c in range(8):
                    chunks.append((iqT[c], [(0, C_IQ + c * 128, 128)], 64))
                chunks.append((ikT, [(0, C_IK, 64), (64, C_IK, 64)], 64))
                for h in range(5):
                    chunks.append((qbT[h], [(0, C_QB + h * 128, 128)], None))
                    chunks.append((kbT[h], [(0, C_KB + h * 128, 128)], None))
                for h in range(5):
                    chunks.append((qcT[h], [(0, C_QC + h * 128, 128)], 128))
                    chunks.append((kcT[h], [(0, C_KC + h * 128, 128)], 128))

                for ci, (dst, pieces, rope) in enumerate(chunks):
                    wt = wts[ci % 2]
                    ws = wss[ci % 2]
                    wk = "wt%d" % (ci % 2)
                    wsk = "ws%d" % (ci % 2)
                    st = stg[ci % 2]
                    sk = "stg%d" % (ci % 2)
                    for (dc0, sc0, wd) in pieces:
                        S.dma("pool", wt[:, :, dc0:dc0 + wd], wl[:, :, sc0:sc0 + wd], w=[wk])
                    if rope is not None:
                        half = rope // 2
                        for (dc0, sc0, wd) in pieces:
                            for b0 in range(0, wd, rope):
                                S.dma("pool", ws[:, :, dc0 + b0:dc0 + b0 + half],
                                      wl[:, :, sc0 + b0 + half:sc0 + b0 + rope], w=[wsk])
                                S.dma("pool", ws[:, :, dc0 + b0 + half:dc0 + b0 + rope],
                                      wl[:, :, sc0 + b0:sc0 + b0 + half], w=[wsk])
                    tab = c128 if rope == 128 else c64
                    tabk = "c128" if rope == 128 else "c64"
                    for tq in range(4):
                        ba = tq % 2
                        for dc in range(16):
                            S.op("pe", lambda e, wt=wt, dc=dc, tq=tq, ba=ba: e.matmul(
                                bankA(ba), lhsT=wt[:, dc, :], rhs=hT[:, dc, tq * 512:(tq + 1) * 512],
                                start=(dc == 0), stop=(dc == 15)), r=[wk, hkeys[dc // 4]], w=["A%d" % ba])
                        if rope is None:
                            S.op("act", lambda e, st=st, tq=tq, ba=ba: e.copy(out=st[:, tq * 512:(tq + 1) * 512], in_=bankA(ba)),
                                 r=["A%d" % ba], w=[sk])
                        else:
                            for dc in range(16):
                                S.op("pe", lambda e, ws=ws, dc=dc, tq=tq, ba=ba: e.matmul(
                                    bankB(ba), lhsT=ws[:, dc, :], rhs=hT[:, dc, tq * 512:(tq + 1) * 512],
                                    start=(dc == 0), stop=(dc == 15)), r=[wsk, hkeys[dc // 4]], w=["B%d" % ba])
                            S.op("dve", lambda e, tq=tq, ba=ba, tab=tab: e.tensor_tensor(
                                out=t1[:], in0=bankA(ba), in1=tab[:, 0, tq * 512:(tq + 1) * 512], op=ALU.mult),
                                r=["A%d" % ba, tabk], w=["rt1"])
                            S.op("dve", lambda e, tq=tq, ba=ba, tab=tab: e.tensor_tensor(
                                out=t2[:], in0=bankB(ba), in1=tab[:, 1, tq * 512:(tq + 1) * 512], op=ALU.mult),
                                r=["B%d" % ba, tabk], w=["rt2"])
                            S.op("pool", lambda e, st=st, tq=tq: e.tensor_tensor(
                                out=st[:, tq * 512:(tq + 1) * 512], in0=t1[:], in1=t2[:], op=ALU.add),
                                r=["rt1", "rt2"], w=[sk])
                    S.dma("sp", dst, st[:], r=[sk], w=["D:qk"])

                wv = sb(ph, "wv", [128, 16, 1424], BF16)
                for (dc0, sc0, wd) in [(0, C_VA, 128), (128, C_VB, 640), (768, C_VC, 640), (1408, C_IW, 16)]:
                    S.dma("pool", wv[:, :, dc0:dc0 + wd], wl[:, :, sc0:sc0 + wd], w=["wv"])
                vst = [sb(ph, "vst%d" % i, [128, 1408], BF16) for i in range(2)]
                iwst = sb(ph, "iwst", [128, NTT, 16])
                for tt in range(NTT):
                    vs = vst[tt % 2]
                    vk = "vst%d" % (tt % 2)
                    for nb, (n0, n1) in enumerate([(0, 512), (512, 1024), (1024, 1424)]):
                        for dc in range(16):
                            S.op("pe", lambda e, tt=tt, dc=dc, nb=nb, n0=n0, n1=n1: e.matmul(
                                psB[:, nb * 512: nb * 512 + (n1 - n0)], lhsT=hT[:, dc, tt * 128:(tt + 1) * 128],
                                rhs=wv[:, dc, n0:n1], start=(dc == 0), stop=(dc == 15)),
                                r=["wv", hkeys[dc // 4]], w=["B%d" % nb])
                    S.op("act", lambda e, vs=vs: e.copy(out=vs[:, 0:1024], in_=psB[:, 0:1024]), r=["B0", "B1"], w=[vk])
                    S.op("dve", lambda e, vs=vs: e.tensor_copy(out=vs[:, 1024:1408], in_=psB[:, 1024:1408]), r=["B2"], w=[vk])
                    S.op("dve", lambda e, tt=tt: e.tensor_scalar(out=iwst[:, tt, :], in0=psB[:, 1408:1424], scalar1=IDX_SCALE,
                                                                 scalar2=None, op0=ALU.mult), r=["B2"], w=["iwst"])
                    S.dma("sp", vtok[tt * 128:(tt + 1) * 128, :], vs[:], r=[vk], w=["D:vtok"])
                S.dma("sp", iw_d.rearrange("(tt p) h -> p tt h", p=128), iwst[:], r=["iwst"], w=["D:iw"])

        def run_chains(gens, disjoint=True):
            gens = list(gens)
            if not disjoint:
                for v in gens[0]:
                    if v == "zfree":
                        break
            while gens:
                for g in list(gens):
                    try:
                        next(g)
                    except StopIteration:
                        gens.remove(g)

        def attn_phase(l):
            S.barrier()
            with contextlib.ExitStack() as ph:
                ogb = sb(ph, "ogb", [128, D])
                S.dma("sp", ogb[:], og[l:l + 1, :].partition_broadcast(128), w=["ogb"])
                qT = [sb(ph, "qT%d" % i, [128, SEQ], BF16) for i in range(2)]
                kT = [sb(ph, "kT%d" % i, [128, SEQ], BF16) for i in range(2)]
                vv = [sb(ph, "vv%d" % i, [128, NTT, 128], BF16) for i in range(2)]
                ynst = [sb(ph, "ynst%d" % i, [128, SEQ], BF16) for i in range(2)]
                F1 = [sb(ph, "f1_%d" % c, [128, SEQ + 1]) for c in range(2)]
                F2 = [sb(ph, "f2_%d" % c, [128, SEQ + 1]) for c in range(2)]
                F3 = [sb(ph, "f3_%d" % c, [128, SEQ + 1]) for c in range(2)]
                PB = [sb(ph, "pb_%d" % c, [128, SEQ], BF16) for c in range(2)]
                AT = [sb(ph, "aT_%d" % c, [128, NTT, 128], BF16) for c in range(2)]
                SM = [sb(ph, "sm_%d" % c, [128, 32]) for c in range(2)]
                YSB = [sb(ph, "ysb_%d" % c, [128, 128]) for c in range(2)]
                YNB = [sb(ph, "ynb_%d" % c, [128, 128], BF16) for c in range(2)]
                JK = [sb(ph, "jk_%d" % c, [128, 128]) for c in range(2)]
                M8 = [sb(ph, "m8_%d" % c, [128, 8]) for c in range(2)]
                PEN16 = [sb(ph, "pen16_%d" % c, [128, 16]) for c in range(2)]
                GS8 = [sb(ph, "gs8_%d" % c, [128, 8]) for c in range(2)]
                for c in range(2):
                    S.op("dve", lambda e, c=c: e.memset(F2[c][:, 0:1], 0.0), w=["f2_%d" % c])
                psBb = psB[:].bitcast(BF16)

                def load_head(slot, qsrc, ksrc, vcol):
                    S.dma("sp", qT[slot][:], qsrc, r=["D:qk"], w=["qT%d" % slot])
                    S.dma("act", kT[slot][:], ksrc, r=["D:qk"], w=["kT%d" % slot])
                    S.dma("sp", vv[slot][:], vtok[:, vcol:vcol + 128].rearrange("(st p) n -> p st n", p=128),
                          r=["D:vtok"], w=["vv%d" % slot])

                def scores(c, qap, qkey, ksb, kkey, W):
                    base = c * 1024 if W <= 1024 else 0
                    keys = []
                    for ch in range((W + 511) // 512):
                        wc = min(512, W - ch * 512)
                        o = base + ch * 512
                        S.op("pe", lambda e, o=o, wc=wc, ch=ch: e.matmul(
                            psA[:, o:o + wc], lhsT=qap, rhs=ksb[:, ch * 512:ch * 512 + wc], start=True, stop=True),
                            r=[qkey, kkey], w=["A%d" % (o // 512)])
                        keys.append("A%d" % (o // 512))
                    return psA[:, base:base + W], keys

                def pv_finish(c, vsb, vkey, qi, W, head, dst, dkey, rinv):
                    nk = W // 128
                    sm, at, pbc = SM[c], AT[c], PB[c]
                    for g0 in range(0, nk, 8):
                        n = min(8, nk - g0)
                        for j in range(n):
                            S.op("pe", lambda e, j=j, g0=g0: e.transpose(
                                out=psBb[:, c * 1024 + j * 128:c * 1024 + (j + 1) * 128],
                                in_=pbc[:, (g0 + j) * 128:(g0 + j + 1) * 128], identity=identb[:]),
                                r=["pb_%d" % c, "identb"], w=["B%d" % c])
                        yield
                        eng = "act" if g0 == 0 else "dve"
                        if eng == "act":
                            S.op("act", lambda e, g0=g0, n=n: e.copy(out=at[:, g0:g0 + n, :].rearrange("p a b -> p (a b)"),
                                                                     in_=psBb[:, c * 1024:c * 1024 + n * 128]),
                                 r=["B%d" % c], w=["aT_%d" % c])
                        else:
                            S.op("dve", lambda e, g0=g0, n=n: e.tensor_copy(out=at[:, g0:g0 + n, :].rearrange("p a b -> p (a b)"),
                                                                            in_=psBb[:, c * 1024:c * 1024 + n * 128]),
                                 r=["B%d" % c], w=["aT_%d" % c])
                        yield
                    yb = 2 + c
                    yps = psB[:, yb * 512:yb * 512 + 128]
                    for sc in range(nk):
                        S.op("pe", lambda e, sc=sc: e.matmul(yps, lhsT=at[:, sc, :], rhs=vsb[:, sc, :],
                                                             start=(sc == 0), stop=(sc == nk - 1)),
                             r=["aT_%d" % c, vkey], w=["B%d" % yb])
                    yield
                    ysb, ynb, jk = YSB[c], YNB[c], JK[c]
                    if rinv is None:
                        S.op("act", lambda e: e.copy(out=ysb[:], in_=yps), r=["B%d" % yb], w=["ysb_%d" % c])
                    else:
                        S.op("dve", lambda e: e.tensor_scalar(out=ysb[:], in0=yps, scalar1=rinv, scalar2=None, op0=ALU.mult),
                             r=["B%d" % yb, "sm_%d" % c], w=["ysb_%d" % c])
                    yield
                    S.op("act", lambda e: e.activation(out=jk[:], in_=ysb[:], func=AF.Square, accum_out=sm[:, 8:9]),
                         r=["ysb_%d" % c], w=["jk_%d" % c, "sm_%d" % c])
                    yield
                    S.op("dve", lambda e: e.tensor_scalar(out=sm[:, 9:10], in0=sm[:, 8:9], scalar1=1.0 / HD, scalar2=EPS,
                                                          op0=ALU.mult, op1=ALU.add), r=["sm_%d" % c], w=["sm_%d" % c])
                    yield
                    S.op("act", lambda e: e.activation(out=sm[:, 9:10], in_=sm[:, 9:10], func=AF.Ln), r=["sm_%d" % c], w=["sm_%d" % c])
                    S.op("act", lambda e: e.activation(out=sm[:, 10:11], in_=sm[:, 9:10], func=AF.Exp, scale=-0.5),
                         r=["sm_%d" % c], w=["sm_%d" % c])
                    yield
                    S.op("dve", lambda e: e.scalar_tensor_tensor(out=ynb[:], in0=ysb[:], scalar=sm[:, 10:11],
                                                                 in1=ogb[:, head * 128:(head + 1) * 128], op0=ALU.mult, op1=ALU.mult),
                         r=["ysb_%d" % c, "sm_%d" % c, "ogb"], w=["ynb_%d" % c])
                    yield
                    tcol = yb * 1024 + 512
                    S.op("pe", lambda e: e.transpose(out=psBb[:, tcol:tcol + 128], in_=ynb[:], identity=identb[:]),
                         r=["ynb_%d" % c, "identb"], w=["B%d" % yb])
                    yield
                    S.op("act", lambda e: e.copy(out=dst[:, qi * 128:(qi + 1) * 128], in_=psBb[:, tcol:tcol + 128]),
                         r=["B%d" % yb], w=[dkey])
                    yield

                def softmax_tail(c, W):
                    sm, f1 = SM[c], F1[c]
                    S.op("dve", lambda e: e.reduce_max(out=sm[:, 0:1], in_=f1[:, 0:W], axis=AX.X), r=["f1_%d" % c], w=["sm_%d" % c])
                    S.op("dve", lambda e: e.tensor_scalar(out=sm[:, 2:3], in0=sm[:, 0:1], scalar1=-1.0, scalar2=None, op0=ALU.mult),
                         r=["sm_%d" % c], w=["sm_%d" % c])
                    yield
                    S.op("act", lambda e: e.activation(out=PB[c][:, 0:W], in_=f1[:, 0:W], func=AF.Exp, bias=sm[:, 2:3],
                                                       accum_out=sm[:, 1:2]), r=["f1_%d" % c, "sm_%d" % c], w=["pb_%d" % c, "sm_%d" % c])
                    yield
                    S.op("dve", lambda e: e.reciprocal(out=sm[:, 3:4], in_=sm[:, 1:2]), r=["sm_%d" % c], w=["sm_%d" % c])
                    yield

                def sb_chain(c, slot, h, qi):
                    W = (qi + 1) * 128
                    f1, f2, f3, sm, pbc = F1[c], F2[c], F3[c], SM[c], PB[c]
                    z, zk = scores(c, qT[slot][:, qi * 128:(qi + 1) * 128], "qT%d" % slot, kT[slot], "kT%d" % slot, W)
                    yield
                    S.op("act", lambda e: e.activation(out=f1[:, 0:W], in_=z, func=AF.Exp, scale=ATT_SCALE), r=zk, w=["f1_%d" % c])
                    S.op("act", lambda e: e.activation(out=f1[:, 0:W], in_=f1[:, 0:W], func=AF.Ln, bias=1.0), r=["f1_%d" % c], w=["f1_%d" % c])
                    yield
                    S.op("pool", lambda e: e.affine_select(out=f1[:, W - 128:W], in_=f1[:, W - 128:W], pattern=[[-1, 128]],
                                                           base=0, channel_multiplier=1, compare_op=ALU.is_gt, fill=0.0),
                         r=["f1_%d" % c], w=["f1_%d" % c])
                    yield
                    S.op("dve", lambda e: e.tensor_tensor_scan(out=f2[:, 1:W + 1], data0=f1[:, 0:W], data1=f1[:, 0:W],
                                                               initial=0.0, op0=ALU.add, op1=ALU.bypass), r=["f1_%d" % c], w=["f2_%d" % c])
                    S.op("dve", lambda e: e.tensor_scalar(out=sm[:, 4:5], in0=f2[:, W:W + 1], scalar1=-1.0, scalar2=None, op0=ALU.mult),
                         r=["f2_%d" % c], w=["sm_%d" % c])
                    S.op("dve", lambda e: e.scalar_tensor_tensor(out=f3[:, 0:W], in0=z, scalar=ATT_SCALE, in1=f2[:, 0:W],
                                                                 op0=ALU.mult, op1=ALU.add), r=zk + ["f2_%d" % c], w=["f3_%d" % c])
                    yield "zfree"
                    S.op("act", lambda e: e.activation(out=pbc[:, 0:W], in_=f3[:, 0:W], func=AF.Exp, bias=sm[:, 4:5]),
                         r=["f3_%d" % c, "sm_%d" % c], w=["pb_%d" % c])
                    yield
                    S.op("pool", lambda e: e.affine_select(out=pbc[:, W - 128:W], in_=pbc[:, W - 128:W], pattern=[[-1, 128]],
                                                           base=0, channel_multiplier=1, compare_op=ALU.is_gt, fill=0.0),
                         r=["pb_%d" % c], w=["pb_%d" % c])
                    yield
                    yield from pv_finish(c, vv[slot], "vv%d" % slot, qi, W, 6 + h, ynst[slot], "ynst%d_%d" % (slot, c), None)

                def moba_chain(c, slot, h, qi, kmb):
                    W = (qi + 1) * 128
                    own = qi // 2
                    nk = W // 128
                    f1, sm, gs8, m8, pen16 = F1[c], SM[c], GS8[c], M8[c], PEN16[c]
                    z, zk = scores(c, qT[slot][:, qi * 128:(qi + 1) * 128], "qT%d" % slot, kT[slot], "kT%d" % slot, W)
                    yield
                    if own > 3:
                        yb = 2 + c
                        gps = psB[:, yb * 512 + 256:yb * 512 + 264]
                        S.op("pe", lambda e: e.matmul(gps, lhsT=qT[slot][:, qi * 128:(qi + 1) * 128], rhs=kmb[:], start=True, stop=True),
                             r=["qT%d" % slot, "kmb"], w=["B%d" % yb])
                        S.op("dve", lambda e: e.memset(gs8[:], NEG), w=["gs8_%d" % c])
                        yield
                        S.op("dve", lambda e: e.tensor_copy(out=gs8[:, 0:own], in_=gps[:, 0:own]), r=["B%d" % yb], w=["gs8_%d" % c])
                        S.op("dve", lambda e: e.max(out=m8[:], in_=gs8[:]), r=["gs8_%d" % c], w=["m8_%d" % c])
                        S.op("dve", lambda e: e.memset(pen16[:], 0.0), w=["pen16_%d" % c])
                        S.op("dve", lambda e: e.tensor_scalar(
                            out=pen16[:, 0:2 * own].rearrange("p (n two) -> p n two", two=2),
                            in0=gs8[:, 0:own].unsqueeze(2).to_broadcast([128, own, 2]),
                            scalar1=m8[:, 2:3], scalar2=NEG, op0=ALU.is_lt, op1=ALU.mult),
                            r=["gs8_%d" % c, "m8_%d" % c], w=["pen16_%d" % c])
                    else:
                        S.op("dve", lambda e: e.memset(pen16[:], 0.0), w=["pen16_%d" % c])
                    yield
                    S.op("dve", lambda e: e.scalar_tensor_tensor(
                        out=f1[:, 0:W].rearrange("p (n s) -> p n s", s=128),
                        in0=z.rearrange("p (n s) -> p n s", s=128), scalar=ATT_SCALE,
                        in1=pen16[:, 0:nk].unsqueeze(2).to_broadcast([128, nk, 128]),
                        op0=ALU.mult, op1=ALU.add), r=zk + ["pen16_%d" % c], w=["f1_%d" % c])
                    yield "zfree"
                    S.op("pool", lambda e: e.affine_select(out=f1[:, W - 128:W], in_=f1[:, W - 128:W], pattern=[[-1, 128]],
                                                           base=0, channel_multiplier=1, compare_op=ALU.is_ge, fill=negreg),
                         r=["f1_%d" % c], w=["f1_%d" % c])
                    yield
                    yield from softmax_tail(c, W)
                    yield from pv_finish(c, vv[slot], "vv%d" % slot, qi, W, 11 + h, ynst[slot], "ynst%d_%d" % (slot, c), sm[:, 3:4])

                hcount = [0]

                def next_slot():
                    s_ = hcount[0] % 2
                    hcount[0] += 1
                    return s_

                for h in range(5):
                    slot = next_slot()
                    load_head(slot, qbT[h], kbT[h], 128 + h * 128)
                    for qi in range(0, NTT, 2):
                        run_chains([sb_chain(0, slot, h, qi), sb_chain(1, slot, h, qi + 1)], disjoint=(qi + 2) * 128 <= 1024)
                    S.dma("sp", ynT[6 + h], ynst[slot][:], r=["ynst%d_0" % slot, "ynst%d_1" % slot], w=["D:ynT"])
                if stop == "attnB":
                    return

                for h in range(5):
                    slot = next_slot()
                    load_head(slot, qcT[h], kcT[h], 768 + h * 128)
                    S.op("dve", lambda e, slot=slot: e.tensor_reduce(out=GS8[0][:], in_=kT[slot][:].rearrange("p (n s) -> p n s", s=256),
                                                                     op=ALU.add, axis=AX.X), r=["kT%d" % slot], w=["gs8_0"])
                    kmb = sb(ph, "kmb%d" % h, [128, 8], BF16)
                    S.op("dve", lambda e, kmb=kmb: e.tensor_scalar(out=kmb[:], in0=GS8[0][:], scalar1=1.0 / 256, scalar2=None, op0=ALU.mult),
                         r=["gs8_0"], w=["kmb"])
                    for qi in range(0, NTT, 2):
                        run_chains([moba_chain(0, slot, h, qi, kmb), moba_chain(1, slot, h, qi + 1, kmb)], disjoint=(qi + 2) * 128 <= 1024)
                    S.dma("sp", ynT[11 + h], ynst[slot][:], r=["ynst%d_0" % slot, "ynst%d_1" % slot], w=["D:ynT"])
                if stop == "attnC":
                    return

                iq = sb(ph, "iq", [128, 8, SEQ], BF16)
                ik2 = sb(ph, "ik2", [128, SEQ], BF16)
                iws = sb(ph, "iws", [128, NTT, 16])
                qa = sb(ph, "qa", [128, 6, SEQ], BF16)
                pen = sb(ph, "pen", [128, SEQ])
                ynsta = sb(ph, "ynsta", [128, 6, SEQ], BF16)
                f2, f3, m8 = F2[0], F3[0], M8[0]
                for c_ in range(8):
                    S.dma("sp" if c_ % 2 else "act", iq[:, c_, :], iqT[c_], r=["D:qk"], w=["iq"])
                for hh in range(6):
                    S.dma("sp" if hh % 2 else "act", qa[:, hh, :], qaT[hh], r=["D:qk"], w=["qa"])
                S.dma("sp", ik2[:], ikT, r=["D:qk"], w=["ik2"])
                S.dma("sp", iws[:], iw_d.rearrange("(tt p) h -> p tt h", p=128), r=["D:iw"], w=["iws"])
                S.dma("act", kT[0][:], kaT, r=["D:qk"], w=["kT0"])
                S.dma("sp", vv[0][:], vtok[:, 0:128].rearrange("(st p) n -> p st n", p=128), r=["D:vtok"], w=["vv0"])

                def dsa_chain(c, hh, qi, W):
                    z, zk = scores(c, qa[:, hh, qi * 128:(qi + 1) * 128], "qa", kT[0], "kT0", W)
                    yield
                    S.op("dve", lambda e: e.scalar_tensor_tensor(out=F1[c][:, 0:W], in0=z, scalar=ATT_SCALE, in1=pen[:, 0:W],
                                                                 op0=ALU.mult, op1=ALU.add), r=zk + ["pen"], w=["f1_%d" % c])
                    yield "zfree"
                    yield from softmax_tail(c, W)
                    yield from pv_finish(c, vv[0], "vv0", qi, W, hh, ynsta[:, hh, :], "ynsta%d" % hh, SM[c][:, 3:4])

                for qi in range(NTT):
                    W = (qi + 1) * 128
                    nch = (W + 511) // 512
                    for hh in range(16):
                        c_, half = hh // 2, hh % 2
                        r0 = 64 * half
                        for ch in range(nch):
                            wc = min(512, W - ch * 512)
                            S.op("pe", lambda e, c_=c_, r0=r0, ch=ch, wc=wc, qi=qi: e.matmul(
                                psB[:, ch * 512:ch * 512 + wc], lhsT=iq[r0:r0 + 64, c_, qi * 128:(qi + 1) * 128],
                                rhs=ik2[r0:r0 + 64, ch * 512:ch * 512 + wc], start=True, stop=True),
                                r=["iq", "ik2"], w=["B%d" % ch])
                        bks = ["B%d" % ch for ch in range(nch)]
                        S.op("act", lambda e, W=W: e.activation(out=f3[:, 0:W], in_=psB[:, 0:W], func=AF.Relu), r=bks, w=["f3_0"])
                        if hh == 0:
                            S.op("dve", lambda e, W=W, qi=qi: e.tensor_scalar(out=f2[:, 0:W], in0=f3[:, 0:W], scalar1=iws[:, qi, 0:1],
                                                                              scalar2=None, op0=ALU.mult), r=["f3_0", "iws"], w=["f2_0"])
                        else:
                            S.op("dve", lambda e, W=W, qi=qi, hh=hh: e.scalar_tensor_tensor(
                                out=f2[:, 0:W], in0=f3[:, 0:W], scalar=iws[:, qi, hh:hh + 1], in1=f2[:, 0:W],
                                op0=ALU.mult, op1=ALU.add), r=["f3_0", "iws", "f2_0"], w=["f2_0"])
                    S.op("pool", lambda e, W=W: e.affine_select(out=f2[:, W - 128:W], in_=f2[:, W - 128:W], pattern=[[-1, 128]],
                                                                base=0, channel_multiplier=1, compare_op=ALU.is_ge, fill=negreg),
                         r=["f2_0"], w=["f2_0"])
                    if W > DSA_TOPK:
                        cur = f2
                        curk = "f2_0"
                        for rnd in range(DSA_TOPK // 8):
                            S.op("dve", lambda e, cur=cur, W=W: e.max(out=m8[:], in_=cur[:, 0:W]), r=[curk], w=["m8_0"])
                            if rnd < DSA_TOPK // 8 - 1:
                                S.op("dve", lambda e, cur=cur, W=W: e.match_replace(out=f3[:, 0:W], in_to_replace=m8[:],
                                                                                    in_values=cur[:, 0:W], imm_value=NEG),
                                     r=[curk, "m8_0"], w=["f3_0"])
                                cur = f3
                                curk = "f3_0"
                        S.op("dve", lambda e, W=W: e.tensor_scalar(out=pen[:, 0:W], in0=f2[:, 0:W], scalar1=m8[:, 7:8], scalar2=NEG,
                                                                   op0=ALU.is_lt, op1=ALU.mult), r=["f2_0", "m8_0"], w=["pen"])
                    else:
                        S.op("dve", lambda e, W=W: e.tensor_scalar(out=pen[:, 0:W], in0=f2[:, 0:W], scalar1=-1.0e29, scalar2=NEG,
                                                                   op0=ALU.is_lt, op1=ALU.mult), r=["f2_0"], w=["pen"])
                    for hp in range(0, 6, 2):
                        run_chains([dsa_chain(0, hp, qi, W), dsa_chain(1, hp + 1, qi, W)], disjoint=W <= 1024)
                for hh in range(6):
                    S.dma("sp", ynT[hh], ynsta[:, hh, :], r=["ynsta%d" % hh], w=["D:ynT"])

        def outproj_phase(l):
            S.barrier()
            with contextlib.ExitStack() as ph:
                yT = sb(ph, "yT", [128, 16, SEQ], BF16)
                wo = sb(ph, "wo", [128, 16, D], BF16)
                g1b = sb(ph, "g1b", [128, D])
                xt = [sb(ph, "ox%d" % i, [128, D]) for i in range(2)]
                tmp = sb(ph, "otmp", [128, D])
                wol = w_out[l].rearrange("(hc p) d -> p hc d", p=128)
                for q in range(4):
                    S.dma("sp" if q % 2 else "act", yT[:, q * 4:(q + 1) * 4, :], ynT[q * 4:(q + 1) * 4].rearrange("h p t -> p h t"),
                          r=["D:ynT"], w=["yT%d" % q])
                    S.dma("pool", wo[:, q * 4:(q + 1) * 4, :], wol[:, q * 4:(q + 1) * 4, :], w=["wo%d" % q])
                S.dma("sp", g1b[:], gate_row(l, 0).partition_broadcast(128), r=["D:modrow"], w=["g1b"])
                xsrc = x_in if l == 0 else xs
                for tt in range(NTT):
                    xb = xt[tt % 2]
                    xk = "ox%d" % (tt % 2)
                    S.dma("act", xb[:], xsrc[tt * 128:(tt + 1) * 128, :], r=["D:xs"], w=[xk])
                    for dq in range(4):
                        for hc in range(16):
                            S.op("pe", lambda e, tt=tt, dq=dq, hc=hc: e.matmul(
                                bankA(dq), lhsT=yT[:, hc, tt * 128:(tt + 1) * 128], rhs=wo[:, hc, dq * 512:(dq + 1) * 512],
                                start=(hc == 0), stop=(hc == 15)), r=["yT%d" % (hc // 4), "wo%d" % (hc // 4)], w=["A%d" % dq])
                    S.op("dve", lambda e: e.tensor_tensor(out=tmp[:], in0=psA[:, 0:D], in1=g1b[:], op=ALU.mult),
                         r=["A0", "A1", "A2", "A3", "g1b"], w=["otmp"])
                    S.op("pool", lambda e, xb=xb: e.tensor_tensor(out=xb[:], in0=xb[:], in1=tmp[:], op=ALU.add),
                         r=[xk, "otmp"], w=[xk])
                    S.dma("sp", xs[tt * 128:(tt + 1) * 128, :], xb[:], r=[xk], w=["D:xs"])

        def peerA_phase(l):
            S.barrier()
            with contextlib.ExitStack() as ph:
                hT = sb(ph, "phT", [128, 16, SEQ], BF16)
                wqb = sb(ph, "wqb", [128, 16, D], BF16)
                skb = sb(ph, "skb", [128, 16, 128], BF16)
                qTg = [sb(ph, "qTg%d" % i, [128, SEQ], BF16) for i in range(2)]
                sst = [sb(ph, "sst%d" % i, [128, NTT, 128]) for i in range(2)]
                wql = wq[l].rearrange("(dc p) n -> p dc n", p=128)
                for q in range(4):
                    S.dma("sp" if q % 2 else "act", hT[:, q * 4:(q + 1) * 4, :], hT_d[:, q * 4:(q + 1) * 4, :], r=["D:hT"], w=["phT%d" % q])
                    S.dma("pool", wqb[:, q * 4:(q + 1) * 4, :], wql[:, q * 4:(q + 1) * 4, :], w=["wqb%d" % q])
                S.dma("pool", skb[:], skT[l].rearrange("g e n -> e g n"), w=["skb"])
                ps_v = ps_d.rearrange("(tt p) (g n) -> p tt g n", p=128, n=128)
                for g in range(16):
                    qg = qTg[g % 2]
                    qk = "qTg%d" % (g % 2)
                    ss_ = sst[g % 2]
                    ssk = "sst%d" % (g % 2)
                    for tq in range(4):
                        ba = tq % 2
                        for dc in range(16):
                            S.op("pe", lambda e, g=g, tq=tq, dc=dc, ba=ba: e.matmul(
                                bankA(ba), lhsT=wqb[:, dc, g * 128:(g + 1) * 128], rhs=hT[:, dc, tq * 512:(tq + 1) * 512],
                                start=(dc == 0), stop=(dc == 15)), r=["wqb%d" % (dc // 4), "phT%d" % (dc // 4)], w=["A%d" % ba])
                        S.op("act", lambda e, qg=qg, tq=tq, ba=ba: e.copy(out=qg[:, tq * 512:(tq + 1) * 512], in_=bankA(ba)),
                             r=["A%d" % ba], w=[qk])
                    for tt in range(NTT):
                        S.op("pe", lambda e, qg=qg, g=g, tt=tt: e.matmul(
                            psB[:, tt * 128:(tt + 1) * 128], lhsT=qg[:, tt * 128:(tt + 1) * 128], rhs=skb[:, g, :],
                            start=True, stop=True), r=[qk, "skb"], w=["B%d" % (tt // 4)])
                    S.op("dve", lambda e, ss_=ss_: e.tensor_copy(out=ss_[:].rearrange("p a b -> p (a b)"), in_=psB[:, 0:2048]),
                         r=["B0", "B1", "B2", "B3"], w=[ssk])
                    S.dma("sp", ps_v[:, :, g, :], ss_[:], r=[ssk], w=["D:ps"])
            S.barrier()
            with contextlib.ExitStack() as ph:
                sall = [sb(ph, "sall%d" % i, [128, 16, 128]) for i in range(2)]
                wk_ = sb(ph, "pwk", [128, 16, 128])
                sv = sb(ph, "psv", [128, 16, 16])
                cand = sb(ph, "pcand", [128, 8, 256])
                cw = sb(ph, "pcw", [128, 8, 256])
                c16 = sb(ph, "pc16", [128, 8, 16])
                e16 = sb(ph, "pe16", [128, 8, 16])
                zz = sb(ph, "pzz", [128, 8])
                stt = sb(ph, "pstt", [128, NTT, 16])
                s0r = [sb(ph, "ps0r%d" % i, [128, 128, 8]) for i in range(2)]
                for tt in range(NTT):
                    sa = sall[tt % 2]
                    sak = "sall%d" % (tt % 2)
                    S.dma("sp", sa[:], ps_d[tt * 128:(tt + 1) * 128, :].rearrange("p (g n) -> p g n", g=16), r=["D:ps"], w=[sak])
                    for g in range(16):
                        S.op("dve", lambda e, sa=sa, g=g: e.max(out=sv[:, g, 0:8], in_=sa[:, g, :]), r=[sak], w=["psv"])
                        S.op("dve", lambda e, sa=sa, g=g: e.match_replace(out=wk_[:, g, :], in_to_replace=sv[:, g, 0:8],
                                                                          in_values=sa[:, g, :], imm_value=NEG),
                             r=[sak, "psv"], w=["pwk"])
                        S.op("dve", lambda e, g=g: e.max(out=sv[:, g, 8:16], in_=wk_[:, g, :]), r=["pwk"], w=["psv"])
                    sv4 = sv[:].rearrange("p (pp c) k -> p pp c k", c=2)
                    S.op("dve", lambda e, sv4=sv4: e.tensor_tensor(
                        out=cand[:].rearrange("p a (k l) -> p a k l", l=16),
                        in0=sv4[:, :, 0, :].unsqueeze(3).to_broadcast([128, 8, 16, 16]),
                        in1=sv4[:, :, 1, :].unsqueeze(2).to_broadcast([128, 8, 16, 16]), op=ALU.add),
                        r=["psv"], w=["pcand"])
                    for p in range(8):
                        S.op("dve", lambda e, p=p: e.max(out=c16[:, p, 0:8], in_=cand[:, p, :]), r=["pcand"], w=["pc16"])
                        S.op("dve", lambda e, p=p: e.match_replace(out=cw[:, p, :], in_to_replace=c16[:, p, 0:8],
                                                                   in_values=cand[:, p, :], imm_value=NEG),
                             r=["pcand", "pc16"], w=["pcw"])
                        S.op("dve", lambda e, p=p: e.max(out=c16[:, p, 8:16], in_=cw[:, p, :]), r=["pcw"], w=["pc16"])
                    S.op("dve", lambda e: e.tensor_tensor(out=e16[:], in0=c16[:], in1=c16[:, :, 0:1].to_broadcast([128, 8, 16]),
                                                          op=ALU.subtract), r=["pc16"], w=["pe16"])
                    S.op("act", lambda e: e.activation(out=e16[:], in_=e16[:], func=AF.Exp), r=["pe16"], w=["pe16"])
                    S.op("dve", lambda e: e.tensor_reduce(out=zz[:], in_=e16[:], op=ALU.add, axis=AX.X), r=["pe16"], w=["pzz"])
                    S.op("act", lambda e: e.activation(out=zz[:], in_=zz[:], func=AF.Ln), r=["pzz"], w=["pzz"])
                    S.op("dve", lambda e, tt=tt: e.tensor_copy(out=stt[:, tt, 0:8], in_=c16[:, :, 15]), r=["pc16"], w=["pstt"])
                    S.op("dve", lambda e, tt=tt: e.tensor_tensor(out=stt[:, tt, 8:16], in0=zz[:], in1=c16[:, :, 0], op=ALU.add),
                         r=["pc16", "pzz"], w=["pstt"])
                    S.op("dve", lambda e, tt=tt: e.tensor_scalar(out=stt[:, tt, 8:16], in0=stt[:, tt, 8:16], scalar1=-1.0, scalar2=None,
                                                                 op0=ALU.mult), r=["pstt"], w=["pstt"])
                    s0 = s0r[tt % 2]
                    s0k = "ps0r%d" % (tt % 2)
                    S.op("pool", lambda e, sa=sa, s0=s0: e.tensor_copy(
                        out=s0[:], in_=sa[:].rearrange("p (pp c) n -> p c n pp", c=2)[:, 0]), r=[sak], w=[s0k])
                    S.dma("sp", s0r_d[tt * 128:(tt + 1) * 128, :, :], s0[:], r=[s0k], w=["D:s0r"])
                S.dma("sp", pst_d.rearrange("(tt p) k -> p tt k", p=128), stt[:], r=["pstt"], w=["D:pst"])

        def peerB_phase(l):
            S.barrier()
            NTG = SEQ // PEER_TG
            TPG = PEER_TG // 128
            NIG = 128 // PEER_IG
            NE2 = PEER_IG // PEER_EG
            with contextlib.ExitStack() as ph:
                g2b = sb(ph, "g2b", [128, D])
                S.dma("sp", g2b[:], gate_row(l, 1).partition_broadcast(128), r=["D:modrow"], w=["g2b"])
                hTg = sb(ph, "hTg", [128, 16, PEER_TG], BF16)
                s1g = sb(ph, "s1g", [128, TPG, 8, 128])
                s0gs = [sb(ph, "s0g%d" % i, [128, TPG, PEER_IG, 8]) for i in range(2)]
                stg_ = sb(ph, "pstg", [128, TPG, 16])
                outacc = sb(ph, "outacc", [128, TPG, D])
                WTs = [sb(ph, "WT%d" % i, [128, PEER_IG, PEER_TG], BF16) for i in range(2)]
                vbuf = [sb(ph, "pv%d" % i, [128, PEER_IG, 128]) for i in range(2)]
                ebuf = [sb(ph, "pe%d" % i, [128, PEER_IG, 128], BF16) for i in range(2)]
                w8 = sb(ph, "pw8", [128, 8, PEER_IG, 128], BF16)
                uTg = [sb(ph, "uTg%d" % i, [128, 16, PEER_EG * 128], BF16) for i in range(2)]
                vg = [sb(ph, "vg%d" % i, [128, PEER_EG, D], BF16) for i in range(2)]
                gl1 = sb(ph, "gl", [128, PEER_EG, PEER_TG])
                gl = [gl1, gl1]
                G = [sb(ph, "G%d" % i, [128, PEER_EG, PEER_TG], BF16) for i in range(2)]
                uTl = uT[l].rearrange("(dc p) e -> p dc e", p=128)
                pvl = pv[l].rearrange("(c p) d -> p c d", p=128)
                ps_v = ps_d.rearrange("(tt p) (pp c n) -> p tt pp c n", p=128, c=2, n=128)
                cnt = {"w": 0}

                def s0_load(tg, ig):
                    t0_ = tg * PEER_TG
                    S.dma("sp", s0gs[ig % 2][:], s0r_d[t0_:t0_ + PEER_TG, ig * PEER_IG:(ig + 1) * PEER_IG, :].rearrange("(tt p) i pp -> p tt i pp", p=128),
                          r=["D:s0r"], w=["s0g%d" % (ig % 2)])

                def wb_p(ig, tt, p):
                    s0g = s0gs[ig % 2]
                    b = cnt["w"] % 2
                    cnt["w"] += 1
                    vb, eb = vbuf[b], ebuf[b]
                    HP = PEER_IG // 2
                    S.op("pool", lambda e: e.tensor_tensor(
                        out=vb[:, 0:HP, :], in0=s1g[:, tt, p, :].unsqueeze(1).to_broadcast([128, HP, 128]),
                        in1=s0g[:, tt, 0:HP, p].unsqueeze(2).to_broadcast([128, HP, 128]), op=ALU.add),
                        r=["s1g", "s0g%d" % (ig % 2)], w=["pv%d" % b])
                    for i in range(HP, PEER_IG):
                        S.op("act", lambda e, i=i: e.activation(out=vb[:, i, :], in_=s1g[:, tt, p, :], func=AF.Identity,
                                                                bias=s0g[:, tt, i, p:p + 1]),
                             r=["s1g", "s0g%d" % (ig % 2)], w=["pva%d_%d" % (b, i)])
                    vkeys = ["pv%d" % b] + ["pva%d_%d" % (b, i) for i in range(HP, PEER_IG)]
                    S.op("act", lambda e: e.activation(
                        out=eb[:], in_=vb[:], func=AF.Exp, bias=stg_[:, tt, 8 + p:9 + p]),
                        r=vkeys + ["pstg"], w=["pe%d" % b])
                    S.op("dve", lambda e: e.scalar_tensor_tensor(
                        out=w8[:, p], in0=vb[:], scalar=stg_[:, tt, p:p + 1], in1=eb[:], op0=ALU.is_ge, op1=ALU.mult),
                        r=vkeys + ["pe%d" % b, "pstg"], w=["pw%d" % p])
                    for i in range(PEER_IG):
                        S.op("pe", lambda e, i=i: e.matmul(
                            psB[:, i * 128:(i + 1) * 128], lhsT=w8[:, p, i, :], rhs=identb[:],
                            start=(p == 0 and i % 4 == 0), stop=(p == 7)), r=["pw%d" % p, "identb"], w=["B%d" % (i // 4)])

                def wb_pe(ig, tt):
                    WT = WTs[ig % 2]
                    S.op("act", lambda e: e.copy(out=WT[:, :, tt * 128:(tt + 1) * 128],
                                                 in_=psB[:, 0:1024].rearrange("p (i t) -> p i t", t=128)),
                         r=["B0", "B1"], w=["WT%d_%d" % (ig % 2, tt)])

                def load_u(n):
                    ub = n % 2
                    S.dma("pool", uTg[ub][:], uTl[:, :, n * PEER_EG * 128:(n + 1) * PEER_EG * 128], w=["uTg%d" % ub])

                def load_v(n):
                    ub = n % 2
                    S.dma("pool", vg[ub][:], pvl[:, n * PEER_EG:(n + 1) * PEER_EG, :], w=["vg%d" % ub])

                def ao_act(n, ec):
                    ub = n % 2
                    for dc in range(16):
                        S.op("pe", lambda e, dc=dc: e.matmul(
                            psA[:, ec * 512:ec * 512 + PEER_TG], lhsT=uTg[ub][:, dc, ec * 128:(ec + 1) * 128], rhs=hTg[:, dc, :],
                            start=(dc == 0), stop=(dc == 15)), r=["uTg%d" % ub, "hTg"], w=["A%d" % ec])

                def ao_gelu(n):
                    for ec in range(PEER_EG):
                        S.op("act", lambda e, ec=ec: e.activation(out=gl1[:, ec, :], in_=psA[:, ec * 512:ec * 512 + PEER_TG], func=AF.Gelu),
                             r=["A%d" % ec], w=["gl_%d" % ec])

                def ao_gmult(n):
                    ub = n % 2
                    ig, eg2 = n // NE2, n % NE2
                    WT = WTs[ig % 2]
                    wkeys = ["WT%d_%d" % (ig % 2, tt) for tt in range(TPG)]
                    for ec in range(PEER_EG):
                        il = eg2 * PEER_EG + ec
                        S.op("dve", lambda e, ec=ec, il=il: e.tensor_tensor(
                            out=G[ub][:, ec, :], in0=gl1[:, ec, :], in1=WT[:, il, :], op=ALU.mult),
                            r=["gl_%d" % ec] + wkeys, w=["G%d" % ub])

                def ao_outacc(n, j):
                    ub = n % 2
                    tt, dq = j // 4, j % 4
                    bk = 2 + (j % 2)
                    for ec in range(PEER_EG):
                        S.op("pe", lambda e, ec=ec: e.matmul(
                            bankB(bk), lhsT=G[ub][:, ec, tt * 128:(tt + 1) * 128], rhs=vg[ub][:, ec, dq * 512:(dq + 1) * 512],
                            start=(ec == 0), stop=(ec == PEER_EG - 1)), r=["G%d" % ub, "vg%d" % ub], w=["B%d" % bk])
                    ok = "outacc%d_%d" % (tt, dq // 2)
                    dst = outacc[:, tt, dq * 512:(dq + 1) * 512]
                    if n == 0:
                        S.op("dve", lambda e: e.tensor_copy(out=dst, in_=bankB(bk)), r=["B%d" % bk], w=[ok])
                    else:
                        S.op("dve", lambda e: e.tensor_tensor(out=dst, in0=bankB(bk), in1=dst, op=ALU.add), r=["B%d" % bk, ok], w=[ok])

                NSTEP = NIG * NE2
                WPS = TPG // NE2
                NOUT = TPG * 4
                assert TPG % NE2 == 0 and PEER_EG == 4
                for tg in range(NTG):
                    t0 = tg * PEER_TG
                    S.dma("sp", hTg[:], hT_d[:, :, t0:t0 + PEER_TG], r=["D:hT"], w=["hTg"])
                    for tt in range(TPG):
                        S.dma("act", s1g[:, tt], ps_v[:, tg * TPG + tt, :, 1, :], r=["D:ps"], w=["s1g"])
                    S.dma("act", stg_[:], pst_d[t0:t0 + PEER_TG, :].rearrange("(tt p) k -> p tt k", p=128), r=["D:pst"], w=["pstg"])
                    s0_load(tg, 0)
                    s0_load(tg, 1)
                    load_u(0)
                    load_v(0)
                    for tt in range(TPG):
                        for p in range(8):
                            wb_p(0, tt, p)
                        wb_pe(0, tt)
                    for n in range(NSTEP + 1):
                        ig, k = n // NE2, n % NE2
                        if n < NSTEP:
                            if n + 1 < NSTEP:
                                load_u(n + 1)
                            if n >= 1:
                                load_v(n)
                        for wt in range(WPS):
                            if n < NSTEP:
                                for ec in range(wt * PEER_EG // WPS, (wt + 1) * PEER_EG // WPS):
                                    ao_act(n, ec)
                            if n < NSTEP and ig + 1 < NIG:
                                for p in range(8):
                                    wb_p(ig + 1, k * WPS + wt, p)
                                wb_pe(ig + 1, k * WPS + wt)
                            if n >= 1:
                                for sl in range(wt * NOUT // WPS, (wt + 1) * NOUT // WPS):
                                    ao_outacc(n - 1, sl)
                        if n < NSTEP:
                            ao_gelu(n)
                            if k == NE2 - 1 and ig + 2 < NIG:
                                s0_load(tg, ig + 2)
                            ao_gmult(n)
                    for tt in range(TPG):
                        r0 = t0 + tt * 128
                        for hf in range(2):
                            c0 = hf * 1024
                            xh = vbuf[0][:].rearrange("p a b -> p (a b)")
                            th = vbuf[1][:].rearrange("p a b -> p (a b)")
                            S.dma("sp", xh, xs[r0:r0 + 128, c0:c0 + 1024], r=["D:xs"], w=["pv0"] + ["pva0_%d" % i for i in range(PEER_IG // 2, PEER_IG)])
                            S.op("dve", lambda e, tt=tt, c0=c0, th=th: e.tensor_tensor(out=th, in0=outacc[:, tt, c0:c0 + 1024], in1=g2b[:, c0:c0 + 1024], op=ALU.mult),
                                 r=["outacc%d_%d" % (tt, hf), "g2b"], w=["pv1"] + ["pva1_%d" % i for i in range(PEER_IG // 2, PEER_IG)])
                            S.op("pool", lambda e, xh=xh, th=th: e.tensor_tensor(out=xh, in0=xh, in1=th, op=ALU.add), r=["pv0", "pv1"], w=["pv0"])
                            S.dma("sp", xs[r0:r0 + 128, c0:c0 + 1024], xh, r=["pv0"], w=["D:xs"])

        def final_phase():
            S.barrier()
            with contextlib.ExitStack() as ph:
                fgb = sb(ph, "fgb", [128, D])
                S.dma("sp", fgb[:], fg.partition_broadcast(128), w=["fgb"])
                xt = [sb(ph, "fx%d" % i, [128, D]) for i in range(2)]
                junk = sb(ph, "fjunk", [128, D])
                st = sb(ph, "fst", [128, 4])
                for tt in range(NTT):
                    xb = xt[tt % 2]
                    xk = "fx%d" % (tt % 2)
                    S.dma("sp" if tt % 2 else "act", xb[:], xs[tt * 128:(tt + 1) * 128, :], r=["D:xs"], w=[xk])
                    S.op("act", lambda e, xb=xb: e.activation(out=junk[:], in_=xb[:], func=AF.Square, accum_out=st[:, 0:1]),
                         r=[xk], w=["fjunk", "fst"])
                    S.op("dve", lambda e: e.tensor_scalar(out=st[:, 1:2], in0=st[:, 0:1], scalar1=1.0 / D, scalar2=EPS, op0=ALU.mult, op1=ALU.add),
                         r=["fst"], w=["fst"])
                    S.op("act", lambda e: e.activation(out=st[:, 1:2], in_=st[:, 1:2], func=AF.Sqrt), r=["fst"], w=["fst"])
                    S.op("dve", lambda e: e.reciprocal(out=st[:, 2:3], in_=st[:, 1:2]), r=["fst"], w=["fst"])
                    S.op("dve", lambda e, xb=xb: e.scalar_tensor_tensor(out=xb[:], in0=xb[:], scalar=st[:, 2:3], in1=fgb[:],
                                                                        op0=ALU.mult, op1=ALU.mult), r=[xk, "fst", "fgb"], w=[xk])
                    S.dma("sp", out_d[tt * 128:(tt + 1) * 128, :], xb[:], r=[xk], w=["D:out"])

        def run_layers():
            for l in range(nlayers):
                norm_phase("n1_%d" % l, x_in if l == 0 else xs, l, 0)
                if stop == "norm1":
                    return
                proj_phase(l)
                if stop == "proj":
                    return
                attn_phase(l)
                if stop in ("attnB", "attnC", "attn"):
                    return
                outproj_phase(l)
                if stop == "outproj":
                    return
                norm_phase("n2_%d" % l, xs, l, 1)
                peerA_phase(l)
                if stop == "peerA":
                    return
                peerB_phase(l)
                if stop == "peerB":
                    return
            final_phase()

        run_layers()
        S.finish("sp")
    return nc


def _rope_tables():
    def tab(hd):
        half = hd // 2
        inv = (10000.0 ** (-np.arange(half, dtype=np.float32) / half)).astype(np.float32)
        ang = np.arange(SEQ, dtype=np.float32)[:, None] * inv[None, :]
        cos = np.cos(ang).astype(np.float32).T
        sin = np.sin(ang).astype(np.float32).T
        cos_f = np.concatenate([cos, cos], axis=0)
        sin_s = np.concatenate([-sin, sin], axis=0)
        reps = 128 // hd
        out = np.stack([np.tile(cos_f, (reps, 1)), np.tile(sin_s, (reps, 1))], axis=1)
        return np.ascontiguousarray(out, dtype=np.float32)
    return tab(128), tab(64)


def prep_inputs(x, c, ada_w, ada_b, norm1_g, norm2_g, w_in, out_norm_g, w_out,
                peer_wq, peer_subkeys, peer_u, peer_v, final_g):
    f = lambda a: np.ascontiguousarray(np.asarray(a), dtype=np.float32)
    x, c = f(x), f(c)
    cs128, cs64 = _rope_tables()
    shared = {
        "ada_w": f(ada_w),
        "ada_bT": f(np.asarray(ada_b).reshape(DEPTH, 96, 128).transpose(2, 0, 1).reshape(128, DEPTH * 96)),
        "gT": f(np.stack([np.asarray(norm1_g).reshape(DEPTH, 16, 128), np.asarray(norm2_g).reshape(DEPTH, 16, 128)], axis=1)
                .transpose(3, 0, 1, 2).reshape(128, DEPTH * 32)),
        "w_in": f(w_in),
        "og": f(out_norm_g),
        "w_out": f(w_out),
        "peer_wq": f(peer_wq),
        "skT": f(np.asarray(peer_subkeys).reshape(DEPTH, 16, 128, 128).transpose(0, 1, 3, 2)),
        "uT": f(np.asarray(peer_u).transpose(0, 2, 1)),
        "pv": f(peer_v),
        "fg": f(np.asarray(final_g).reshape(1, D)),
        "ident": np.eye(128, dtype=np.float32),
        "cs128": cs128,
        "cs64": cs64,
    }
    maps = []
    for b in range(x.shape[0]):
        m = dict(shared)
        m["x"] = x[b]
        m["cT"] = f(c[b].reshape(16, 128).T)
        maps.append(m)
    return maps


def kernel(**inputs):
    maps = prep_inputs(**inputs)
    nc = build()
    res = run_bass_kernel_spmd(nc, maps, core_ids=list(range(len(maps))))
    return np.stack([r["out"] for r in res.results], axis=0).astype(np.float32)
```

```python
import contextlib
import numpy as np
import concourse.bass as bass
import concourse.mybir as mybir
from concourse.bass_utils import run_bass_kernel_spmd

F32 = mybir.dt.float32
BF16 = mybir.dt.bfloat16
AF = mybir.ActivationFunctionType
ALU = mybir.AluOpType
AX = mybir.AxisListType

SEQ = 2048
D = 2048
NTT = SEQ // 128
DEPTH = 2
HD = 128
EPS = 1e-6
NEG = -1.0e30
IN_WIDTH = 5968
C_QA, C_KA, C_VA, C_IQ, C_IK, C_IW = 0, 768, 896, 1024, 2048, 2112
C_QB, C_KB, C_VB, C_QC, C_KC, C_VC = 2128, 2768, 3408, 4048, 4688, 5328
ATT_SCALE = HD ** -0.5
IDX_SCALE = (64 ** -0.5) * (16 ** -0.5)
DSA_TOPK = 256
NKEYS = 128
PEER_TG = 512
PEER_EG = 4
PEER_IG = 8


class Sched:
    NDMA = 40
    NHW = 24

    def __init__(self, nc, es):
        self.nc = nc
        self.engs = {"pe": nc.tensor, "act": nc.scalar, "dve": nc.vector,
                     "pool": nc.gpsimd, "sp": nc.sync}
        self.sem = {k: es.enter_context(nc.semaphore("sem_" + k)) for k in self.engs}
        self.cnt = {k: 0 for k in self.engs}
        self.dsem = [es.enter_context(nc.semaphore("dsem%d" % i)) for i in range(self.NDMA)]
        self.dcnt = [0] * self.NDMA
        self.dnext = 0
        self.dnext_sw = 0
        self.known = {k: {} for k in self.engs}
        self.res = {}
        self.ninstr = 0

    def _semobj(self, key):
        return self.sem[key] if isinstance(key, str) else self.dsem[key]

    def _wait(self, eng, key, val):
        if val <= 0:
            return
        kn = self.known[eng]
        if kn.get(key, 0) >= val:
            return
        self.engs[eng].wait_ge(self._semobj(key), val)
        kn[key] = val

    def _deps(self, r, w):
        deps = {}

        def add(k, v):
            if deps.get(k, 0) < v:
                deps[k] = v
        for key in r:
            st = self.res.get(key)
            if st is not None and st["w"] is not None:
                add(*st["w"])
        for key in w:
            st = self.res.get(key)
            if st is not None:
                if st["w"] is not None:
                    add(*st["w"])
                for k, v in st["r"].items():
                    add(k, v)
        return deps

    def _commit(self, r, w, stamp):
        k, v = stamp
        for key in r:
            st = self.res.setdefault(key, {"w": None, "r": {}})
            if st["r"].get(k, 0) < v:
                st["r"][k] = v
        for key in w:
            self.res[key] = {"w": stamp, "r": {}}

    def op(self, eng, fn, r=(), w=()):
        for k, v in self._deps(r, w).items():
            if eng == "pe" and k == "pe":
                continue
            self._wait(eng, k, v)
        ins = fn(self.engs[eng])
        self.cnt[eng] += 1
        ins.then_inc(self.sem[eng], 1)
        self._commit(r, w, (eng, self.cnt[eng]))
        self.ninstr += 1
        return ins

    def dma(self, eng, out, in_, r=(), w=(), **kw):
        for k, v in self._deps(r, w).items():
            self._wait(eng, k, v)
        if eng == "pool":
            i = self.NHW + self.dnext_sw
            self.dnext_sw = (self.dnext_sw + 1) % (self.NDMA - self.NHW)
        else:
            i = self.dnext
            self.dnext = (self.dnext + 1) % self.NHW
        self._wait(eng, i, self.dcnt[i])
        ins = self.engs[eng].dma_start(out=out, in_=in_, **kw)
        self.dcnt[i] += 16
        ins.then_inc(self.dsem[i], 16)
        self._commit(r, w, (i, self.dcnt[i]))
        self.ninstr += 1
        return ins

    def barrier(self):
        for eng in self.engs:
            for k in self.engs:
                if k != eng:
                    self._wait(eng, k, self.cnt[k])
            for i in range(self.NDMA):
                self._wait(eng, i, self.dcnt[i])

    def finish(self, eng="sp"):
        for k in self.engs:
            self._wait(eng, k, self.cnt[k])
        for i in range(self.NDMA):
            self._wait(eng, i, self.dcnt[i])


def build(nlayers=DEPTH, taps=(), stop=None, peer_dummy=False):
    nc = bass.Bass("TRN2", target_bir_lowering=False)

    def din(name, shape, dt=F32):
        return nc.dram_tensor(name, shape, dt, kind="ExternalInput").ap()

    def dscr(name, shape, dt=F32):
        kind = "ExternalOutput" if name in taps else "Internal"
        return nc.dram_tensor(name, shape, dt, kind=kind).ap()

    x_in = din("x", [SEQ, D])
    cT = din("cT", [128, 16])
    ada_w = din("ada_w", [DEPTH, D, 6 * D])
    ada_bT = din("ada_bT", [128, DEPTH * 96])
    gT = din("gT", [128, DEPTH * 32])
    w_in = din("w_in", [DEPTH, D, IN_WIDTH])
    og = din("og", [DEPTH, D])
    w_out = din("w_out", [DEPTH, D, D])
    wq = din("peer_wq", [DEPTH, D, D])
    skT = din("skT", [DEPTH, 16, 128, 128])
    uT = din("uT", [1, 128, 128] if peer_dummy else [nlayers, D, NKEYS * NKEYS])
    pv = din("pv", [1, 128, 128] if peer_dummy else [nlayers, NKEYS * NKEYS, D])
    fg = din("fg", [1, D])
    ident_d = din("ident", [128, 128])
    cs128 = din("cs128", [128, 2, SEQ])
    cs64 = din("cs64", [128, 2, SEQ])
    out_d = nc.dram_tensor("out", [SEQ, D], F32, kind="ExternalOutput").ap()

    xs = dscr("xs", [SEQ, D])
    hT_d = dscr("hT", [128, 16, SEQ], BF16)
    modrow = dscr("modrow", [DEPTH * 96, 128])
    qaT = dscr("qaT", [6, 128, SEQ], BF16)
    kaT = dscr("kaT", [128, SEQ], BF16)
    iqT = dscr("iqT", [8, 128, SEQ], BF16)
    ikT = dscr("ikT", [128, SEQ], BF16)
    qbT = dscr("qbT", [5, 128, SEQ], BF16)
    kbT = dscr("kbT", [5, 128, SEQ], BF16)
    qcT = dscr("qcT", [5, 128, SEQ], BF16)
    kcT = dscr("kcT", [5, 128, SEQ], BF16)
    vtok = dscr("vtok", [SEQ, 1408], BF16)
    iw_d = dscr("iw", [SEQ, 16])
    ynT = dscr("ynT", [16, 128, SEQ], BF16)
    ps_d = dscr("peer_s", [SEQ, 2048])
    s0r_d = dscr("peer_s0r", [SEQ, 128, 8])
    pst_d = dscr("peer_st", [SEQ, 16])

    es = contextlib.ExitStack()
    with es:
        S = Sched(nc, es)

        sbn = [0]

        def sb(stack, name, shape, dt=F32):
            sbn[0] += 1
            return stack.enter_context(nc.sbuf_tensor("s%d_%s" % (sbn[0], name), shape, dt))

        psA = es.enter_context(nc.psum_tensor("psA", [128, 2048], F32))
        psB = es.enter_context(nc.psum_tensor("psB", [128, 2048], F32))

        def bankA(i):
            return psA[:, i * 512:(i + 1) * 512]

        def bankB(i):
            return psB[:, i * 512:(i + 1) * 512]

        negreg = nc.gpsimd.to_reg(NEG)
        ident = sb(es, "ident", [128, 128])
        identb = sb(es, "identb", [128, 128], BF16)
        modT = sb(es, "modT", [128, DEPTH * 96])
        scs = sb(es, "scs", [128, DEPTH * 32])
        gsb = sb(es, "gsb", [128, DEPTH * 32])
        S.dma("sp", ident[:], ident_d[:, :], w=["ident"])
        S.op("dve", lambda e: e.tensor_copy(out=identb[:], in_=ident[:]), r=["ident"], w=["identb"])
        S.dma("sp", gsb[:], gT[:, :], w=["gsb"])

        with contextlib.ExitStack() as ph:
            cT_sb = sb(ph, "cT_sb", [128, 16])
            cond = sb(ph, "cond", [128, 16])
            abT = sb(ph, "abT", [128, DEPTH * 96])
            wbuf = [sb(ph, "adaw%d" % i, [128, 16, 512]) for i in range(2)]
            S.dma("sp", cT_sb[:], cT[:, :], w=["cT"])
            S.dma("sp", abT[:], ada_bT[:, :], w=["abT"])
            S.op("act", lambda e: e.activation(out=cond[:], in_=cT_sb[:], func=AF.Silu), r=["cT"], w=["cond"])
            gi = 0
            for l in range(nlayers):
                awl = ada_w[l].rearrange("(kc p) n -> p kc n", p=128)
                for g in range(24):
                    wb = wbuf[gi % 2]
                    key = "adaw%d" % (gi % 2)
                    S.dma("sp" if gi % 2 == 0 else "act", wb[:], awl[:, :, g * 512:(g + 1) * 512], w=[key])
                    for j in range(4):
                        col = l * 96 + g * 4 + j
                        for kc in range(16):
                            S.op("pe", lambda e, wb=wb, j=j, kc=kc, col=col: e.matmul(
                                psA[:, col:col + 1], lhsT=wb[:, kc, j * 128:(j + 1) * 128],
                                rhs=cond[:, kc:kc + 1], start=(kc == 0), stop=(kc == 15)),
                                r=[key, "cond"], w=["A0"])
                    gi += 1
            ncol = nlayers * 96
            S.op("dve", lambda e: e.tensor_tensor(out=modT[:, 0:ncol], in0=psA[:, 0:ncol], in1=abT[:, 0:ncol], op=ALU.add),
                 r=["A0", "abT"], w=["modT"])
            for l in range(nlayers):
                for which in range(2):
                    src = modT[:, l * 96 + 16 + which * 48: l * 96 + 32 + which * 48]
                    dst = scs[:, l * 32 + which * 16: l * 32 + which * 16 + 16]
                    gsl = gsb[:, l * 32 + which * 16: l * 32 + which * 16 + 16]
                    S.op("dve", lambda e, src=src, dst=dst: e.tensor_scalar(out=dst, in0=src, scalar1=1.0, scalar2=None, op0=ALU.add),
                         r=["modT"], w=["scs"])
                    S.op("dve", lambda e, dst=dst, gsl=gsl: e.tensor_tensor(out=dst, in0=dst, in1=gsl, op=ALU.mult),
                         r=["scs", "gsb"], w=["scs"])
            mr = sb(ph, "mr", [128, 2, 128])
            S.op("pe", lambda e: e.transpose(out=psA[:, 512:640], in_=modT[:, 0:128], identity=ident[:]), r=["modT", "ident"], w=["A1"])
            S.op("dve", lambda e: e.tensor_copy(out=mr[:, 0, :], in_=psA[:, 512:640]), r=["A1"], w=["mr"])
            if ncol > 128:
                S.op("pe", lambda e: e.transpose(out=psA[0:64, 1024:1152], in_=modT[:, 128:192], identity=ident[:]), r=["modT", "ident"], w=["A2"])
                S.op("dve", lambda e: e.tensor_copy(out=mr[0:64, 1, :], in_=psA[0:64, 1024:1152]), r=["A2"], w=["mr"])
            S.dma("sp", modrow[0:min(ncol, 128), :], mr[0:min(ncol, 128), 0, :], r=["mr"], w=["D:modrow"])
            if ncol > 128:
                S.dma("sp", modrow[128:192, :], mr[0:64, 1, :], r=["mr"], w=["D:modrow"])

        def gate_row(l, which):
            r0 = l * 96 + 32 + which * 48
            return modrow[r0:r0 + 16, :].rearrange("(o a) b -> o (a b)", o=1)

        def norm_phase(tag, xsrc, l, which):
            sc = scs[:, l * 32 + which * 16: l * 32 + which * 16 + 16]
            shb = l * 96 + which * 48
            sh = modT[:, shb:shb + 16]
            S.barrier()
            with contextlib.ExitStack() as ph:
                xt = [sb(ph, "%sx%d" % (tag, i), [128, 4, D]) for i in range(2)]
                junk = sb(ph, tag + "junk", [128, D])
                ss = sb(ph, tag + "ss", [128, 4])
                rstd = sb(ph, tag + "rstd", [128, 4])
                hst = [sb(ph, "%shst%d" % (tag, i), [128, 16, 512], BF16) for i in range(2)]
                for tg in range(4):
                    xb = xt[tg % 2]
                    xk = "%sx%d" % (tag, tg % 2)
                    hk = "%shst%d" % (tag, tg % 2)
                    S.dma("sp", xb[:], xsrc[tg * 512:(tg + 1) * 512, :].rearrange("(i p) d -> p i d", p=128),
                          r=["D:xs"], w=[xk])
                    for i in range(4):
                        S.op("act", lambda e, xb=xb, i=i: e.activation(out=junk[:], in_=xb[:, i, :], func=AF.Square,
                                                                       accum_out=ss[:, i:i + 1]),
                             r=[xk], w=[tag + "junk", tag + "ss"])
                    S.op("dve", lambda e: e.tensor_scalar(out=rstd[:], in0=ss[:], scalar1=1.0 / D, scalar2=EPS,
                                                          op0=ALU.mult, op1=ALU.add), r=[tag + "ss"], w=[tag + "rstd"])
                    S.op("act", lambda e: e.activation(out=rstd[:], in_=rstd[:], func=AF.Sqrt), r=[tag + "rstd"], w=[tag + "rstd"])
                    S.op("dve", lambda e: e.reciprocal(out=rstd[:], in_=rstd[:]), r=[tag + "rstd"], w=[tag + "rstd"])
                    for i in range(4):
                        if i % 2:
                            S.op("act", lambda e, xb=xb, i=i: e.activation(out=xb[:, i, :], in_=xb[:, i, :], func=AF.Copy,
                                                                           scale=rstd[:, i:i + 1]), r=[xk, tag + "rstd"], w=[xk])
                        else:
                            S.op("dve", lambda e, xb=xb, i=i: e.tensor_scalar(
                                out=xb[:, i, :], in0=xb[:, i, :], scalar1=rstd[:, i:i + 1], scalar2=None, op0=ALU.mult),
                                r=[xk, tag + "rstd"], w=[xk])
                    hs = hst[tg % 2]
                    for dc in range(16):
                        bk = dc % 4
                        for i in range(4):
                            S.op("pe", lambda e, xb=xb, i=i, dc=dc, bk=bk: e.transpose(
                                out=psA[:, bk * 512 + i * 128: bk * 512 + (i + 1) * 128],
                                in_=xb[:, i, dc * 128:(dc + 1) * 128], identity=ident[:]),
                                r=[xk, "ident"], w=["A%d" % bk])
                        S.op("act", lambda e, hs=hs, dc=dc, bk=bk: e.activation(
                            out=hs[:, dc, :], in_=bankA(bk), func=AF.Identity,
                            scale=sc[:, dc:dc + 1], bias=sh[:, dc:dc + 1]),
                            r=["A%d" % bk, "scs", "modT"], w=[hk])
                    S.dma("sp", hT_d[:, :, tg * 512:(tg + 1) * 512], hs[:], r=[hk], w=["D:hT"])

        def proj_phase(l):
            wl = w_in[l].rearrange("(dc p) n -> p dc n", p=128)
            S.barrier()
            with contextlib.ExitStack() as ph:
                hT = sb(ph, "hT", [128, 16, SEQ], BF16)
                for q in range(4):
                    S.dma("sp" if q % 2 == 0 else "act", hT[:, q * 4:(q + 1) * 4, :], hT_d[:, q * 4:(q + 1) * 4, :],
                          r=["D:hT"], w=["hT%d" % q])
                hkeys = ["hT%d" % q for q in range(4)]
                c128 = sb(ph, "c128", [128, 2, SEQ])
                c64 = sb(ph, "c64", [128, 2, SEQ])
                S.dma("sp", c128[:], cs128[:, :, :], w=["c128"])
                S.dma("act", c64[:], cs64[:, :, :], w=["c64"])
                wts = [sb(ph, "wt%d" % i, [128, 16, 128], BF16) for i in range(2)]
                wss = [sb(ph, "ws%d" % i, [128, 16, 128], BF16) for i in range(2)]
                stg = [sb(ph, "stg%d" % i, [128, SEQ], BF16) for i in range(2)]
                t1 = sb(ph, "rt1", [128, 512])
                t2 = sb(ph, "rt2", [128, 512])

                chunks = []
                for h in range(6):
                    chunks.append((qaT[h], [(0, C_QA + h * 128, 128)], 128))
                chunks.append((kaT, [(0, C_KA, 128)], 128))
                for c in range(8):
                    chunks.append((iqT[c], [(0, C_IQ + c * 128, 128)], 64))
                chunks.append((ikT, [(0, C_IK, 64), (64, C_IK, 64)], 64))
                for h in range(5):
                    chunks.append((qbT[h], [(0, C_QB + h * 128, 128)], None))
                    chunks.append((kbT[h], [(0, C_KB + h * 128, 128)], None))
                for h in range(5):
                    chunks.append((qcT[h], [(0, C_QC + h * 128, 128)], 128))
                    chunks.append((kcT[h], [(0, C_KC + h * 128, 128)], 128))

                for ci, (dst, pieces, rope) in enumerate(chunks):
                    wt = wts[ci % 2]
                    ws = wss[ci % 2]
                    wk = "wt%d" % (ci % 2)
                    wsk = "ws%d" % (ci % 2)
                    st = stg[ci % 2]
                    sk = "stg%d" % (ci % 2)
                    for (dc0, sc0, wd) in pieces:
                        S.dma("pool", wt[:, :, dc0:dc0 + wd], wl[:, :, sc0:sc0 + wd], w=[wk])
                    if rope is not None:
                        half = rope // 2
                        for (dc0, sc0, wd) in pieces:
                            for b0 in range(0, wd, rope):
                                S.dma("pool", ws[:, :, dc0 + b0:dc0 + b0 + half],
                                      wl[:, :, sc0 + b0 + half:sc0 + b0 + rope], w=[wsk])
                                S.dma("pool", ws[:, :, dc0 + b0 + half:dc0 + b0 + rope],
                                      wl[:, :, sc0 + b0:sc0 + b0 + half], w=[wsk])
                    tab = c128 if rope == 128 else c64
                    tabk = "c128" if rope == 128 else "c64"
                    for tq in range(4):
                        ba = tq % 2
                        for dc in range(16):
                            S.op("pe", lambda e, wt=wt, dc=dc, tq=tq, ba=ba: e.matmul(
                                bankA(ba), lhsT=wt[:, dc, :], rhs=hT[:, dc, tq * 512:(tq + 1) * 512],
                                start=(dc == 0), stop=(dc == 15)), r=[wk, hkeys[dc // 4]], w=["A%d" % ba])
                        if rope is None:
                            S.op("act", lambda e, st=st, tq=tq, ba=ba: e.copy(out=st[:, tq * 512:(tq + 1) * 512], in_=bankA(ba)),
                                 r=["A%d" % ba], w=[sk])
                        else:
                            for dc in range(16):
                                S.op("pe", lambda e, ws=ws, dc=dc, tq=tq, ba=ba: e.matmul(
                                    bankB(ba), lhsT=ws[:, dc, :], rhs=hT[:, dc, tq * 512:(tq + 1) * 512],
                                    start=(dc == 0), stop=(dc == 15)), r=[wsk, hkeys[dc // 4]], w=["B%d" % ba])
                            S.op("dve", lambda e, tq=tq, ba=ba, tab=tab: e.tensor_tensor(
                                out=t1[:], in0=bankA(ba), in1=tab[:, 0, tq * 512:(tq + 1) * 512], op=ALU.mult),
                                r=["A%d" % ba, tabk], w=["rt1"])
                            S.op("dve", lambda e, tq=tq, ba=ba, tab=tab: e.tensor_tensor(
                                out=t2[:], in0=bankB(ba), in1=tab[:, 1, tq * 512:(tq + 1) * 512], op=ALU.mult),
                                r=["B%d" % ba, tabk], w=["rt2"])
                            S.op("pool", lambda e, st=st, tq=tq: e.tensor_tensor(
                                out=st[:, tq * 512:(tq + 1) * 512], in0=t1[:], in1=t2[:], op=ALU.add),
                                r=["rt1", "rt2"], w=[sk])
                    S.dma("sp", dst, st[:], r=[sk], w=["D:qk"])

                wv = sb(ph, "wv", [128, 16, 1424], BF16)
                for (dc0, sc0, wd) in [(0, C_VA, 128), (128, C_VB, 640), (768, C_VC, 640), (1408, C_IW, 16)]:
                    S.dma("pool", wv[:, :, dc0:dc0 + wd], wl[:, :, sc0:sc0 + wd], w=["wv"])
                vst = [sb(ph, "vst%d" % i, [128, 1408], BF16) for i in range(2)]
                iwst = sb(ph, "iwst", [128, NTT, 16])
                for tt in range(NTT):
                    vs = vst[tt % 2]
                    vk = "vst%d" % (tt % 2)
                    for nb, (n0, n1) in enumerate([(0, 512), (512, 1024), (1024, 1424)]):
                        for dc in range(16):
                            S.op("pe", lambda e, tt=tt, dc=dc, nb=nb, n0=n0, n1=n1: e.matmul(
                                psB[:, nb * 512: nb * 512 + (n1 - n0)], lhsT=hT[:, dc, tt * 128:(tt + 1) * 128],
                                rhs=wv[:, dc, n0:n1], start=(dc == 0), stop=(dc == 15)),
                                r=["wv", hkeys[dc // 4]], w=["B%d" % nb])
                    S.op("act", lambda e, vs=vs: e.copy(out=vs[:, 0:1024], in_=psB[:, 0:1024]), r=["B0", "B1"], w=[vk])
                    S.op("dve", lambda e, vs=vs: e.tensor_copy(out=vs[:, 1024:1408], in_=psB[:, 1024:1408]), r=["B2"], w=[vk])
                    S.op("dve", lambda e, tt=tt: e.tensor_scalar(out=iwst[:, tt, :], in0=psB[:, 1408:1424], scalar1=IDX_SCALE,
                                                                 scalar2=None, op0=ALU.mult), r=["B2"], w=["iwst"])
                    S.dma("sp", vtok[tt * 128:(tt + 1) * 128, :], vs[:], r=[vk], w=["D:vtok"])
                S.dma("sp", iw_d.rearrange("(tt p) h -> p tt h", p=128), iwst[:], r=["iwst"], w=["D:iw"])

        def run_chains(gens, disjoint=True):
            gens = list(gens)
            if not disjoint:
                for v in gens[0]:
                    if v == "zfree":
                        break
            while gens:
                for g in list(gens):
                    try:
                        next(g)
                    except StopIteration:
                        gens.remove(g)

        def attn_phase(l):
            S.barrier()
            with contextlib.ExitStack() as ph:
                ogb = sb(ph, "ogb", [128, D])
                S.dma("sp", ogb[:], og[l:l + 1, :].partition_broadcast(128), w=["ogb"])
                qT = [sb(ph, "qT%d" % i, [128, SEQ], BF16) for i in range(2)]
                kT = [sb(ph, "kT%d" % i, [128, SEQ], BF16) for i in range(2)]
                vv = [sb(ph, "vv%d" % i, [128, NTT, 128], BF16) for i in range(2)]
                ynst = [sb(ph, "ynst%d" % i, [128, SEQ], BF16) for i in range(2)]
                F1 = [sb(ph, "f1_%d" % c, [128, SEQ + 1]) for c in range(2)]
                F2 = [sb(ph, "f2_%d" % c, [128, SEQ + 1]) for c in range(2)]
                F3 = [sb(ph, "f3_%d" % c, [128, SEQ + 1]) for c in range(2)]
                PB = [sb(ph, "pb_%d" % c, [128, SEQ], BF16) for c in range(2)]
                AT = [sb(ph, "aT_%d" % c, [128, NTT, 128], BF16) for c in range(2)]
                SM = [sb(ph, "sm_%d" % c, [128, 32]) for c in range(2)]
                YSB = [sb(ph, "ysb_%d" % c, [128, 128]) for c in range(2)]
                YNB = [sb(ph, "ynb_%d" % c, [128, 128], BF16) for c in range(2)]
                JK = [sb(ph, "jk_%d" % c, [128, 128]) for c in range(2)]
                M8 = [sb(ph, "m8_%d" % c, [128, 8]) for c in range(2)]
                PEN16 = [sb(ph, "pen16_%d" % c, [128, 16]) for c in range(2)]
                GS8 = [sb(ph, "gs8_%d" % c, [128, 8]) for c in range(2)]
                for c in range(2):
                    S.op("dve", lambda e, c=c: e.memset(F2[c][:, 0:1], 0.0), w=["f2_%d" % c])
                psBb = psB[:].bitcast(BF16)

                def load_head(slot, qsrc, ksrc, vcol):
                    S.dma("sp", qT[slot][:], qsrc, r=["D:qk"], w=["qT%d" % slot])
                    S.dma("act", kT[slot][:], ksrc, r=["D:qk"], w=["kT%d" % slot])
                    S.dma("sp", vv[slot][:], vtok[:, vcol:vcol + 128].rearrange("(st p) n -> p st n", p=128),
                          r=["D:vtok"], w=["vv%d" % slot])

                def scores(c, qap, qkey, ksb, kkey, W):
                    base = c * 1024 if W <= 1024 else 0
                    keys = []
                    for ch in range((W + 511) // 512):
                        wc = min(512, W - ch * 512)
                        o = base + ch * 512
                        S.op("pe", lambda e, o=o, wc=wc, ch=ch: e.matmul(
                            psA[:, o:o + wc], lhsT=qap, rhs=ksb[:, ch * 512:ch * 512 + wc], start=True, stop=True),
                            r=[qkey, kkey], w=["A%d" % (o // 512)])
                        keys.append("A%d" % (o // 512))
                    return psA[:, base:base + W], keys

                def pv_finish(c, vsb, vkey, qi, W, head, dst, dkey, rinv):
                    nk = W // 128
                    sm, at, pbc = SM[c], AT[c], PB[c]
                    for g0 in range(0, nk, 8):
                        n = min(8, nk - g0)
                        for j in range(n):
                            S.op("pe", lambda e, j=j, g0=g0: e.transpose(
                                out=psBb[:, c * 1024 + j * 128:c * 1024 + (j + 1) * 128],
                                in_=pbc[:, (g0 + j) * 128:(g0 + j + 1) * 128], identity=identb[:]),
                                r=["pb_%d" % c, "identb"], w=["B%d" % c])
                        yield
                        eng = "act" if g0 == 0 else "dve"
                        if eng == "act":
                            S.op("act", lambda e, g0=g0, n=n: e.copy(out=at[:, g0:g0 + n, :].rearrange("p a b -> p (a b)"),
                                                                     in_=psBb[:, c * 1024:c * 1024 + n * 128]),
                                 r=["B%d" % c], w=["aT_%d" % c])
                        else:
                            S.op("dve", lambda e, g0=g0, n=n: e.tensor_copy(out=at[:, g0:g0 + n, :].rearrange("p a b -> p (a b)"),
                                                                            in_=psBb[:, c * 1024:c * 1024 + n * 128]),
                                 r=["B%d" % c], w=["aT_%d" % c])
                        yield
                    yb = 2 + c
                    yps = psB[:, yb * 512:yb * 512 + 128]
                    for sc in range(nk):
                        S.op("pe", lambda e, sc=sc: e.matmul(yps, lhsT=at[:, sc, :], rhs=vsb[:, sc, :],
                                                             start=(sc == 0), stop=(sc == nk - 1)),
                             r=["aT_%d" % c, vkey], w=["B%d" % yb])
                    yield
                    ysb, ynb, jk = YSB[c], YNB[c], JK[c]
                    if rinv is None:
                        S.op("act", lambda e: e.copy(out=ysb[:], in_=yps), r=["B%d" % yb], w=["ysb_%d" % c])
                    else:
                        S.op("dve", lambda e: e.tensor_scalar(out=ysb[:], in0=yps, scalar1=rinv, scalar2=None, op0=ALU.mult),
                             r=["B%d" % yb, "sm_%d" % c], w=["ysb_%d" % c])
                    yield
                    S.op("act", lambda e: e.activation(out=jk[:], in_=ysb[:], func=AF.Square, accum_out=sm[:, 8:9]),
                         r=["ysb_%d" % c], w=["jk_%d" % c, "sm_%d" % c])
                    yield
                    S.op("dve", lambda e: e.tensor_scalar(out=sm[:, 9:10], in0=sm[:, 8:9], scalar1=1.0 / HD, scalar2=EPS,
                                                          op0=ALU.mult, op1=ALU.add), r=["sm_%d" % c], w=["sm_%d" % c])
                    yield
                    S.op("act", lambda e: e.activation(out=sm[:, 9:10], in_=sm[:, 9:10], func=AF.Ln), r=["sm_%d" % c], w=["sm_%d" % c])
                    S.op("act", lambda e: e.activation(out=sm[:, 10:11], in_=sm[:, 9:10], func=AF.Exp, scale=-0.5),
                         r=["sm_%d" % c], w=["sm_%d" % c])
                    yield
                    S.op("dve", lambda e: e.scalar_tensor_tensor(out=ynb[:], in0=ysb[:], scalar=sm[:, 10:11],
                                                                 in1=ogb[:, head * 128:(head + 1) * 128], op0=ALU.mult, op1=ALU.mult),
                         r=["ysb_%d" % c, "sm_%d" % c, "ogb"], w=["ynb_%d" % c])
                    yield
                    tcol = yb * 1024 + 512
                    S.op("pe", lambda e: e.transpose(out=psBb[:, tcol:tcol + 128], in_=ynb[:], identity=identb[:]),
                         r=["ynb_%d" % c, "identb"], w=["B%d" % yb])
                    yield
                    S.op("act", lambda e: e.copy(out=dst[:, qi * 128:(qi + 1) * 128], in_=psBb[:, tcol:tcol + 128]),
                         r=["B%d" % yb], w=[dkey])
                    yield

                def softmax_tail(c, W):
                    sm, f1 = SM[c], F1[c]
                    S.op("dve", lambda e: e.reduce_max(out=sm[:, 0:1], in_=f1[:, 0:W], axis=AX.X), r=["f1_%d" % c], w=["sm_%d" % c])
                    S.op("dve", lambda e: e.tensor_scalar(out=sm[:, 2:3], in0=sm[:, 0:1], scalar1=-1.0, scalar2=None, op0=ALU.mult),
                         r=["sm_%d" % c], w=["sm_%d" % c])
                    yield
                    S.op("act", lambda e: e.activation(out=PB[c][:, 0:W], in_=f1[:, 0:W], func=AF.Exp, bias=sm[:, 2:3],
                                                       accum_out=sm[:, 1:2]), r=["f1_%d" % c, "sm_%d" % c], w=["pb_%d" % c, "sm_%d" % c])
                    yield
                    S.op("dve", lambda e: e.reciprocal(out=sm[:, 3:4], in_=sm[:, 1:2]), r=["sm_%d" % c], w=["sm_%d" % c])
                    yield

                def sb_chain(c, slot, h, qi):
                    W = (qi + 1) * 128
                    f1, f2, f3, sm, pbc = F1[c], F2[c], F3[c], SM[c], PB[c]
                    z, zk = scores(c, qT[slot][:, qi * 128:(qi + 1) * 128], "qT%d" % slot, kT[slot], "kT%d" % slot, W)
                    yield
                    S.op("act", lambda e: e.activation(out=f1[:, 0:W], in_=z, func=AF.Exp, scale=ATT_SCALE), r=zk, w=["f1_%d" % c])
                    S.op("act", lambda e: e.activation(out=f1[:, 0:W], in_=f1[:, 0:W], func=AF.Ln, bias=1.0), r=["f1_%d" % c], w=["f1_%d" % c])
                    yield
                    S.op("pool", lambda e: e.affine_select(out=f1[:, W - 128:W], in_=f1[:, W - 128:W], pattern=[[-1, 128]],
                                                           base=0, channel_multiplier=1, compare_op=ALU.is_gt, fill=0.0),
                         r=["f1_%d" % c], w=["f1_%d" % c])
                    yield
                    S.op("dve", lambda e: e.tensor_tensor_scan(out=f2[:, 1:W + 1], data0=f1[:, 0:W], data1=f1[:, 0:W],
                                                               initial=0.0, op0=ALU.add, op1=ALU.bypass), r=["f1_%d" % c], w=["f2_%d" % c])
                    S.op("dve", lambda e: e.tensor_scalar(out=sm[:, 4:5], in0=f2[:, W:W + 1], scalar1=-1.0, scalar2=None, op0=ALU.mult),
                         r=["f2_%d" % c], w=["sm_%d" % c])
                    S.op("dve", lambda e: e.scalar_tensor_tensor(out=f3[:, 0:W], in0=z, scalar=ATT_SCALE, in1=f2[:, 0:W],
                                                                 op0=ALU.mult, op1=ALU.add), r=zk + ["f2_%d" % c], w=["f3_%d" % c])
                    yield "zfree"
                    S.op("act", lambda e: e.activation(out=pbc[:, 0:W], in_=f3[:, 0:W], func=AF.Exp, bias=sm[:, 4:5]),
                         r=["f3_%d" % c, "sm_%d" % c], w=["pb_%d" % c])
                    yield
                    S.op("pool", lambda e: e.affine_select(out=pbc[:, W - 128:W], in_=pbc[:, W - 128:W], pattern=[[-1, 128]],
                                                           base=0, channel_multiplier=1, compare_op=ALU.is_gt, fill=0.0),
                         r=["pb_%d" % c], w=["pb_%d" % c])
                    yield
                    yield from pv_finish(c, vv[slot], "vv%d" % slot, qi, W, 6 + h, ynst[slot], "ynst%d_%d" % (slot, c), None)

                def moba_chain(c, slot, h, qi, kmb):
                    W = (qi + 1) * 128
                    own = qi // 2
                    nk = W // 128
                    f1, sm, gs8, m8, pen16 = F1[c], SM[c], GS8[c], M8[c], PEN16[c]
                    z, zk = scores(c, qT[slot][:, qi * 128:(qi + 1) * 128], "qT%d" % slot, kT[slot], "kT%d" % slot, W)
                    yield
                    if own > 3:
                        yb = 2 + c
                        gps = psB[:, yb * 512 + 256:yb * 512 + 264]
                        S.op("pe", lambda e: e.matmul(gps, lhsT=qT[slot][:, qi * 128:(qi + 1) * 128], rhs=kmb[:], start=True, stop=True),
                             r=["qT%d" % slot, "kmb"], w=["B%d" % yb])
                        S.op("dve", lambda e: e.memset(gs8[:], NEG), w=["gs8_%d" % c])
                        yield
                        S.op("dve", lambda e: e.tensor_copy(out=gs8[:, 0:own], in_=gps[:, 0:own]), r=["B%d" % yb], w=["gs8_%d" % c])
                        S.op("dve", lambda e: e.max(out=m8[:], in_=gs8[:]), r=["gs8_%d" % c], w=["m8_%d" % c])
                        S.op("dve", lambda e: e.memset(pen16[:], 0.0), w=["pen16_%d" % c])
                        S.op("dve", lambda e: e.tensor_scalar(
                            out=pen16[:, 0:2 * own].rearrange("p (n two) -> p n two", two=2),
                            in0=gs8[:, 0:own].unsqueeze(2).to_broadcast([128, own, 2]),
                            scalar1=m8[:, 2:3], scalar2=NEG, op0=ALU.is_lt, op1=ALU.mult),
                            r=["gs8_%d" % c, "m8_%d" % c], w=["pen16_%d" % c])
                    else:
                        S.op("dve", lambda e: e.memset(pen16[:], 0.0), w=["pen16_%d" % c])
                    yield
                    S.op("dve", lambda e: e.scalar_tensor_tensor(
                        out=f1[:, 0:W].rearrange("p (n s) -> p n s", s=128),
                        in0=z.rearrange("p (n s) -> p n s", s=128), scalar=ATT_SCALE,
                        in1=pen16[:, 0:nk].unsqueeze(2).to_broadcast([128, nk, 128]),
                        op0=ALU.mult, op1=ALU.add), r=zk + ["pen16_%d" % c], w=["f1_%d" % c])
                    yield "zfree"
                    S.op("pool", lambda e: e.affine_select(out=f1[:, W - 128:W], in_=f1[:, W - 128:W], pattern=[[-1, 128]],
                                                           base=0, channel_multiplier=1, compare_op=ALU.is_ge, fill=negreg),
                         r=["f1_%d" % c], w=["f1_%d" % c])
                    yield
                    yield from softmax_tail(c, W)
                    yield from pv_finish(c, vv[slot], "vv%d" % slot, qi, W, 11 + h, ynst[slot], "ynst%d_%d" % (slot, c), sm[:, 3:4])

                hcount = [0]

                def next_slot():
                    s_ = hcount[0] % 2
                    hcount[0] += 1
                    return s_

                for h in range(5):
                    slot = next_slot()
                    load_head(slot, qbT[h], kbT[h], 128 + h * 128)
                    for qi in range(0, NTT, 2):
                        run_chains([sb_chain(0, slot, h, qi), sb_chain(1, slot, h, qi + 1)], disjoint=(qi + 2) * 128 <= 1024)
                    S.dma("sp", ynT[6 + h], ynst[slot][:], r=["ynst%d_0" % slot, "ynst%d_1" % slot], w=["D:ynT"])
                if stop == "attnB":
                    return

                for h in range(5):
                    slot = next_slot()
                    load_head(slot, qcT[h], kcT[h], 768 + h * 128)
                    S.op("dve", lambda e, slot=slot: e.tensor_reduce(out=GS8[0][:], in_=kT[slot][:].rearrange("p (n s) -> p n s", s=256),
                                                                     op=ALU.add, axis=AX.X), r=["kT%d" % slot], w=["gs8_0"])
                    kmb = sb(ph, "kmb%d" % h, [128, 8], BF16)
                    S.op("dve", lambda e, kmb=kmb: e.tensor_scalar(out=kmb[:], in0=GS8[0][:], scalar1=1.0 / 256, scalar2=None, op0=ALU.mult),
                         r=["gs8_0"], w=["kmb"])
                    for qi in range(0, NTT, 2):
                        run_chains([moba_chain(0, slot, h, qi, kmb), moba_chain(1, slot, h, qi + 1, kmb)], disjoint=(qi + 2) * 128 <= 1024)
                    S.dma("sp", ynT[11 + h], ynst[slot][:], r=["ynst%d_0" % slot, "ynst%d_1" % slot], w=["D:ynT"])
                if stop == "attnC":
                    return

                iq = sb(ph, "iq", [128, 8, SEQ], BF16)
                ik2 = sb(ph, "ik2", [128, SEQ], BF16)
                iws = sb(ph, "iws", [128, NTT, 16])
                qa = sb(ph, "qa", [128, 6, SEQ], BF16)
                pen = sb(ph, "pen", [128, SEQ])
                ynsta = sb(ph, "ynsta", [128, 6, SEQ], BF16)
                f2, f3, m8 = F2[0], F3[0], M8[0]
                for c_ in range(8):
                    S.dma("sp" if c_ % 2 else "act", iq[:, c_, :], iqT[c_], r=["D:qk"], w=["iq"])
                for hh in range(6):
                    S.dma("sp" if hh % 2 else "act", qa[:, hh, :], qaT[hh], r=["D:qk"], w=["qa"])
                S.dma("sp", ik2[:], ikT, r=["D:qk"], w=["ik2"])
                S.dma("sp", iws[:], iw_d.rearrange("(tt p) h -> p tt h", p=128), r=["D:iw"], w=["iws"])
                S.dma("act", kT[0][:], kaT, r=["D:qk"], w=["kT0"])
                S.dma("sp", vv[0][:], vtok[:, 0:128].rearrange("(st p) n -> p st n", p=128), r=["D:vtok"], w=["vv0"])

                def dsa_chain(c, hh, qi, W):
                    z, zk = scores(c, qa[:, hh, qi * 128:(qi + 1) * 128], "qa", kT[0], "kT0", W)
                    yield
                    S.op("dve", lambda e: e.scalar_tensor_tensor(out=F1[c][:, 0:W], in0=z, scalar=ATT_SCALE, in1=pen[:, 0:W],
                                                                 op0=ALU.mult, op1=ALU.add), r=zk + ["pen"], w=["f1_%d" % c])
                    yield "zfree"
                    yield from softmax_tail(c, W)
                    yield from pv_finish(c, vv[0], "vv0", qi, W, hh, ynsta[:, hh, :], "ynsta%d" % hh, SM[c][:, 3:4])

                for qi in range(NTT):
                    W = (qi + 1) * 128
                    nch = (W + 511) // 512
                    for hh in range(16):
                        c_, half = hh // 2, hh % 2
                        r0 = 64 * half
                        for ch in range(nch):
                            wc = min(512, W - ch * 512)
                            S.op("pe", lambda e, c_=c_, r0=r0, ch=ch, wc=wc, qi=qi: e.matmul(
                                psB[:, ch * 512:ch * 512 + wc], lhsT=iq[r0:r0 + 64, c_, qi * 128:(qi + 1) * 128],
                                rhs=ik2[r0:r0 + 64, ch * 512:ch * 512 + wc], start=True, stop=True),
                                r=["iq", "ik2"], w=["B%d" % ch])
                        bks = ["B%d" % ch for ch in range(nch)]
                        S.op("act", lambda e, W=W: e.activation(out=f3[:, 0:W], in_=psB[:, 0:W], func=AF.Relu), r=bks, w=["f3_0"])
                        if hh == 0:
                            S.op("dve", lambda e, W=W, qi=qi: e.tensor_scalar(out=f2[:, 0:W], in0=f3[:, 0:W], scalar1=iws[:, qi, 0:1],
                                                                              scalar2=None, op0=ALU.mult), r=["f3_0", "iws"], w=["f2_0"])
                        else:
                            S.op("dve", lambda e, W=W, qi=qi, hh=hh: e.scalar_tensor_tensor(
                                out=f2[:, 0:W], in0=f3[:, 0:W], scalar=iws[:, qi, hh:hh + 1], in1=f2[:, 0:W],
                                op0=ALU.mult, op1=ALU.add), r=["f3_0", "iws", "f2_0"], w=["f2_0"])
                    S.op("pool", lambda e, W=W: e.affine_select(out=f2[:, W - 128:W], in_=f2[:, W - 128:W], pattern=[[-1, 128]],
                                                                base=0, channel_multiplier=1, compare_op=ALU.is_ge, fill=negreg),
                         r=["f2_0"], w=["f2_0"])
                    if W > DSA_TOPK:
                        cur = f2
                        curk = "f2_0"
                        for rnd in range(DSA_TOPK // 8):
                            S.op("dve", lambda e, cur=cur, W=W: e.max(out=m8[:], in_=cur[:, 0:W]), r=[curk], w=["m8_0"])
                            if rnd < DSA_TOPK // 8 - 1:
                                S.op("dve", lambda e, cur=cur, W=W: e.match_replace(out=f3[:, 0:W], in_to_replace=m8[:],
                                                                                    in_values=cur[:, 0:W], imm_value=NEG),
                                     r=[curk, "m8_0"], w=["f3_0"])
                                cur = f3
                                curk = "f3_0"
                        S.op("dve", lambda e, W=W: e.tensor_scalar(out=pen[:, 0:W], in0=f2[:, 0:W], scalar1=m8[:, 7:8], scalar2=NEG,
                                                                   op0=ALU.is_lt, op1=ALU.mult), r=["f2_0", "m8_0"], w=["pen"])
                    else:
                        S.op("dve", lambda e, W=W: e.tensor_scalar(out=pen[:, 0:W], in0=f2[:, 0:W], scalar1=-1.0e29, scalar2=NEG,
                                                                   op0=ALU.is_lt, op1=ALU.mult), r=["f2_0"], w=["pen"])
                    for hp in range(0, 6, 2):
                        run_chains([dsa_chain(0, hp, qi, W), dsa_chain(1, hp + 1, qi, W)], disjoint=W <= 1024)
                for hh in range(6):
                    S.dma("sp", ynT[hh], ynsta[:, hh, :], r=["ynsta%d" % hh], w=["D:ynT"])

        def outproj_phase(l):
            S.barrier()
            with contextlib.ExitStack() as ph:
                yT = sb(ph, "yT", [128, 16, SEQ], BF16)
                wo = sb(ph, "wo", [128, 16, D], BF16)
                g1b = sb(ph, "g1b", [128, D])
                xt = [sb(ph, "ox%d" % i, [128, D]) for i in range(2)]
                tmp = sb(ph, "otmp", [128, D])
                wol = w_out[l].rearrange("(hc p) d -> p hc d", p=128)
                for q in range(4):
                    S.dma("sp" if q % 2 else "act", yT[:, q * 4:(q + 1) * 4, :], ynT[q * 4:(q + 1) * 4].rearrange("h p t -> p h t"),
                          r=["D:ynT"], w=["yT%d" % q])
                    S.dma("pool", wo[:, q * 4:(q + 1) * 4, :], wol[:, q * 4:(q + 1) * 4, :], w=["wo%d" % q])
                S.dma("sp", g1b[:], gate_row(l, 0).partition_broadcast(128), r=["D:modrow"], w=["g1b"])
                xsrc = x_in if l == 0 else xs
                for tt in range(NTT):
                    xb = xt[tt % 2]
                    xk = "ox%d" % (tt % 2)
                    S.dma("act", xb[:], xsrc[tt * 128:(tt + 1) * 128, :], r=["D:xs"], w=[xk])
                    for dq in range(4):
                        for hc in range(16):
                            S.op("pe", lambda e, tt=tt, dq=dq, hc=hc: e.matmul(
                                bankA(dq), lhsT=yT[:, hc, tt * 128:(tt + 1) * 128], rhs=wo[:, hc, dq * 512:(dq + 1) * 512],
                                start=(hc == 0), stop=(hc == 15)), r=["yT%d" % (hc // 4), "wo%d" % (hc // 4)], w=["A%d" % dq])
                    S.op("dve", lambda e: e.tensor_tensor(out=tmp[:], in0=psA[:, 0:D], in1=g1b[:], op=ALU.mult),
                         r=["A0", "A1", "A2", "A3", "g1b"], w=["otmp"])
                    S.op("pool", lambda e, xb=xb: e.tensor_tensor(out=xb[:], in0=xb[:], in1=tmp[:], op=ALU.add),
                         r=[xk, "otmp"], w=[xk])
                    S.dma("sp", xs[tt * 128:(tt + 1) * 128, :], xb[:], r=[xk], w=["D:xs"])

        def peerA_phase(l):
            S.barrier()
            with contextlib.ExitStack() as ph:
                hT = sb(ph, "phT", [128, 16, SEQ], BF16)
                wqb = sb(ph, "wqb", [128, 16, D], BF16)
                skb = sb(ph, "skb", [128, 16, 128], BF16)
                qTg = [sb(ph, "qTg%d" % i, [128, SEQ], BF16) for i in range(2)]
                sst = [sb(ph, "sst%d" % i, [128, NTT, 128]) for i in range(2)]
                wql = wq[l].rearrange("(dc p) n -> p dc n", p=128)
                for q in range(4):
                    S.dma("sp" if q % 2 else "act", hT[:, q * 4:(q + 1) * 4, :], hT_d[:, q * 4:(q + 1) * 4, :], r=["D:hT"], w=["phT%d" % q])
                    S.dma("pool", wqb[:, q * 4:(q + 1) * 4, :], wql[:, q * 4:(q + 1) * 4, :], w=["wqb%d" % q])
                S.dma("pool", skb[:], skT[l].rearrange("g e n -> e g n"), w=["skb"])
                ps_v = ps_d.rearrange("(tt p) (g n) -> p tt g n", p=128, n=128)
                for g in range(16):
                    qg = qTg[g % 2]
                    qk = "qTg%d" % (g % 2)
                    ss_ = sst[g % 2]
                    ssk = "sst%d" % (g % 2)
                    for tq in range(4):
                        ba = tq % 2
                        for dc in range(16):
                            S.op("pe", lambda e, g=g, tq=tq, dc=dc, ba=ba: e.matmul(
                                bankA(ba), lhsT=wqb[:, dc, g * 128:(g + 1) * 128], rhs=hT[:, dc, tq * 512:(tq + 1) * 512],
                                start=(dc == 0), stop=(dc == 15)), r=["wqb%d" % (dc // 4), "phT%d" % (dc // 4)], w=["A%d" % ba])
                        S.op("act", lambda e, qg=qg, tq=tq, ba=ba: e.copy(out=qg[:, tq * 512:(tq + 1) * 512], in_=bankA(ba)),
                             r=["A%d" % ba], w=[qk])
                    for tt in range(NTT):
                        S.op("pe", lambda e, qg=qg, g=g, tt=tt: e.matmul(
                            psB[:, tt * 128:(tt + 1) * 128], lhsT=qg[:, tt * 128:(tt + 1) * 128], rhs=skb[:, g, :],
                            start=True, stop=True), r=[qk, "skb"], w=["B%d" % (tt // 4)])
                    S.op("dve", lambda e, ss_=ss_: e.tensor_copy(out=ss_[:].rearrange("p a b -> p (a b)"), in_=psB[:, 0:2048]),
                         r=["B0", "B1", "B2", "B3"], w=[ssk])
                    S.dma("sp", ps_v[:, :, g, :], ss_[:], r=[ssk], w=["D:ps"])
            S.barrier()
            with contextlib.ExitStack() as ph:
                sall = [sb(ph, "sall%d" % i, [128, 16, 128]) for i in range(2)]
                wk_ = sb(ph, "pwk", [128, 16, 128])
                sv = sb(ph, "psv", [128, 16, 16])
                cand = sb(ph, "pcand", [128, 8, 256])
                cw = sb(ph, "pcw", [128, 8, 256])
                c16 = sb(ph, "pc16", [128, 8, 16])
                e16 = sb(ph, "pe16", [128, 8, 16])
                zz = sb(ph, "pzz", [128, 8])
                stt = sb(ph, "pstt", [128, NTT, 16])
                s0r = [sb(ph, "ps0r%d" % i, [128, 128, 8]) for i in range(2)]
                for tt in range(NTT):
                    sa = sall[tt % 2]
                    sak = "sall%d" % (tt % 2)
                    S.dma("sp", sa[:], ps_d[tt * 128:(tt + 1) * 128, :].rearrange("p (g n) -> p g n", g=16), r=["D:ps"], w=[sak])
                    for g in range(16):
                        S.op("dve", lambda e, sa=sa, g=g: e.max(out=sv[:, g, 0:8], in_=sa[:, g, :]), r=[sak], w=["psv"])
                        S.op("dve", lambda e, sa=sa, g=g: e.match_replace(out=wk_[:, g, :], in_to_replace=sv[:, g, 0:8],
                                                                          in_values=sa[:, g, :], imm_value=NEG),
                             r=[sak, "psv"], w=["pwk"])
                        S.op("dve", lambda e, g=g: e.max(out=sv[:, g, 8:16], in_=wk_[:, g, :]), r=["pwk"], w=["psv"])
                    sv4 = sv[:].rearrange("p (pp c) k -> p pp c k", c=2)
                    S.op("dve", lambda e, sv4=sv4: e.tensor_tensor(
                        out=cand[:].rearrange("p a (k l) -> p a k l", l=16),
                        in0=sv4[:, :, 0, :].unsqueeze(3).to_broadcast([128, 8, 16, 16]),
                        in1=sv4[:, :, 1, :].unsqueeze(2).to_broadcast([128, 8, 16, 16]), op=ALU.add),
                        r=["psv"], w=["pcand"])
                    for p in range(8):
                        S.op("dve", lambda e, p=p: e.max(out=c16[:, p, 0:8], in_=cand[:, p, :]), r=["pcand"], w=["pc16"])
                        S.op("dve", lambda e, p=p: e.match_replace(out=cw[:, p, :], in_to_replace=c16[:, p, 0:8],
                                                                   in_values=cand[:, p, :], imm_value=NEG),
                             r=["pcand", "pc16"], w=["pcw"])
                        S.op("dve", lambda e, p=p: e.max(out=c16[:, p, 8:16], in_=cw[:, p, :]), r=["pcw"], w=["pc16"])
                    S.op("dve", lambda e: e.tensor_tensor(out=e16[:], in0=c16[:], in1=c16[:, :, 0:1].to_broadcast([128, 8, 16]),
                                                          op=ALU.subtract), r=["pc16"], w=["pe16"])
                    S.op("act", lambda e: e.activation(out=e16[:], in_=e16[:], func=AF.Exp), r=["pe16"], w=["pe16"])
                    S.op("dve", lambda e: e.tensor_reduce(out=zz[:], in_=e16[:], op=ALU.add, axis=AX.X), r=["pe16"], w=["pzz"])
                    S.op("act", lambda e: e.activation(out=zz[:], in_=zz[:], func=AF.Ln), r=["pzz"], w=["pzz"])
                    S.op("dve", lambda e, tt=tt: e.tensor_copy(out=stt[:, tt, 0:8], in_=c16[:, :, 15]), r=["pc16"], w=["pstt"])
                    S.op("dve", lambda e, tt=tt: e.tensor_tensor(out=stt[:, tt, 8:16], in0=zz[:], in1=c16[:, :, 0], op=ALU.add),
                         r=["pc16", "pzz"], w=["pstt"])
                    S.op("dve", lambda e, tt=tt: e.tensor_scalar(out=stt[:, tt, 8:16], in0=stt[:, tt, 8:16], scalar1=-1.0, scalar2=None,
                                                                 op0=ALU.mult), r=["pstt"], w=["pstt"])
                    s0 = s0r[tt % 2]
                    s0k = "ps0r%d" % (tt % 2)
                    S.op("pool", lambda e, sa=sa, s0=s0: e.tensor_copy(
                        out=s0[:], in_=sa[:].rearrange("p (pp c) n -> p c n pp", c=2)[:, 0]), r=[sak], w=[s0k])
                    S.dma("sp", s0r_d[tt * 128:(tt + 1) * 128, :, :], s0[:], r=[s0k], w=["D:s0r"])
                S.dma("sp", pst_d.rearrange("(tt p) k -> p tt k", p=128), stt[:], r=["pstt"], w=["D:pst"])

        def peerB_phase(l):
            S.barrier()
            NTG = SEQ // PEER_TG
            TPG = PEER_TG // 128
            NIG = 128 // PEER_IG
            NE2 = PEER_IG // PEER_EG
            with contextlib.ExitStack() as ph:
                hTg = sb(ph, "hTg", [128, 16, PEER_TG], BF16)
                s1g = sb(ph, "s1g", [128, TPG, 8, 128])
                s0gs = [sb(ph, "s0g%d" % i, [128, TPG, PEER_IG, 8]) for i in range(2)]
                stg_ = sb(ph, "pstg", [128, TPG, 16])
                outacc = sb(ph, "outacc", [128, TPG, D])
                WTs = [sb(ph, "WT%d" % i, [128, PEER_IG, PEER_TG], BF16) for i in range(2)]
                vbuf = [sb(ph, "pv%d" % i, [128, PEER_IG, 128]) for i in range(2)]
                ebuf = [sb(ph, "pe%d" % i, [128, PEER_IG, 128], BF16) for i in range(2)]
                w8s = [sb(ph, "pw8_%d" % i, [128, 8, PEER_IG, 128], BF16) for i in range(2)]
                uTg = [sb(ph, "uTg%d" % i, [128, 16, PEER_EG * 128], BF16) for i in range(2)]
                vg = [sb(ph, "vg%d" % i, [128, PEER_EG, D], BF16) for i in range(2)]
                gl1 = sb(ph, "gl", [128, PEER_EG, PEER_TG], BF16)
                gl = [gl1, gl1]
                G = [sb(ph, "G%d" % i, [128, PEER_EG, PEER_TG], BF16) for i in range(2)]
                uTl = uT[l].rearrange("(dc p) e -> p dc e", p=128)
                pvl = pv[l].rearrange("(c p) d -> p c d", p=128)
                ps_v = ps_d.rearrange("(tt p) (pp c n) -> p tt pp c n", p=128, c=2, n=128)
                cnt = {"w": 0}

                def s0_load(tg, ig):
                    t0_ = tg * PEER_TG
                    S.dma("sp", s0gs[ig % 2][:], s0r_d[t0_:t0_ + PEER_TG, ig * PEER_IG:(ig + 1) * PEER_IG, :].rearrange("(tt p) i pp -> p tt i pp", p=128),
                          r=["D:s0r"], w=["s0g%d" % (ig % 2)])

                def wb_p(ig, tt, p, wi):
                    s0g = s0gs[ig % 2]
                    w8 = w8s[wi]
                    b = cnt["w"] % 2
                    cnt["w"] += 1
                    vb, eb = vbuf[b], ebuf[b]
                    HP = PEER_IG // 2
                    S.op("pool", lambda e: e.tensor_tensor(
                        out=vb[:, 0:HP, :], in0=s1g[:, tt, p, :].unsqueeze(1).to_broadcast([128, HP, 128]),
                        in1=s0g[:, tt, 0:HP, p].unsqueeze(2).to_broadcast([128, HP, 128]), op=ALU.add),
                        r=["s1g", "s0g%d" % (ig % 2)], w=["pv%d" % b])
                    for i in range(HP, PEER_IG):
                        S.op("act", lambda e, i=i: e.activation(out=vb[:, i, :], in_=s1g[:, tt, p, :], func=AF.Identity,
                                                                bias=s0g[:, tt, i, p:p + 1]),
                             r=["s1g", "s0g%d" % (ig % 2)], w=["pva%d_%d" % (b, i)])
                    vkeys = ["pv%d" % b] + ["pva%d_%d" % (b, i) for i in range(HP, PEER_IG)]
                    S.op("act", lambda e: e.activation(
                        out=eb[:], in_=vb[:], func=AF.Exp, bias=stg_[:, tt, 8 + p:9 + p]),
                        r=vkeys + ["pstg"], w=["pe%d" % b])
                    S.op("dve", lambda e: e.scalar_tensor_tensor(
                        out=w8[:, p], in0=vb[:], scalar=stg_[:, tt, p:p + 1], in1=eb[:], op0=ALU.is_ge, op1=ALU.mult),
                        r=vkeys + ["pe%d" % b, "pstg"], w=["pw%d_%d" % (wi, p)])

                def wb_mm(wi):
                    w8 = w8s[wi]
                    for i in range(PEER_IG):
                        for p in range(8):
                            S.op("pe", lambda e, i=i, p=p: e.matmul(
                                psB[:, i * 128:(i + 1) * 128], lhsT=w8[:, p, i, :], rhs=identb[:],
                                start=(p == 0), stop=(p == 7)), r=["pw%d_%d" % (wi, p), "identb"], w=["B%d" % (i // 4)])

                def wb_pe(ig, tt):
                    WT = WTs[ig % 2]
                    S.op("act", lambda e: e.copy(out=WT[:, :, tt * 128:(tt + 1) * 128],
                                                 in_=psB[:, 0:1024].rearrange("p (i t) -> p i t", t=128)),
                         r=["B0", "B1"], w=["WT%d_%d" % (ig % 2, tt)])

                def load_u(n):
                    ub = n % 2
                    S.dma("pool", uTg[ub][:], uTl[:, :, n * PEER_EG * 128:(n + 1) * PEER_EG * 128], w=["uTg%d" % ub])

                def load_v(n):
                    ub = n % 2
                    S.dma("pool", vg[ub][:], pvl[:, n * PEER_EG:(n + 1) * PEER_EG, :], w=["vg%d" % ub])

                def ao_act(n, ec):
                    ub = n % 2
                    for dc in range(16):
                        S.op("pe", lambda e, dc=dc: e.matmul(
                            psA[:, ec * 512:ec * 512 + PEER_TG], lhsT=uTg[ub][:, dc, ec * 128:(ec + 1) * 128], rhs=hTg[:, dc, :],
                            start=(dc == 0), stop=(dc == 15)), r=["uTg%d" % ub, "hTg"], w=["A%d" % ec])

                def ao_gelu(n):
                    for ec in range(PEER_EG):
                        S.op("act", lambda e, ec=ec: e.activation(out=gl1[:, ec, :], in_=psA[:, ec * 512:ec * 512 + PEER_TG], func=AF.Gelu),
                             r=["A%d" % ec], w=["gl_%d" % ec])

                def ao_gmult(n):
                    ub = n % 2
                    ig, eg2 = n // NE2, n % NE2
                    WT = WTs[ig % 2]
                    wkeys = ["WT%d_%d" % (ig % 2, tt) for tt in range(TPG)]
                    for ec in range(PEER_EG):
                        il = eg2 * PEER_EG + ec
                        S.op("dve", lambda e, ec=ec, il=il: e.tensor_tensor(
                            out=G[ub][:, ec, :], in0=gl1[:, ec, :], in1=WT[:, il, :], op=ALU.mult),
                            r=["gl_%d" % ec] + wkeys, w=["G%d" % ub])

                def ao_outacc(n, j):
                    ub = n % 2
                    tt, dq = j // 4, j % 4
                    bk = 2 + (j % 2)
                    for ec in range(PEER_EG):
                        S.op("pe", lambda e, ec=ec: e.matmul(
                            bankB(bk), lhsT=G[ub][:, ec, tt * 128:(tt + 1) * 128], rhs=vg[ub][:, ec, dq * 512:(dq + 1) * 512],
                            start=(ec == 0), stop=(ec == PEER_EG - 1)), r=["G%d" % ub, "vg%d" % ub], w=["B%d" % bk])
                    ok = "outacc%d_%d" % (tt, dq // 2)
                    dst = outacc[:, tt, dq * 512:(dq + 1) * 512]
                    if n == 0:
                        S.op("dve", lambda e: e.tensor_copy(out=dst, in_=bankB(bk)), r=["B%d" % bk], w=[ok])
                    else:
                        S.op("dve", lambda e: e.tensor_tensor(out=dst, in0=bankB(bk), in1=dst, op=ALU.add), r=["B%d" % bk, ok], w=[ok])

                NSTEP = NIG * NE2
                WPS = TPG // NE2
                NOUT = TPG * 4
                assert TPG % NE2 == 0 and PEER_EG == 4
                for tg in range(NTG):
                    t0 = tg * PEER_TG
                    S.dma("sp", hTg[:], hT_d[:, :, t0:t0 + PEER_TG], r=["D:hT"], w=["hTg"])
                    for tt in range(TPG):
                        S.dma("act", s1g[:, tt], ps_v[:, tg * TPG + tt, :, 1, :], r=["D:ps"], w=["s1g"])
                    S.dma("act", stg_[:], pst_d[t0:t0 + PEER_TG, :].rearrange("(tt p) k -> p tt k", p=128), r=["D:pst"], w=["pstg"])
                    s0_load(tg, 0)
                    s0_load(tg, 1)
                    load_u(0)
                    load_v(0)
                    tile_n = [0]
                    pending = [None]

                    def flush_cast():
                        if pending[0] is not None:
                            wb_pe(*pending[0])
                            pending[0] = None

                    for tt in range(TPG):
                        wi = tile_n[0] % 2
                        tile_n[0] += 1
                        for p in range(8):
                            wb_p(0, tt, p, wi)
                        wb_mm(wi)
                        wb_pe(0, tt)
                    for n in range(NSTEP + 1):
                        ig, k = n // NE2, n % NE2
                        building = n < NSTEP and ig + 1 < NIG
                        if n < NSTEP:
                            if n + 1 < NSTEP:
                                load_u(n + 1)
                            if n >= 1:
                                load_v(n)
                        for wt in range(WPS):
                            wi = tile_n[0] % 2
                            if building:
                                tile_n[0] += 1
                            for p in range(8):
                                if building:
                                    wb_p(ig + 1, k * WPS + wt, p, wi)
                                if n >= 1:
                                    ao_outacc(n - 1, wt * 8 + p)
                                if p == 2:
                                    flush_cast()
                                if n < NSTEP and p in (1, 5):
                                    ao_act(n, wt * 2 + (1 if p == 5 else 0))
                            if building:
                                wb_mm(wi)
                                pending[0] = (ig + 1, k * WPS + wt)
                        if n < NSTEP:
                            ao_gelu(n)
                            if k == NE2 - 1 and ig + 2 < NIG:
                                s0_load(tg, ig + 2)
                            if not building:
                                flush_cast()
                            ao_gmult(n)
                    flush_cast()
                    for tt in range(TPG):
                        r0 = t0 + tt * 128
                        for hf in range(2):
                            c0 = hf * 1024
                            xh = vbuf[0][:].rearrange("p a b -> p (a b)")
                            th = vbuf[1][:].rearrange("p a b -> p (a b)")
                            S.dma("sp", xh, xs[r0:r0 + 128, c0:c0 + 1024], r=["D:xs"], w=["pv0"] + ["pva0_%d" % i for i in range(PEER_IG // 2, PEER_IG)])
                            pv1k = ["pv1"] + ["pva1_%d" % i for i in range(PEER_IG // 2, PEER_IG)]
                            S.dma("act", th, gate_row(l, 1)[:, c0:c0 + 1024].partition_broadcast(128), r=["D:modrow"], w=pv1k)
                            S.op("dve", lambda e, tt=tt, c0=c0, th=th: e.tensor_tensor(out=th, in0=outacc[:, tt, c0:c0 + 1024], in1=th, op=ALU.mult),
                                 r=["outacc%d_%d" % (tt, hf)] + pv1k, w=pv1k)
                            S.op("pool", lambda e, xh=xh, th=th: e.tensor_tensor(out=xh, in0=xh, in1=th, op=ALU.add), r=["pv0"] + pv1k, w=["pv0"])
                            S.dma("sp", xs[r0:r0 + 128, c0:c0 + 1024], xh, r=["pv0"], w=["D:xs"])

        def final_phase():
            S.barrier()
            with contextlib.ExitStack() as ph:
                fgb = sb(ph, "fgb", [128, D])
                S.dma("sp", fgb[:], fg.partition_broadcast(128), w=["fgb"])
                xt = [sb(ph, "fx%d" % i, [128, D]) for i in range(2)]
                junk = sb(ph, "fjunk", [128, D])
                st = sb(ph, "fst", [128, 4])
                for tt in range(NTT):
                    xb = xt[tt % 2]
                    xk = "fx%d" % (tt % 2)
                    S.dma("sp" if tt % 2 else "act", xb[:], xs[tt * 128:(tt + 1) * 128, :], r=["D:xs"], w=[xk])
                    S.op("act", lambda e, xb=xb: e.activation(out=junk[:], in_=xb[:], func=AF.Square, accum_out=st[:, 0:1]),
                         r=[xk], w=["fjunk", "fst"])
                    S.op("dve", lambda e: e.tensor_scalar(out=st[:, 1:2], in0=st[:, 0:1], scalar1=1.0 / D, scalar2=EPS, op0=ALU.mult, op1=ALU.add),
                         r=["fst"], w=["fst"])
                    S.op("act", lambda e: e.activation(out=st[:, 1:2], in_=st[:, 1:2], func=AF.Sqrt), r=["fst"], w=["fst"])
                    S.op("dve", lambda e: e.reciprocal(out=st[:, 2:3], in_=st[:, 1:2]), r=["fst"], w=["fst"])
                    S.op("dve", lambda e, xb=xb: e.scalar_tensor_tensor(out=xb[:], in0=xb[:], scalar=st[:, 2:3], in1=fgb[:],
                                                                        op0=ALU.mult, op1=ALU.mult), r=[xk, "fst", "fgb"], w=[xk])
                    S.dma("sp", out_d[tt * 128:(tt + 1) * 128, :], xb[:], r=[xk], w=["D:out"])

        def run_layers():
            for l in range(nlayers):
                norm_phase("n1_%d" % l, x_in if l == 0 else xs, l, 0)
                if stop == "norm1":
                    return
                proj_phase(l)
                if stop == "proj":
                    return
                attn_phase(l)
                if stop in ("attnB", "attnC", "attn"):
                    return
                outproj_phase(l)
                if stop == "outproj":
                    return
                norm_phase("n2_%d" % l, xs, l, 1)
                peerA_phase(l)
                if stop == "peerA":
                    return
                peerB_phase(l)
                if stop == "peerB":
                    return
            final_phase()

        run_layers()
        S.finish("sp")
    return nc


def _rope_tables():
    def tab(hd):
        half = hd // 2
        inv = (10000.0 ** (-np.arange(half, dtype=np.float32) / half)).astype(np.float32)
        ang = np.arange(SEQ, dtype=np.float32)[:, None] * inv[None, :]
        cos = np.cos(ang).astype(np.float32).T
        sin = np.sin(ang).astype(np.float32).T
        cos_f = np.concatenate([cos, cos], axis=0)
        sin_s = np.concatenate([-sin, sin], axis=0)
        reps = 128 // hd
        out = np.stack([np.tile(cos_f, (reps, 1)), np.tile(sin_s, (reps, 1))], axis=1)
        return np.ascontiguousarray(out, dtype=np.float32)
    return tab(128), tab(64)


def prep_inputs(x, c, ada_w, ada_b, norm1_g, norm2_g, w_in, out_norm_g, w_out,
                peer_wq, peer_subkeys, peer_u, peer_v, final_g):
    f = lambda a: np.ascontiguousarray(np.asarray(a), dtype=np.float32)
    x, c = f(x), f(c)
    cs128, cs64 = _rope_tables()
    shared = {
        "ada_w": f(ada_w),
        "ada_bT": f(np.asarray(ada_b).reshape(DEPTH, 96, 128).transpose(2, 0, 1).reshape(128, DEPTH * 96)),
        "gT": f(np.stack([np.asarray(norm1_g).reshape(DEPTH, 16, 128), np.asarray(norm2_g).reshape(DEPTH, 16, 128)], axis=1)
                .transpose(3, 0, 1, 2).reshape(128, DEPTH * 32)),
        "w_in": f(w_in),
        "og": f(out_norm_g),
        "w_out": f(w_out),
        "peer_wq": f(peer_wq),
        "skT": f(np.asarray(peer_subkeys).reshape(DEPTH, 16, 128, 128).transpose(0, 1, 3, 2)),
        "uT": f(np.asarray(peer_u).transpose(0, 2, 1)),
        "pv": f(peer_v),
        "fg": f(np.asarray(final_g).reshape(1, D)),
        "ident": np.eye(128, dtype=np.float32),
        "cs128": cs128,
        "cs64": cs64,
    }
    maps = []
    for b in range(x.shape[0]):
        m = dict(shared)
        m["x"] = x[b]
        m["cT"] = f(c[b].reshape(16, 128).T)
        maps.append(m)
    return maps


def kernel(**inputs):
    maps = prep_inputs(**inputs)
    nc = build()
    res = run_bass_kernel_spmd(nc, maps, core_ids=list(range(len(maps))))
    return np.stack([r["out"] for r in res.results], axis=0).astype(np.float32)
```
